# Optimizing a Trainium2 kernel written in Bass

```python
import jax, jax.numpy as jnp
from jax import lax
import numpy as np

D_MODEL = 1024
BATCH = 16
SEQ = 2048
DEPTH = 4

GRID_W = 64
CTX_LEN = 256
N_MIXERS = 2
N_A_LAYERS = (DEPTH + N_MIXERS - 1) // N_MIXERS
N_B_LAYERS = DEPTH // N_MIXERS
DN_ALPHA = (2.0 * DEPTH) ** 0.25
DN_BETA = (8.0 * DEPTH) ** -0.25
LN_EPS = 1e-5
CHUNK = 128
ROWS_PER_CHUNK = CHUNK // GRID_W
A_WIDTH = D_MODEL
A_HEADS = 8
A_GROUP = A_WIDTH // A_HEADS
GLA_HEADS = 4
GLA_DK = D_MODEL // 2 // GLA_HEADS
GLA_DV = D_MODEL // GLA_HEADS
GLA_QK = GLA_HEADS * GLA_DK
GLA_V = GLA_HEADS * GLA_DV
GLA_GATE_RANK = 16
GLA_TAU = 16.0
GLA_CHUNK = 64
GLA_IN = 2 * GLA_QK + 2 * GLA_V + 2 * GLA_GATE_RANK
PEER_HEADS = 8
PEER_NKEYS = 128
PEER_EXPERTS = PEER_NKEYS * PEER_NKEYS
PEER_QDIM = 128
PEER_HALF = PEER_QDIM // 2
PEER_TOPK = 16
PEER_BLOCK = 128

kernel_name = "hybrid_chunkmlp_gla_peer_prefix_dit"


def layer_norm(x, g, b):
    xf = x.astype(jnp.float32)
    mu = jnp.mean(xf, axis=-1, keepdims=True)
    var = jnp.mean(jnp.square(xf - mu), axis=-1, keepdims=True)
    y = (xf - mu) * lax.rsqrt(var + LN_EPS)
    return (y * g + b).astype(x.dtype)


def chunk_mlp(h, n_chunks, w_in, norm_g, norm_b, w_s, b_s, w_out):
    bsz, t, _ = h.shape
    z = jax.nn.gelu(h @ w_in)
    u, v = jnp.split(z, 2, axis=-1)
    v = layer_norm(v, norm_g, norm_b)
    vb = v.reshape(bsz, n_chunks, CHUNK, A_HEADS, A_GROUP)
    s = jnp.einsum('hpq,bnqhc->bnphc', w_s, vb) + b_s.T[None, None, :, :, None]
    return (u * s.reshape(bsz, t, A_WIDTH)) @ w_out


def gla_chunked(q, k, v, g, s0):
    bsz, t, h, dk = q.shape
    dv = v.shape[-1]
    n = t // GLA_CHUNK
    f32 = jnp.float32
    q = q.astype(f32).reshape(bsz, n, GLA_CHUNK, h, dk)
    k = k.astype(f32).reshape(bsz, n, GLA_CHUNK, h, dk)
    v = v.astype(f32).reshape(bsz, n, GLA_CHUNK, h, dv)
    b = jnp.cumsum(g.astype(f32).reshape(bsz, n, GLA_CHUNK, h, dk), axis=2)
    b_end = b[:, :, -1:]
    q_in = q * jnp.exp(b)
    k_in = k * jnp.exp(-b)
    k_end = k * jnp.exp(b_end - b)
    att = jnp.einsum('bnihd,bnjhd->bnhij', q_in, k_in)
    mask = jnp.tril(jnp.ones((GLA_CHUNK, GLA_CHUNK), dtype=bool))
    att = jnp.where(mask, att, 0.0)
    o_intra = jnp.einsum('bnhij,bnjhe->bnihe', att, v)

    def step(state, inp):
        qc, kc, vc, dc = inp
        o = jnp.einsum('bihd,bhde->bihe', qc, state)
        state = dc[..., None] * state + jnp.einsum('bjhd,bjhe->bhde', kc, vc)
        return state, o

    xs = (jnp.moveaxis(q_in, 1, 0), jnp.moveaxis(k_end, 1, 0), jnp.moveaxis(v, 1, 0),
          jnp.moveaxis(jnp.exp(b_end[:, :, 0]), 1, 0))
    s_fin, o_inter = lax.scan(step, s0.astype(f32), xs)
    o = o_intra + jnp.moveaxis(o_inter, 0, 1)
    return o.reshape(bsz, t, h, dv), s_fin


def gla_reverse(q, k, v, g, s0):
    o, s_fin = gla_chunked(jnp.flip(q, 1), jnp.flip(k, 1), jnp.flip(v, 1), jnp.flip(g, 1), s0)
    return jnp.flip(o, 1), s_fin


def gla_project(h, w_in, w_gate, gate_bias):
    bsz, t, _ = h.shape
    z = h @ w_in
    q, k, v, r, gl = jnp.split(z, [GLA_QK, 2 * GLA_QK, 2 * GLA_QK + GLA_V, 2 * GLA_QK + 2 * GLA_V], axis=-1)
    q = q.reshape(bsz, t, GLA_HEADS, GLA_DK) * (GLA_DK ** -0.5)
    k = k.reshape(bsz, t, GLA_HEADS, GLA_DK)
    v = v.reshape(bsz, t, GLA_HEADS, GLA_DV)
    gl = gl.reshape(bsz, t, 2, GLA_GATE_RANK)
    zg = (jnp.einsum('btjr,jrd->btjd', gl, w_gate) + gate_bias).astype(jnp.float32)
    glog = (jax.nn.log_sigmoid(zg) / GLA_TAU).reshape(bsz, t, 2, GLA_HEADS, GLA_DK)
    return q, k, v, r, glog


def gla_readout(o, r, gn_g, w_out):
    bsz, t = o.shape[:2]
    mu = jnp.mean(o, axis=-1, keepdims=True)
    var = jnp.mean(jnp.square(o - mu), axis=-1, keepdims=True)
    y = ((o - mu) * lax.rsqrt(var + LN_EPS)).reshape(bsz, t, GLA_V) * gn_g
    return (y.astype(r.dtype) * jax.nn.silu(r)) @ w_out


def gla_mixer(h_lat, h_ctx, w_in, w_gate, gate_bias, gn_g, w_out, need_ctx_out):
    qc, kc, vc, rc, gc = gla_project(h_ctx, w_in, w_gate, gate_bias)
    ql, kl, vl, rl, gll = gla_project(h_lat, w_in, w_gate, gate_bias)
    s0 = jnp.zeros((h_ctx.shape[0], GLA_HEADS, GLA_DK, GLA_DV), jnp.float32)
    o_cf, s_f = gla_chunked(qc, kc, vc, gc[:, :, 0], s0)
    o_cb, s_b = gla_reverse(qc, kc, vc, gc[:, :, 1], s0)
    o_lf, _ = gla_chunked(ql, kl, vl, gll[:, :, 0], s_f)
    o_lb, _ = gla_reverse(ql, kl, vl, gll[:, :, 1], s_b)
    out_lat = gla_readout(o_lf + o_lb, rl, gn_g, w_out)
    out_ctx = gla_readout(o_cf + o_cb, rc, gn_g, w_out) if need_ctx_out else None
    return out_lat, out_ctx


def peer_ffn(h, w_q, sub_keys, u_tab, v_tab):
    bsz, t, d = h.shape
    blocks = h.reshape(-1, PEER_BLOCK, d)

    def one_block(xb):
        q = (xb @ w_q).reshape(PEER_BLOCK, PEER_HEADS, 2, PEER_HALF)
        s = jnp.einsum('thpd,pkd->thpk', q, sub_keys).astype(jnp.float32)
        s1, i1 = lax.top_k(s[:, :, 0], PEER_TOPK)
        s2, i2 = lax.top_k(s[:, :, 1], PEER_TOPK)
        cand_s = (s1[..., :, None] + s2[..., None, :]).reshape(PEER_BLOCK, PEER_HEADS, PEER_TOPK * PEER_TOPK)
        cand_i = (i1[..., :, None] * PEER_NKEYS + i2[..., None, :]).reshape(PEER_BLOCK, PEER_HEADS, PEER_TOPK * PEER_TOPK)
        top_s, pos = lax.top_k(cand_s, PEER_TOPK)
        idx = jnp.take_along_axis(cand_i, pos, axis=-1)
        wts = jax.nn.softmax(top_s, axis=-1).astype(xb.dtype)
        act = jax.nn.gelu(jnp.einsum('thkd,td->thk', u_tab[idx], xb)) * wts
        return jnp.einsum('thk,thkd->td', act, v_tab[idx])

    return lax.map(one_block, blocks).reshape(bsz, t, d)


def setup_inputs(seed: int = 0) -> dict:
    key = jax.random.key(seed)
    ks = jax.random.split(key, 24)
    f32 = jnp.float32

    def nrm(k, shape, s):
        return jax.random.normal(k, shape, f32) * s

    d = D_MODEL
    return {
        "x": nrm(ks[0], (BATCH, SEQ, d), 1.0),
        "c": nrm(ks[1], (BATCH, d), 1.0),
        "ctx": nrm(ks[2], (BATCH, CTX_LEN, d), 1.0),
        "c_ctx": nrm(ks[3], (d,), 1.0),
        "w_mod": nrm(ks[4], (DEPTH, d, 6 * d), 0.5 * d ** -0.5),
        "b_mod": nrm(ks[5], (DEPTH, 6 * d), 0.01),
        "ln_g": 1.0 + nrm(ks[6], (DEPTH, 2, d), 0.01),
        "ln_b": nrm(ks[7], (DEPTH, 2, d), 0.01),
        "a_w_in": nrm(ks[8], (N_A_LAYERS, d, 2 * A_WIDTH), d ** -0.5),
        "a_norm_g": 1.0 + nrm(ks[9], (N_A_LAYERS, A_WIDTH), 0.01),
        "a_norm_b": nrm(ks[10], (N_A_LAYERS, A_WIDTH), 0.01),
        "a_w_s": nrm(ks[11], (N_A_LAYERS, A_HEADS, CHUNK, CHUNK), CHUNK ** -0.5),
        "a_b_s": 1.0 + nrm(ks[12], (N_A_LAYERS, A_HEADS, CHUNK), 0.01),
        "a_w_out": nrm(ks[13], (N_A_LAYERS, A_WIDTH, d), DN_BETA * A_WIDTH ** -0.5),
        "b_w_in": nrm(ks[14], (N_B_LAYERS, d, GLA_IN), d ** -0.5),
        "b_w_gate": nrm(ks[15], (N_B_LAYERS, 2, GLA_GATE_RANK, GLA_QK), GLA_GATE_RANK ** -0.5),
        "b_gate_bias": nrm(ks[16], (N_B_LAYERS, 2, GLA_QK), 0.01),
        "b_gn_g": 1.0 + nrm(ks[17], (N_B_LAYERS, GLA_V), 0.01),
        "b_w_out": nrm(ks[18], (N_B_LAYERS, GLA_V, d), DN_BETA * GLA_V ** -0.5),
        "p_w_q": nrm(ks[19], (DEPTH, d, PEER_HEADS * PEER_QDIM), d ** -0.5),
        "p_keys": nrm(ks[20], (DEPTH, 2, PEER_NKEYS, PEER_HALF), PEER_HALF ** -0.5),
        "p_u": nrm(ks[21], (DEPTH, PEER_EXPERTS, d), d ** -0.5),
        "p_v": nrm(ks[22], (DEPTH, PEER_EXPERTS, d), DN_BETA),
    }


def reference(x, c, ctx, c_ctx, w_mod, b_mod, ln_g, ln_b,
              a_w_in, a_norm_g, a_norm_b, a_w_s, a_b_s, a_w_out,
              b_w_in, b_w_gate, b_gate_bias, b_gn_g, b_w_out,
              p_w_q, p_keys, p_u, p_v):
    bsz, seq, d = x.shape
    rows = seq // GRID_W
    lat_chunks = rows // ROWS_PER_CHUNK
    ctx_chunks = ctx.shape[1] // CHUNK
    sc = jax.nn.silu(c)
    sc_ctx = jax.nn.silu(c_ctx)
    for i in range(DEPTH):
        last = i == DEPTH - 1
        m = (sc @ w_mod[i] + b_mod[i]).reshape(bsz, 6, d)
        mc = (sc_ctx @ w_mod[i] + b_mod[i]).reshape(6, d)
        sh1, s1, g1, sh2, s2, g2 = [m[:, j, None, :] for j in range(6)]
        sh1c, s1c, g1c, sh2c, s2c, g2c = [mc[j] for j in range(6)]
        h_lat = x * (1.0 + s1) + sh1
        if i % N_MIXERS == 0:
            j = i // N_MIXERS
            prm = (a_w_in[j], a_norm_g[j], a_norm_b[j], a_w_s[j], a_b_s[j], a_w_out[j])
            mix_lat = chunk_mlp(h_lat, lat_chunks, *prm)
            mix_ctx = None if last else chunk_mlp(ctx * (1.0 + s1c) + sh1c, ctx_chunks, *prm)
        else:
            j = i // N_MIXERS
            h_ctx = ctx * (1.0 + s1c) + sh1c
            mix_lat, mix_ctx = gla_mixer(h_lat, h_ctx, b_w_in[j], b_w_gate[j], b_gate_bias[j],
                                         b_gn_g[j], b_w_out[j], not last)
        x = layer_norm(DN_ALPHA * x + g1 * mix_lat, ln_g[i, 0], ln_b[i, 0])
        ffn_lat = peer_ffn(x * (1.0 + s2) + sh2, p_w_q[i], p_keys[i], p_u[i], p_v[i])
        x = layer_norm(DN_ALPHA * x + g2 * ffn_lat, ln_g[i, 1], ln_b[i, 1])
        if not last:
            ctx = layer_norm(DN_ALPHA * ctx + g1c * mix_ctx, ln_g[i, 0], ln_b[i, 0])
            ffn_ctx = peer_ffn(ctx * (1.0 + s2c) + sh2c, p_w_q[i], p_keys[i], p_u[i], p_v[i])
            ctx = layer_norm(DN_ALPHA * ctx + g2c * ffn_ctx, ln_g[i, 1], ln_b[i, 1])
    return x
```

```python
import contextlib
import numpy as np
import concourse.bass as bass
import concourse.mybir as mybir
from concourse.bass_utils import run_bass_kernel_spmd

F32 = mybir.dt.float32
F32R = mybir.dt.float32r
BF16 = mybir.dt.bfloat16
I32 = mybir.dt.int32
U32 = mybir.dt.uint32
ALU = mybir.AluOpType
AF = mybir.ActivationFunctionType
AX = mybir.AxisListType

D = 1024
SEQ = 2048
CTX = 256
DEPTH = 4
NCORES = 8
TCTX = CTX // 128
TLAT = SEQ // 128
TPB = TCTX + TLAT
ALPHA = float((2.0 * DEPTH) ** 0.25)
EPS = 1e-5
NEXP = 16384
GSL = 2
NRING = 5

ENGS = ("pe", "dve", "act", "pool", "sp")
HANDLES = {"pe": "tensor", "dve": "vector", "act": "scalar", "pool": "gpsimd", "sp": "sync"}


class Res:
    __slots__ = ("name", "w", "r", "dsem_in", "dsem_out")

    def __init__(self, name):
        self.name = name
        self.w = None
        self.r = []
        self.dsem_in = None
        self.dsem_out = None


class Prog:
    def __init__(self, nc):
        self.nc = nc
        self.gstack = contextlib.ExitStack()
        self.pstack = None
        self.ops = {e: [] for e in ENGS}
        self.sems = {}
        self.semval = {}
        self.waited = {e: {} for e in ENGS}
        for e in ENGS:
            if e != "sp":
                self._newsem("P_" + e)
        self.ndsem = 0
        self.free_dsems = []
        self.phase_dsems = []
        self.banks = []
        self.bank_i = 0
        self.bank_mod = 8
        self.nops = 0
        self.rec = None

    def _newsem(self, key):
        h = self.gstack.enter_context(self.nc.semaphore(key))
        self.sems[key] = h
        self.semval[key] = 0
        return key

    def dsem(self):
        if self.free_dsems:
            k = self.free_dsems.pop()
        else:
            self.ndsem += 1
            k = self._newsem("D%d" % self.ndsem)
        self.phase_dsems.append(k)
        return k

    def swsem(self, i):
        k = "SW%d" % i
        if k not in self.sems:
            self._newsem(k)
        return k

    def gsbuf(self, name, shape, dt):
        t = self.gstack.enter_context(self.nc.sbuf_tensor(name, list(shape), dt))
        r = Res("sb:" + name)
        self.ndsem += 1
        r.dsem_in = self._newsem("G%d" % self.ndsem)
        return t, r

    def sbuf(self, name, shape, dt):
        name = "%s_p%d" % (name, self.phase_id)
        t = self.pstack.enter_context(self.nc.sbuf_tensor(name, list(shape), dt))
        return t, Res("sb:" + name)

    def init_banks(self):
        for i in range(8):
            t = self.gstack.enter_context(self.nc.psum_tensor("bank%d" % i, [128, 512], F32))
            self.banks.append((t, Res("ps:bank%d" % i)))

    def bank(self):
        b = self.banks[self.bank_i % self.bank_mod]
        self.bank_i += 1
        return b

    def begin_phase(self):
        self.phase_id = getattr(self, "phase_id", 0) + 1
        self.pstack = contextlib.ExitStack()
        self.ops = {e: [] for e in ENGS}
        self.phase_dsems = []

    def end_phase(self, final=False):
        self.emit(final)
        self.pstack.close()
        self.pstack = None
        for e in ENGS:
            for k, v in self.semval.items():
                self.waited[e][k] = v
        self.free_dsems.extend(self.phase_dsems)
        self.phase_dsems = []

    def _waits(self, eng, reads, writes, mysem, acc):
        evs = []
        for r in reads:
            if r.w is not None:
                evs.append(r.w)
        for w in writes:
            if w.w is not None and not (acc and w.w[0] == mysem):
                evs.append(w.w)
            for ev in w.r:
                evs.append(ev)
        wd = self.waited[eng]
        best = {}
        for (k, v) in evs:
            if wd.get(k, 0) >= v:
                continue
            best[k] = max(best.get(k, 0), v)
        for k, v in best.items():
            wd[k] = v
        return list(best.items())

    def op(self, eng, fn, reads=(), writes=(), acc=False):
        if self.rec is not None:
            self.rec.append((self.op, (eng, fn, reads, writes, acc)))
            return None
        key = "P_" + eng
        waits = self._waits(eng, reads, writes, key, acc)
        self.semval[key] += 1
        ev = (key, self.semval[key])
        self.ops[eng].append((fn, waits, (key, 1)))
        self.nops += 1
        for r in reads:
            r.r.append(ev)
        for w in writes:
            w.w = ev
            w.r = []
        return ev

    def dma(self, eng, fn, reads=(), writes=(), sem=None):
        if self.rec is not None:
            self.rec.append((self.dma, (eng, fn, reads, writes, sem)))
            return None
        if sem is None:
            if writes and writes[0].name.startswith("sb:"):
                t = writes[0]
                if t.dsem_in is None:
                    t.dsem_in = self.dsem()
                sem = t.dsem_in
            else:
                t = reads[0]
                if t.dsem_out is None:
                    t.dsem_out = self.dsem()
                sem = t.dsem_out
        waits = self._waits(eng, reads, writes, sem, True)
        self.semval[sem] += 16
        ev = (sem, self.semval[sem])
        self.ops[eng].append((fn, waits, (sem, 16)))
        self.nops += 1
        for r in reads:
            r.r.append(ev)
        for w in writes:
            w.w = ev
            w.r = []
        return ev

    def emit(self, final=False):
        nc = self.nc
        sems = self.sems
        fin = [(k, v) for k, v in self.semval.items() if v > 0]
        with nc.Block() as block:
            for e in ENGS:
                ops = self.ops[e]

                def body(eh, ops=ops, e=e):
                    for fn, waits, inc in ops:
                        for k, v in waits:
                            eh.wait_ge(sems[k], v)
                        fn(eh).then_inc(sems[inc[0]], inc[1])
                    if e == "sp":
                        for k, v in fin:
                            eh.wait_ge(sems[k], v)

                getattr(block, HANDLES[e])(body)

    def close(self):
        self.gstack.close()


class Builder:
    def __init__(self, nb=2, depth=DEPTH, dbg=None):
        self.nb = nb
        self.depth = depth
        self.R = nb + 1
        self.dbg = dbg or {}
        nc = self.nc = bass.Bass("TRN2", target_bir_lowering=False)
        self.P = Prog(nc)
        dt = nc.dram_tensor
        A = {}

        def inp(name, shape, dtype=F32):
            A[name] = dt(name, list(shape), dtype, kind="ExternalInput").ap()

        inp("x", [nb, SEQ, D]); inp("c", [nb, D]); inp("ctx", [nb, CTX, D]); inp("c_ctx", [1, D])
        inp("w_mod", [DEPTH, D, 6 * D]); inp("b_mod", [DEPTH, 6 * D])
        inp("ln_g", [DEPTH, 2, D]); inp("ln_b", [DEPTH, 2, D])
        inp("a_w_in", [2, D, 2 * D]); inp("a_norm_g", [2, D]); inp("a_norm_b", [2, D])
        inp("a_w_s", [2, 8, 128, 128]); inp("a_b_s", [2, 8, 128]); inp("a_w_out", [2, D, D])
        inp("b_w_in", [2, D, 3104]); inp("b_w_gate", [2, 2, 16, 512]); inp("b_gate_bias", [2, 2, 512])
        inp("b_gn_g", [2, D]); inp("b_w_out", [2, D, D])
        inp("p_w_q", [DEPTH, D, D]); inp("p_keys", [DEPTH, 2, 128, 64])
        inp("p_u", [DEPTH * NEXP, D]); inp("p_v", [DEPTH * NEXP, D])
        A["y"] = dt("y", [nb, SEQ, D], F32, kind="ExternalOutput").ap()
        skind = "ExternalOutput" if self.dbg else "Internal"
        A["X0"] = dt("X0", [nb, CTX + SEQ, D], F32, kind=skind).ap()
        A["X1"] = dt("X1", [nb, CTX + SEQ, D], F32, kind=skind).ap()
        A["OF"] = dt("OF", [nb, CTX + SEQ, D], F32, kind="Internal").ap()
        self.A = A
        self.rX0 = Res("dr:X0"); self.rX1 = Res("dr:X1"); self.rOF = Res("dr:OF"); self.rY = Res("dr:y")
        self.rIN = Res("dr:in")

    def src_ap(self, layer, b, s):
        if layer == 0:
            if s < TCTX:
                return self.A["ctx"][b, s * 128:(s + 1) * 128, :], self.rIN
            return self.A["x"][b, (s - TCTX) * 128:(s - TCTX + 1) * 128, :], self.rIN
        return self.A["X0"][b, s * 128:(s + 1) * 128, :], self.rX0

    def row_of(self, b, s):
        return self.nb if s < TCTX else b

    def consts(self):
        P = self.P
        nb, R = self.nb, self.R
        self.ident, self.r_ident = P.gsbuf("ident", [128, 128], F32)
        self.tri = {}
        for nm in ("IF", "EF", "IB", "EB"):
            self.tri[nm] = P.gsbuf("tri" + nm, [128, 128], F32)
        self.maskF, self.r_maskF = P.gsbuf("maskF", [128, 4, 128], F32)
        self.maskB, self.r_maskB = P.gsbuf("maskB", [128, 4, 128], F32)
        self.ones1, self.r_ones1 = P.gsbuf("ones1", [1, 128], F32)
        self.iota16, self.r_iota16 = P.gsbuf("iota16", [128, 16], F32)
        self.sg, self.r_sg = P.gsbuf("sg", [128, R, 8], F32)
        self.thr17, self.r_thr17 = P.gsbuf("thr17", [128, 17], F32)
        self.modt, self.r_modt = P.gsbuf("modt", [128, R, 3, D], F32)
        self.lng, self.r_lng = P.gsbuf("lng", [128, D], F32)
        self.lnb, self.r_lnb = P.gsbuf("lnb", [128, D], F32)
        P.init_banks()

        P.begin_phase()
        cT, r_cT = P.sbuf("cT", [128, R, 8], F32)
        sg, r_sg = self.sg, self.r_sg
        tmpc, r_tmpc = P.sbuf("tmpc", [128, 4, 128], F32)
        ident = self.ident
        P.op("pool", lambda e: e.memset(ident[:], 0.0), writes=[self.r_ident])
        P.op("pool", lambda e: e.affine_select(out=ident[:], in_=ident[:], pattern=[[-1, 128]], compare_op=ALU.not_equal,
                                               fill=1.0, base=0, channel_multiplier=1),
             reads=[self.r_ident], writes=[self.r_ident])
        P.op("pool", lambda e: e.memset(tmpc[:], -1.0 / 16.0), writes=[r_tmpc])
        spec = {"IF": ([[1, 128]], -1, ALU.is_ge),
                "EF": ([[-1, 128]], 1, ALU.is_gt),
                "IB": ([[-1, 128]], 1, ALU.is_ge),
                "EB": ([[1, 128]], -1, ALU.is_gt)}
        for nm, (pat, cm, cmp) in spec.items():
            t, r = self.tri[nm]
            P.op("pool", lambda e, t=t, pat=pat, cm=cm, cmp=cmp: e.affine_select(
                out=t[:], in_=tmpc[:, 0, :], pattern=pat, compare_op=cmp, fill=0.0, base=0, channel_multiplier=cm),
                reads=[r_tmpc], writes=[r])
        ones4, r_ones4 = P.sbuf("ones4", [128, 4, 128], F32)
        P.op("pool", lambda e: e.memset(ones4[:], 1.0), writes=[r_ones4])
        mF, mB = self.maskF, self.maskB
        P.op("pool", lambda e: e.affine_select(out=mF[:], in_=ones4[:], pattern=[[0, 4], [1, 128]], compare_op=ALU.is_ge,
                                               fill=0.0, base=0, channel_multiplier=-1), reads=[r_ones4], writes=[self.r_maskF])
        P.op("pool", lambda e: e.affine_select(out=mB[:], in_=ones4[:], pattern=[[0, 4], [-1, 128]], compare_op=ALU.is_ge,
                                               fill=0.0, base=0, channel_multiplier=1), reads=[r_ones4], writes=[self.r_maskB])
        o1 = self.ones1
        P.op("pool", lambda e: e.memset(o1[:], 1.0), writes=[self.r_ones1])
        io = self.iota16
        P.op("pool", lambda e: e.iota(io[:], pattern=[[1, 16]], base=0, channel_multiplier=0,
                                      allow_small_or_imprecise_dtypes=True), writes=[self.r_iota16])
        for r in range(R):
            src = self.A["c"][r, :] if r < nb else self.A["c_ctx"][0, :]
            P.dma("sp", lambda e, r=r, src=src: e.dma_start(out=cT[:, r, :], in_=src.rearrange("(kc k) -> k kc", k=128),
                                                            allow_slow_non_contiguous=True),
                  reads=[self.rIN], writes=[r_cT])
        P.op("act", lambda e: e.activation(out=sg[:], in_=cT[:], func=AF.Sigmoid), reads=[r_cT], writes=[r_sg])
        P.op("dve", lambda e: e.tensor_tensor(out=sg[:], in0=sg[:], in1=cT[:], op=ALU.mult), reads=[r_sg, r_cT], writes=[r_sg])
        th = self.thr17
        P.op("pool", lambda e: e.iota(th[:], pattern=[[16, 17]], base=0, channel_multiplier=0,
                                      allow_small_or_imprecise_dtypes=True), writes=[self.r_thr17])
        P.end_phase()

    def mod_phase(self, layer, half):
        P = self.P
        R = self.R
        P.begin_phase()
        bm, r_bm = P.sbuf("bm", [1, 3 * D], F32)
        scr, r_screp = P.sbuf("screp", [128, R, 8, 128], F32)
        sg = self.sg
        for r in range(R):
            P.op("dve", lambda e, r=r: e.tensor_copy(out=scr[:, r, :, :],
                                                     in_=sg[:, r, :].unsqueeze(2).to_broadcast([128, 8, 128])),
                 reads=[self.r_sg], writes=[r_screp])
        wst = [P.sbuf("wst%d" % k, [128, 8, 512], F32) for k in range(2)]
        P.dma("sp", lambda e: e.dma_start(out=bm[:], in_=self.A["b_mod"][layer:layer + 1, half * 3 * D:(half + 1) * 3 * D]),
              reads=[self.rIN], writes=[r_bm])
        lng, lnb = self.lng, self.lnb
        P.dma("sp", lambda e: e.dma_start(out=lng[:], in_=self.A["ln_g"][layer, half, :].partition_broadcast(128)),
              reads=[self.rIN], writes=[self.r_lng])
        P.dma("sp", lambda e: e.dma_start(out=lnb[:], in_=self.A["ln_b"][layer, half, :].partition_broadcast(128)),
              reads=[self.rIN], writes=[self.r_lnb])
        modt, ones1 = self.modt, self.ones1
        it = 0
        for jj in range(3):
            for nn in range(2):
                c0 = (half * 3 + jj) * D + nn * 512
                w, r_w = wst[it % 2]
                it += 1
                P.dma("sp", lambda e, w=w, c0=c0: e.dma_start(
                    out=w[:], in_=self.A["w_mod"][layer, :, c0:c0 + 512].rearrange("(kc p) n -> p kc n", p=128)),
                    reads=[self.rIN], writes=[r_w])
                for r in range(R):
                    bk, r_bk = P.bank()
                    for kc in range(8):
                        P.op("pe", lambda e, bk=bk, r=r, kc=kc, w=w: e.matmul(bk[:], lhsT=scr[:, r, kc, :], rhs=w[:, kc, :],
                                                                             start=(kc == 0), stop=False),
                             reads=[r_screp, r_w], writes=[r_bk], acc=(kc > 0))
                    P.op("pe", lambda e, bk=bk, jj=jj, nn=nn: e.matmul(bk[:], lhsT=ones1[0:1, :],
                                                                       rhs=bm[0:1, jj * D + nn * 512: jj * D + nn * 512 + 512],
                                                                       start=False, stop=True),
                         reads=[self.r_ones1, r_bm], writes=[r_bk], acc=True)
                    addc = 1.0 if jj == 1 else 0.0
                    P.op("dve", lambda e, bk=bk, r=r, jj=jj, nn=nn, addc=addc: e.tensor_scalar(
                        out=modt[:, r, jj, nn * 512:(nn + 1) * 512], in0=bk[:], scalar1=addc, scalar2=None, op0=ALU.add),
                        reads=[r_bk], writes=[self.r_modt])
        P.end_phase()

    def load_w_bf16(self, dst, r_dst, src, K, N, stg):
        P = self.P
        it = 0
        for kc in range(K // 128):
            SW = stg[0][0].shape[-1]
            for n0 in range(0, N, SW):
                n1 = min(N, n0 + SW)
                s, r_s = stg[it % len(stg)]
                eng = ("act", "dve")[it % 2]
                it += 1
                P.dma("sp", lambda e, s=s, kc=kc, n0=n0, n1=n1: e.dma_start(out=s[:, 0:n1 - n0],
                                                                            in_=src[kc * 128:(kc + 1) * 128, n0:n1]),
                      reads=[self.rIN], writes=[r_s])
                if eng == "act":
                    P.op("act", lambda e, s=s, kc=kc, n0=n0, n1=n1: e.copy(out=dst[:, kc, n0:n1], in_=s[:, 0:n1 - n0]),
                         reads=[r_s], writes=[r_dst])
                else:
                    P.op("dve", lambda e, s=s, kc=kc, n0=n0, n1=n1: e.tensor_copy(out=dst[:, kc, n0:n1], in_=s[:, 0:n1 - n0]),
                         reads=[r_s], writes=[r_dst])

    def bcast_load(self, dst, r_dst, vec):
        self.P.dma("sp", lambda e: e.dma_start(out=dst[:], in_=vec.partition_broadcast(128)), reads=[self.rIN], writes=[r_dst])

    def modulate(self, out, r_out, xt, r_xt, row):
        P = self.P
        modt = self.modt
        P.op("dve", lambda e: e.tensor_tensor(out=out[:], in0=xt[:], in1=modt[:, row, 1, :], op=ALU.mult),
             reads=[r_xt, self.r_modt], writes=[r_out])
        P.op("dve", lambda e: e.tensor_tensor(out=out[:], in0=out[:], in1=modt[:, row, 0, :], op=ALU.add),
             reads=[r_out, self.r_modt], writes=[r_out])

    def transpose_to(self, dst, r_dst, src, r_src, nchunks=8):
        P = self.P
        ident = self.ident
        for g in range(0, nchunks, 4):
            bk, r_bk = P.bank()
            n = min(4, nchunks - g)
            for k in range(n):
                kc = g + k
                P.op("pe", lambda e, bk=bk, k=k, kc=kc: e.transpose(out=bk[:, k * 128:(k + 1) * 128],
                                                                    in_=src[:, kc * 128:(kc + 1) * 128], identity=ident[:]),
                     reads=[r_src, self.r_ident], writes=[r_bk], acc=(k > 0))
            P.op("act", lambda e, bk=bk, g=g, n=n: e.copy(out=dst[:, g:g + n, :].rearrange("p a b -> p (a b)"),
                                                          in_=bk[:, 0:n * 128]),
                 reads=[r_bk], writes=[r_dst])

    def layer_norm(self, out, r_out, yin, r_yin, sm, gb=None):
        P = self.P
        st, r_st = sm["st"]
        mv, r_mv = sm["mv"]
        sd, r_sd = sm["sd"]
        for h2 in range(2):
            P.op("dve", lambda e, h2=h2: e.bn_stats(out=st[:, h2, :], in_=yin[:, h2 * 512:(h2 + 1) * 512]),
                 reads=[r_yin], writes=[r_st])
        P.op("dve", lambda e: e.bn_aggr(out=mv[:], in_=st[:].rearrange("p a b -> p (a b)")), reads=[r_st], writes=[r_mv])
        P.op("act", lambda e: e.activation(out=sd[:, 0:1], in_=mv[:, 1:2], func=AF.Sqrt, bias=sm["eps"][0][:, 0:1], scale=1.0),
             reads=[r_mv, sm["eps"][1]], writes=[r_sd])
        P.op("dve", lambda e: e.reciprocal(out=sd[:, 1:2], in_=sd[:, 0:1]), reads=[r_sd], writes=[r_sd])
        P.op("dve", lambda e: e.tensor_scalar(out=out[:], in0=yin[:], scalar1=mv[:, 0:1], scalar2=sd[:, 1:2],
                                              op0=ALU.subtract, op1=ALU.mult),
             reads=[r_yin, r_mv, r_sd], writes=[r_out])
        if gb is not None:
            (g, r_g), (b, r_b) = gb
            P.op("dve", lambda e: e.tensor_tensor(out=out[:], in0=out[:], in1=g[:], op=ALU.mult), reads=[r_out, r_g], writes=[r_out])
            P.op("dve", lambda e: e.tensor_tensor(out=out[:], in0=out[:], in1=b[:], op=ALU.add), reads=[r_out, r_b], writes=[r_out])

    def small_scratch(self):
        P = self.P
        sm = {"st": P.sbuf("ln_st", [128, 2, 6], F32), "mv": P.sbuf("ln_mv", [128, 2], F32), "sd": P.sbuf("ln_sd", [128, 2], F32),
              "eps": P.sbuf("ln_eps", [128, 1], F32)}
        ep = sm["eps"][0]
        P.op("pool", lambda e: e.memset(ep[:], EPS), writes=[sm["eps"][1]])
        return sm

    def residual_ln_store(self, mix_banks, xt, r_xt, row, sm, ytile, x1tile, dst_ap, r_dst):
        P = self.P
        modt = self.modt
        y, r_y = ytile
        x1, r_x1 = x1tile
        if isinstance(mix_banks, list):
            for n2, (bk, r_bk) in enumerate(mix_banks):
                P.op("dve", lambda e, bk=bk, n2=n2: e.tensor_tensor(out=y[:, n2 * 512:(n2 + 1) * 512], in0=bk[:],
                                                                    in1=modt[:, row, 2, n2 * 512:(n2 + 1) * 512], op=ALU.mult),
                     reads=[r_bk, self.r_modt], writes=[r_y])
        else:
            mt, r_mt = mix_banks
            P.op("dve", lambda e: e.tensor_tensor(out=y[:], in0=mt[:], in1=modt[:, row, 2, :], op=ALU.mult),
                 reads=[r_mt, self.r_modt], writes=[r_y])
        P.op("dve", lambda e: e.scalar_tensor_tensor(out=y[:], in0=xt[:], scalar=ALPHA, in1=y[:], op0=ALU.mult, op1=ALU.add),
             reads=[r_xt, r_y], writes=[r_y])
        self.layer_norm(x1, r_x1, y, r_y, sm, gb=((self.lng, self.r_lng), (self.lnb, self.r_lnb)))
        P.dma("sp", lambda e: e.dma_start(out=dst_ap, in_=x1[:]), reads=[r_x1], writes=[r_dst])

    def cmlp_phase(self, layer):
        P = self.P
        A = self.A
        j = layer // 2
        last = layer == DEPTH - 1
        P.begin_phase()
        sm = self.small_scratch()
        stg = [P.sbuf("stg%d" % k, [128, 2048], F32) for k in range(2)]
        w_in, r_w_in = P.sbuf("w_in", [128, 8, 2048], BF16)
        w_out, r_w_out = P.sbuf("w_out", [128, 8, D], BF16)
        wsT, r_wsT = P.sbuf("wsT", [128, 8, 128], BF16)
        wsraw, r_wsraw = P.sbuf("wsraw", [128, 8, 128], F32)
        bsb, r_bsb = P.sbuf("bsb", [128, 8, 128], F32)
        ng, r_ng = P.sbuf("ng", [128, D], F32)
        nbt, r_nbt = P.sbuf("nbt", [128, D], F32)
        self.load_w_bf16(w_in, r_w_in, A["a_w_in"][j], D, 2 * D, stg)
        self.load_w_bf16(w_out, r_w_out, A["a_w_out"][j], D, D, stg)
        self.bcast_load(ng, r_ng, A["a_norm_g"][j, :])
        self.bcast_load(nbt, r_nbt, A["a_norm_b"][j, :])
        P.dma("sp", lambda e: e.dma_start(out=bsb[:].rearrange("p a b -> p (a b)"),
                                          in_=A["a_b_s"][j].rearrange("h p -> (h p)").partition_broadcast(128)),
              reads=[self.rIN], writes=[r_bsb])
        P.dma("sp", lambda e: e.dma_start(out=wsraw[:], in_=A["a_w_s"][j].rearrange("h p q -> p h q")),
              reads=[self.rIN], writes=[r_wsraw])
        self.transpose_to(wsT, r_wsT, wsraw[:].rearrange("p a b -> p (a b)"), r_wsraw)

        xts = [P.sbuf("xt%d" % k, [128, D], F32) for k in range(2)]
        hf, r_hf = P.sbuf("hf", [128, D], F32)
        hT, r_hT = P.sbuf("hT", [128, 8, 128], BF16)
        uT, r_uT = P.sbuf("uT", [128, 8, 128], F32)
        vs, r_vs = P.sbuf("vs", [128, D], F32)
        vn, r_vn = P.sbuf("vn", [128, D], BF16)
        vnf, r_vnf = P.sbuf("vnf", [128, D], F32)
        usT, r_usT = P.sbuf("usT", [128, 8, 128], BF16)
        tmp, r_tmp = P.sbuf("tmpu", [128, 512], F32)
        ytile = P.sbuf("yt", [128, D], F32)
        x1tile = P.sbuf("x1t", [128, D], F32)

        tiles = [(b, s) for b in range(self.nb) for s in range(TPB) if not (last and s < TCTX)]
        tiles = tiles[:self.dbg.get("max_tiles", 10 ** 9)]
        for ti, (b, s) in enumerate(tiles):
            xt, r_xt = xts[ti % 2]
            src, r_src = self.src_ap(layer, b, s)
            row = self.row_of(b, s)
            P.dma("sp", lambda e, xt=xt, src=src: e.dma_start(out=xt[:], in_=src), reads=[r_src], writes=[r_xt])
            self.modulate(hf, r_hf, xt, r_xt, row)
            self.transpose_to(hT, r_hT, hf, r_hf)
            for g in range(2):
                bk, r_bk = P.bank()
                for k in range(4):
                    fc = g * 4 + k
                    for kc in range(8):
                        P.op("pe", lambda e, bk=bk, k=k, fc=fc, kc=kc: e.matmul(
                            bk[:, k * 128:(k + 1) * 128], lhsT=w_in[:, kc, fc * 128:(fc + 1) * 128], rhs=hT[:, kc, :],
                            start=(kc == 0), stop=(kc == 7)),
                            reads=[r_w_in, r_hT], writes=[r_bk], acc=not (k == 0 and kc == 0))
                P.op("act", lambda e, bk=bk, g=g: e.activation(out=uT[:, g * 4:(g + 1) * 4, :].rearrange("p a b -> p (a b)"),
                                                               in_=bk[:], func=AF.Gelu_apprx_tanh),
                     reads=[r_bk], writes=[r_uT])
            for n2 in range(2):
                bk, r_bk = P.bank()
                for kc in range(8):
                    P.op("pe", lambda e, bk=bk, n2=n2, kc=kc: e.matmul(
                        bk[:], lhsT=hT[:, kc, :], rhs=w_in[:, kc, D + n2 * 512: D + (n2 + 1) * 512],
                        start=(kc == 0), stop=(kc == 7)),
                        reads=[r_w_in, r_hT], writes=[r_bk], acc=(kc > 0))
                P.op("act", lambda e, bk=bk, n2=n2: e.activation(out=vs[:, n2 * 512:(n2 + 1) * 512], in_=bk[:],
                                                                 func=AF.Gelu_apprx_tanh),
                     reads=[r_bk], writes=[r_vs])
            self.layer_norm(vnf, r_vnf, vs, r_vs, sm, gb=((ng, r_ng), (nbt, r_nbt)))
            P.op("act", lambda e: e.copy(out=vn[:], in_=vnf[:]), reads=[r_vnf], writes=[r_vn])
            for g in range(2):
                bk, r_bk = P.bank()
                for k in range(4):
                    hd = g * 4 + k
                    P.op("pe", lambda e, bk=bk, k=k, hd=hd: e.matmul(bk[:, k * 128:(k + 1) * 128],
                                                                     lhsT=vn[:, hd * 128:(hd + 1) * 128], rhs=wsT[:, hd, :],
                                                                     start=True, stop=True),
                         reads=[r_vn, r_wsT], writes=[r_bk], acc=(k > 0))
                P.op("dve", lambda e, bk=bk, g=g: e.tensor_tensor(
                    out=tmp[:], in0=bk[:], in1=bsb[:, g * 4:(g + 1) * 4, :].rearrange("p a b -> p (a b)"), op=ALU.add),
                    reads=[r_bk, r_bsb], writes=[r_tmp])
                P.op("dve", lambda e, g=g: e.tensor_tensor(
                    out=usT[:, g * 4:(g + 1) * 4, :].rearrange("p a b -> p (a b)"), in0=tmp[:],
                    in1=uT[:, g * 4:(g + 1) * 4, :].rearrange("p a b -> p (a b)"), op=ALU.mult),
                    reads=[r_tmp, r_uT], writes=[r_usT])
            mixb = []
            for n2 in range(2):
                bk, r_bk = P.bank()
                for fc in range(8):
                    P.op("pe", lambda e, bk=bk, n2=n2, fc=fc: e.matmul(bk[:], lhsT=usT[:, fc, :],
                                                                       rhs=w_out[:, fc, n2 * 512:(n2 + 1) * 512],
                                                                       start=(fc == 0), stop=(fc == 7)),
                         reads=[r_usT, r_w_out], writes=[r_bk], acc=(fc > 0))
                mixb.append((bk, r_bk))
            self.residual_ln_store(mixb, xt, r_xt, row, sm, ytile, x1tile, A["X1"][b, s * 128:(s + 1) * 128, :], self.rX1)
        P.end_phase()

    def gla_phase(self, layer):
        P = self.P
        A = self.A
        j = layer // 2
        last = layer == DEPTH - 1
        P.begin_phase()
        sm = self.small_scratch()
        ofs = [P.sbuf("of%d" % k, [128, D], F32) for k in range(2)]
        stg = ofs
        w_in, r_w_in = P.sbuf("gw_in", [128, 8, 3104], BF16)
        w_out, r_w_out = P.sbuf("gw_out", [128, 8, D], BF16)
        wg, r_wg = P.sbuf("wg", [16, 2, 512], F32)
        gbias, r_gbias = P.sbuf("gbias", [1, 2, 512], F32)
        gng, r_gng = P.sbuf("gng", [128, D], F32)
        self.load_w_bf16(w_in, r_w_in, A["b_w_in"][j], D, 3104, stg)
        self.load_w_bf16(w_out, r_w_out, A["b_w_out"][j], D, D, stg)
        self.bcast_load(gng, r_gng, A["b_gn_g"][j, :])
        P.dma("sp", lambda e: e.dma_start(out=wg[:], in_=A["b_w_gate"][j].rearrange("d r n -> r d n")),
              reads=[self.rIN], writes=[r_wg])
        P.dma("sp", lambda e: e.dma_start(out=gbias[:], in_=A["b_gate_bias"][j:j + 1, :, :]), reads=[self.rIN], writes=[r_gbias])

        xts = [P.sbuf("xt%d" % k, [128, D], F32) for k in range(2)]
        hf, r_hf = P.sbuf("hf", [128, D], F32)
        hT, r_hT = P.sbuf("hT", [128, 8, 128], BF16)
        v_sb, r_v = P.sbuf("v_sb", [128, D], BF16)
        r_sb, r_r = P.sbuf("r_sb", [128, D], F32)
        glT, r_glT = P.sbuf("glT", [16, 128], F32)
        e1, r_e1 = P.sbuf("e1", [128, 512], F32)
        lsp, r_lsp = P.sbuf("lsp", [128, 512], F32)
        ebT, r_ebT = P.sbuf("ebT", [128, 4, 128], F32)
        enbT, r_enbT = P.sbuf("enbT", [128, 4, 128], F32)
        ebx, r_ebx = e1, r_e1
        qiT, r_qiT = P.sbuf("qiT", [128, 4, 128], BF16)
        kiT, r_kiT = P.sbuf("kiT", [128, 4, 128], BF16)
        kend, r_kend = P.sbuf("kend", [128, 512], BF16)
        attm, r_attm = P.sbuf("attm", [128, 4, 128], BF16)
        S, r_S = P.sbuf("S", [128, 4, 256], F32)
        Sb, r_Sb = P.sbuf("Sb", [128, 4, 256], BF16)
        osum, r_osum = P.sbuf("osum", [128, D], F32)
        gst, r_gst = P.sbuf("gst", [128, 4, 6], F32)
        gmv, r_gmv = P.sbuf("gmv", [128, 4, 2], F32)
        gsd, r_gsd = P.sbuf("gsd", [128, 4, 2], F32)
        yf, r_yf = P.sbuf("yf", [128, D], F32)
        yT, r_yT = P.sbuf("yT", [128, 8, 128], BF16)
        ytile = (osum, r_osum)
        x1tile = (yf, r_yf)
        eps = sm["eps"]

        def project_and_scan(ti, b, s, d):
            xt, r_xt = xts[ti % 2]
            src, r_src = self.src_ap(layer, b, s)
            row = self.row_of(b, s)
            P.dma("sp", lambda e: e.dma_start(out=xt[:], in_=src), reads=[r_src], writes=[r_xt])
            self.modulate(hf, r_hf, xt, r_xt, row)
            self.transpose_to(hT, r_hT, hf, r_hf)
            triI, r_triI = self.tri["IF" if d == 0 else "IB"]
            triE, r_triE = self.tri["EF" if d == 0 else "EB"]
            mask, r_mask = (self.maskF, self.r_maskF) if d == 0 else (self.maskB, self.r_maskB)
            endcol = 127 if d == 0 else 0

            def proj_fm(col0):
                bk, r_bk = P.bank()
                for h in range(4):
                    for kc in range(8):
                        P.op("pe", lambda e, h=h, kc=kc: e.matmul(
                            bk[:, h * 128:(h + 1) * 128], lhsT=w_in[:, kc, col0 + h * 128: col0 + (h + 1) * 128], rhs=hT[:, kc, :],
                            start=(kc == 0), stop=(kc == 7)),
                            reads=[r_w_in, r_hT], writes=[r_bk], acc=not (h == 0 and kc == 0))
                return bk, r_bk

            def proj_tm(col0):
                bk, r_bk = P.bank()
                for kc in range(8):
                    P.op("pe", lambda e, kc=kc: e.matmul(bk[:], lhsT=hT[:, kc, :], rhs=w_in[:, kc, col0:col0 + 512],
                                                         start=(kc == 0), stop=(kc == 7)),
                         reads=[r_w_in, r_hT], writes=[r_bk], acc=(kc > 0))
                return bk, r_bk

            bkg, r_bkg = P.bank()
            gcol = 3072 + 16 * d
            for kc in range(8):
                P.op("pe", lambda e, kc=kc: e.matmul(bkg[0:16, 0:128], lhsT=w_in[:, kc, gcol:gcol + 16], rhs=hT[:, kc, :],
                                                     start=(kc == 0), stop=(kc == 7)),
                     reads=[r_w_in, r_hT], writes=[r_bkg], acc=(kc > 0))
            P.op("act", lambda e: e.copy(out=glT[:], in_=bkg[0:16, 0:128]), reads=[r_bkg], writes=[r_glT])
            bkz, r_bkz = P.bank()
            P.op("pe", lambda e: e.matmul(bkz[:], lhsT=glT[:], rhs=wg[:, d, :], start=True, stop=False),
                 reads=[r_glT, r_wg], writes=[r_bkz])
            P.op("pe", lambda e: e.matmul(bkz[:], lhsT=self.ones1[0:1, :], rhs=gbias[0:1, d, :], start=False, stop=True),
                 reads=[self.r_ones1, r_gbias], writes=[r_bkz], acc=True)
            P.op("act", lambda e: e.activation(out=e1[:], in_=bkz[:], func=AF.Exp, scale=-1.0), reads=[r_bkz], writes=[r_e1])
            P.op("act", lambda e: e.activation(out=lsp[:], in_=e1[:], func=AF.Ln, bias=1.0, scale=1.0), reads=[r_e1], writes=[r_lsp])
            bkb, r_bkb = P.bank()
            for h in range(4):
                P.op("pe", lambda e, h=h: e.matmul(bkb[:, h * 128:(h + 1) * 128], lhsT=lsp[:, h * 128:(h + 1) * 128], rhs=triI[:],
                                                   start=True, stop=True),
                     reads=[r_lsp, r_triI], writes=[r_bkb], acc=(h > 0))
            bkx, r_bkx = P.bank()
            P.op("pe", lambda e: e.matmul(bkx[:], lhsT=triE[:], rhs=lsp[:], start=True, stop=True),
                 reads=[r_lsp, r_triE], writes=[r_bkx])
            P.op("act", lambda e: e.activation(out=ebT[:].rearrange("p a b -> p (a b)"), in_=bkb[:], func=AF.Exp),
                 reads=[r_bkb], writes=[r_ebT])
            P.op("act", lambda e: e.activation(out=enbT[:].rearrange("p a b -> p (a b)"), in_=bkb[:], func=AF.Exp, scale=-1.0),
                 reads=[r_bkb], writes=[r_enbT])
            P.op("act", lambda e: e.activation(out=ebx[:], in_=bkx[:], func=AF.Exp), reads=[r_bkx], writes=[r_ebx])
            bq, r_bq = proj_fm(0)
            P.op("dve", lambda e: e.scalar_tensor_tensor(out=qiT[:].rearrange("p a b -> p (a b)"), in0=bq[:], scalar=128.0 ** -0.5,
                                                         in1=ebT[:].rearrange("p a b -> p (a b)"), op0=ALU.mult, op1=ALU.mult),
                 reads=[r_bq, r_ebT], writes=[r_qiT])
            bkT, r_bkT = proj_fm(512)
            P.op("dve", lambda e: e.tensor_tensor(out=kiT[:].rearrange("p a b -> p (a b)"), in0=bkT[:],
                                                  in1=enbT[:].rearrange("p a b -> p (a b)"), op=ALU.mult),
                 reads=[r_bkT, r_enbT], writes=[r_kiT])
            bk_, r_bk_ = proj_tm(512)
            P.op("dve", lambda e: e.tensor_tensor(out=kend[:], in0=bk_[:], in1=ebx[:], op=ALU.mult),
                 reads=[r_bk_, r_ebx], writes=[r_kend])
            for n2 in range(2):
                bv, r_bv = proj_tm(1024 + n2 * 512)
                P.op("act", lambda e, n2=n2, bv=bv: e.copy(out=v_sb[:, n2 * 512:(n2 + 1) * 512], in_=bv[:]),
                     reads=[r_bv], writes=[r_v])
            if d == 1:
                for n2 in range(2):
                    br, r_br = proj_tm(2048 + n2 * 512)
                    P.op("act", lambda e, n2=n2, br=br: e.activation(out=r_sb[:, n2 * 512:(n2 + 1) * 512], in_=br[:], func=AF.Silu),
                         reads=[r_br], writes=[r_r])
            bka, r_bka = P.bank()
            for h in range(4):
                P.op("pe", lambda e, h=h: e.matmul(bka[:, h * 128:(h + 1) * 128], lhsT=kiT[:, h, :], rhs=qiT[:, h, :],
                                                   start=True, stop=True),
                     reads=[r_kiT, r_qiT], writes=[r_bka], acc=(h > 0))
            P.op("dve", lambda e: e.tensor_tensor(out=attm[:].rearrange("p a b -> p (a b)"), in0=bka[:],
                                                  in1=mask[:].rearrange("p a b -> p (a b)"), op=ALU.mult),
                 reads=[r_bka, r_mask], writes=[r_attm])
            obanks = []
            for g in range(2):
                bo, r_bo = P.bank()
                for k in range(2):
                    h = g * 2 + k
                    P.op("pe", lambda e, bo=bo, k=k, h=h: e.matmul(bo[:, k * 256:(k + 1) * 256], lhsT=attm[:, h, :],
                                                                   rhs=v_sb[:, h * 256:(h + 1) * 256], start=True, stop=False),
                         reads=[r_attm, r_v], writes=[r_bo], acc=(k > 0))
                    P.op("pe", lambda e, bo=bo, k=k, h=h: e.matmul(bo[:, k * 256:(k + 1) * 256], lhsT=qiT[:, h, :],
                                                                   rhs=Sb[:, h, :], start=False, stop=True),
                         reads=[r_qiT, r_Sb], writes=[r_bo], acc=True)
                obanks.append((bo, r_bo))
            for g in range(2):
                bs, r_bs = P.bank()
                for k in range(2):
                    h = g * 2 + k
                    P.op("pe", lambda e, bs=bs, k=k, h=h: e.matmul(bs[:, k * 256:(k + 1) * 256], lhsT=kend[:, h * 128:(h + 1) * 128],
                                                                   rhs=v_sb[:, h * 256:(h + 1) * 256], start=True, stop=True),
                         reads=[r_kend, r_v], writes=[r_bs], acc=(k > 0))
                for k in range(2):
                    h = g * 2 + k
                    P.op("dve", lambda e, bs=bs, k=k, h=h: e.scalar_tensor_tensor(
                        out=S[:, h, :], in0=S[:, h, :], scalar=ebT[:, h, endcol:endcol + 1], in1=bs[:, k * 256:(k + 1) * 256],
                        op0=ALU.mult, op1=ALU.add),
                        reads=[r_S, r_ebT, r_bs], writes=[r_S])
            P.op("act", lambda e: e.copy(out=Sb[:].rearrange("p a b -> p (a b)"), in_=S[:].rearrange("p a b -> p (a b)")),
                 reads=[r_S], writes=[r_Sb])
            return obanks, (xt, r_xt), row

        def reset_state():
            P.op("pool", lambda e: e.memset(S[:], 0.0), writes=[r_S])
            P.op("pool", lambda e: e.memset(Sb[:], 0.0), writes=[r_Sb])

        ti = 0
        for b in range(self.nb):
            reset_state()
            for s in range(TPB):
                obanks, _, _ = project_and_scan(ti, b, s, 0)
                of, r_of = ofs[ti % 2]
                for g, (bo, r_bo) in enumerate(obanks):
                    P.op("act", lambda e, bo=bo, g=g, of=of: e.copy(out=of[:, g * 512:(g + 1) * 512], in_=bo[:]),
                         reads=[r_bo], writes=[r_of])
                P.dma("sp", lambda e, of=of, b=b, s=s: e.dma_start(out=A["OF"][b, s * 128:(s + 1) * 128, :], in_=of[:]),
                      reads=[r_of], writes=[self.rOF])
                ti += 1
        for b in range(self.nb):
            reset_state()
            order = [1, 0] + list(range(TPB - 1, TCTX - 1, -1))
            for s in order:
                of, r_of = ofs[ti % 2]
                if not (last and s < TCTX):
                    P.dma("sp", lambda e, of=of, b=b, s=s: e.dma_start(out=of[:], in_=A["OF"][b, s * 128:(s + 1) * 128, :]),
                          reads=[self.rOF], writes=[r_of])
                obanks, (xt, r_xt), row = project_and_scan(ti, b, s, 1)
                ti += 1
                if last and s < TCTX:
                    continue
                for g, (bo, r_bo) in enumerate(obanks):
                    P.op("dve", lambda e, bo=bo, g=g, of=of: e.tensor_tensor(out=osum[:, g * 512:(g + 1) * 512], in0=bo[:],
                                                                            in1=of[:, g * 512:(g + 1) * 512], op=ALU.add),
                         reads=[r_bo, r_of], writes=[r_osum])
                for h in range(4):
                    P.op("dve", lambda e, h=h: e.bn_stats(out=gst[:, h, :], in_=osum[:, h * 256:(h + 1) * 256]),
                         reads=[r_osum], writes=[r_gst])
                for h in range(4):
                    P.op("dve", lambda e, h=h: e.bn_aggr(out=gmv[:, h, :], in_=gst[:, h, :]), reads=[r_gst], writes=[r_gmv])
                P.op("act", lambda e: e.activation(out=gsd[:, :, 0], in_=gmv[:, :, 1], func=AF.Sqrt, bias=eps[0][:, 0:1], scale=1.0),
                     reads=[r_gmv, eps[1]], writes=[r_gsd])
                P.op("dve", lambda e: e.reciprocal(out=gsd[:, :, 1], in_=gsd[:, :, 0]), reads=[r_gsd], writes=[r_gsd])
                for h in range(4):
                    P.op("dve", lambda e, h=h: e.tensor_scalar(out=yf[:, h * 256:(h + 1) * 256], in0=osum[:, h * 256:(h + 1) * 256],
                                                               scalar1=gmv[:, h, 0:1], scalar2=gsd[:, h, 1:2],
                                                               op0=ALU.subtract, op1=ALU.mult),
                         reads=[r_osum, r_gmv, r_gsd], writes=[r_yf])
                P.op("dve", lambda e: e.tensor_tensor(out=yf[:], in0=yf[:], in1=gng[:], op=ALU.mult), reads=[r_yf, r_gng], writes=[r_yf])
                P.op("dve", lambda e: e.tensor_tensor(out=yf[:], in0=yf[:], in1=r_sb[:], op=ALU.mult), reads=[r_yf, r_r], writes=[r_yf])
                self.transpose_to(yT, r_yT, yf, r_yf)
                mixb = []
                for n2 in range(2):
                    bk, r_bk = P.bank()
                    for fc in range(8):
                        P.op("pe", lambda e, bk=bk, n2=n2, fc=fc: e.matmul(bk[:], lhsT=yT[:, fc, :],
                                                                           rhs=w_out[:, fc, n2 * 512:(n2 + 1) * 512],
                                                                           start=(fc == 0), stop=(fc == 7)),
                             reads=[r_yT, r_w_out], writes=[r_bk], acc=(fc > 0))
                    mixb.append((bk, r_bk))
                self.residual_ln_store(mixb, xt, r_xt, row, sm, ytile, x1tile, A["X1"][b, s * 128:(s + 1) * 128, :], self.rX1)
        P.end_phase()

    def peer_phase(self, layer, final_out):
        P = self.P
        A = self.A
        last = layer == DEPTH - 1
        P.begin_phase()
        sm = self.small_scratch()
        wq, r_wq = P.sbuf("wq", [128, 8, D], F32)
        kbd, r_kbd = P.sbuf("kbd", [128, 256], F32)
        kraw, r_kraw = P.sbuf("kraw", [128, 2, 64], F32)
        P.dma("sp", lambda e: e.dma_start(out=wq[:], in_=A["p_w_q"][layer].rearrange("(kc p) n -> p kc n", p=128)),
              reads=[self.rIN], writes=[r_wq])
        P.dma("sp", lambda e: e.dma_start(out=kraw[:], in_=A["p_keys"][layer].rearrange("p k d -> k p d")),
              reads=[self.rIN], writes=[r_kraw])
        P.op("pool", lambda e: e.memset(kbd[:], 0.0), writes=[r_kbd])
        bkk, r_bkk = P.bank()
        P.op("pe", lambda e: e.transpose(out=bkk[:, 0:128], in_=kraw[:].rearrange("p a b -> p (a b)"), identity=self.ident[:]),
             reads=[r_kraw, self.r_ident], writes=[r_bkk])
        P.op("dve", lambda e: e.tensor_copy(out=kbd[0:64, 0:128], in_=bkk[0:64, 0:128]), reads=[r_bkk], writes=[r_kbd])
        P.op("dve", lambda e: e.tensor_copy(out=kbd[64:128, 128:256], in_=bkk[64:128, 0:128]), reads=[r_bkk], writes=[r_kbd])

        xts = [P.sbuf("xt%d" % k, [128, D], F32) for k in range(2)]
        h2s = [P.sbuf("h2_%d" % k, [128, D], F32) for k in range(2)]
        h2T, r_h2T = P.sbuf("h2T", [128, 8, 128], F32)
        qT, r_qT = P.sbuf("qT", [128, 8, 128], F32)
        sc, r_sc = P.sbuf("sc", [128, 16, 128], F32)
        work, r_work = P.sbuf("work", [128, 256], F32)
        s12, r_s12 = P.sbuf("s12", [128, 16, 16], F32)
        i12, r_i12 = P.sbuf("i12", [128, 16, 16], U32)
        i12f, r_i12f = P.sbuf("i12f", [128, 16, 16], F32)
        cand, r_cand = P.sbuf("cand", [128, 8, 256], F32)
        tops, r_tops = P.sbuf("tops", [128, 8, 16], F32)
        pos, r_pos = P.sbuf("pos", [128, 8, 16], U32)
        posf, r_posf = P.sbuf("posf", [128, 128], F32)
        pjf, r_pjf = P.sbuf("pjf", [128, 128], F32)
        pkf, r_pkf = P.sbuf("pkf", [128, 128], F32)
        ge, r_ge = P.sbuf("ge", [128, 128, 17], F32)
        oh, r_oh = cand[:].rearrange("p h (a b) -> p h a b", a=16), r_cand
        sel1, r_sel1 = P.sbuf("sel1", [128, 8, 16], F32)
        sel2, r_sel2 = P.sbuf("sel2", [128, 8, 16], F32)
        eidf, r_eidf = P.sbuf("eidf", [128, 128], F32)
        eids = [P.sbuf("eid%d" % k, [128, 128], I32) for k in range(2)]
        ex, r_ex = P.sbuf("ex", [128, 8, 16], F32)
        esum, r_esum = P.sbuf("esum", [128, 8], F32)
        wtss = [P.sbuf("wts%d" % k, [128, 128], F32) for k in range(2)]
        dots, r_dots = P.sbuf("dots", [128, 128], F32)
        actw, r_actw = P.sbuf("actw", [128, 128], F32)
        junk, r_junk = P.sbuf("junk", [128, D], F32)
        acc, r_acc = P.sbuf("acc", [128, D], F32)
        ring = [P.sbuf("ring%d" % k, [128, GSL, D], F32) for k in range(NRING)]
        for k in range(NRING):
            ring[k][1].dsem_in = P.swsem(k)
        ytile = (acc, r_acc)
        x1tile = P.sbuf("x1t", [128, D], F32)
        ring_i = [0]
        P.bank_mod = 6
        accb = [P.banks[6], P.banks[7]]
        dgs = [P.sbuf("dg%d" % k, [128, 128], F32) for k in range(4)]
        ident = self.ident

        tiles = [(b, s) for b in range(self.nb) for s in range(TPB) if not (last and s < TCTX)]
        tiles = tiles[:self.dbg.get("max_tiles", 10 ** 9)]
        utab = A["p_u"]
        vtab = A["p_v"]
        def stage1(ti):
            b, s = tiles[ti]
            xt, r_xt = xts[ti % 2]
            eid, r_eid = eids[ti % 2]
            h2, r_h2 = h2s[ti % 2]
            wts, r_wts = wtss[ti % 2]
            row = self.row_of(b, s)
            P.dma("sp", lambda e, xt=xt, b=b, s=s: e.dma_start(out=xt[:], in_=A["X1"][b, s * 128:(s + 1) * 128, :]),
                  reads=[self.rX1], writes=[r_xt])
            self.modulate(h2, r_h2, xt, r_xt, row)
            self.transpose_to(h2T, r_h2T, h2, r_h2)
            for g in range(2):
                bk, r_bk = P.bank()
                for k in range(4):
                    hd = g * 4 + k
                    for kc in range(8):
                        P.op("pe", lambda e, bk=bk, k=k, hd=hd, kc=kc: e.matmul(
                            bk[:, k * 128:(k + 1) * 128], lhsT=wq[:, kc, hd * 128:(hd + 1) * 128], rhs=h2T[:, kc, :],
                            start=(kc == 0), stop=(kc == 7)),
                            reads=[r_wq, r_h2T], writes=[r_bk], acc=not (k == 0 and kc == 0))
                P.op("act", lambda e, bk=bk, g=g: e.copy(out=qT[:, g * 4:(g + 1) * 4, :].rearrange("p a b -> p (a b)"), in_=bk[:]),
                     reads=[r_bk], writes=[r_qT])
            for g in range(4):
                bk, r_bk = P.bank()
                for k in range(2):
                    hd = g * 2 + k
                    P.op("pe", lambda e, bk=bk, k=k, hd=hd: e.matmul(bk[:, k * 256:(k + 1) * 256], lhsT=qT[:, hd, :], rhs=kbd[:],
                                                                     start=True, stop=True),
                         reads=[r_qT, r_kbd], writes=[r_bk], acc=(k > 0))
                P.op("act", lambda e, bk=bk, g=g: e.copy(out=sc[:, g * 4:(g + 1) * 4, :].rearrange("p a b -> p (a b)"), in_=bk[:]),
                     reads=[r_bk], writes=[r_sc])
            for g in range(16):
                P.op("dve", lambda e, g=g: e.max(out=s12[:, g, 0:8], in_=sc[:, g, :]), reads=[r_sc], writes=[r_s12])
                P.op("dve", lambda e, g=g: e.max_index(out=i12[:, g, 0:8], in_max=s12[:, g, 0:8], in_values=sc[:, g, :]),
                     reads=[r_sc, r_s12], writes=[r_i12])
                P.op("dve", lambda e, g=g: e.match_replace(out=work[:, 0:128], in_to_replace=s12[:, g, 0:8], in_values=sc[:, g, :],
                                                           imm_value=-1e30), reads=[r_sc, r_s12], writes=[r_work])
                P.op("dve", lambda e, g=g: e.max(out=s12[:, g, 8:16], in_=work[:, 0:128]), reads=[r_work], writes=[r_s12])
                P.op("dve", lambda e, g=g: e.max_index(out=i12[:, g, 8:16], in_max=s12[:, g, 8:16], in_values=work[:, 0:128]),
                     reads=[r_work, r_s12], writes=[r_i12])
            P.op("dve", lambda e: e.tensor_copy(out=i12f[:], in_=i12[:]), reads=[r_i12], writes=[r_i12f])
            s12v = s12[:].rearrange("p (h two) n -> p h two n", two=2)
            P.op("dve", lambda e: e.tensor_tensor(out=cand[:].rearrange("p h (a b) -> p h a b", a=16),
                                                  in0=s12v[:, :, 0, :].unsqueeze(3).to_broadcast([128, 8, 16, 16]),
                                                  in1=s12v[:, :, 1, :].unsqueeze(2).to_broadcast([128, 8, 16, 16]), op=ALU.add),
                 reads=[r_s12], writes=[r_cand])
            for hd in range(8):
                P.op("dve", lambda e, hd=hd: e.max(out=tops[:, hd, 0:8], in_=cand[:, hd, :]), reads=[r_cand], writes=[r_tops])
                P.op("dve", lambda e, hd=hd: e.max_index(out=pos[:, hd, 0:8], in_max=tops[:, hd, 0:8], in_values=cand[:, hd, :]),
                     reads=[r_cand, r_tops], writes=[r_pos])
                P.op("dve", lambda e, hd=hd: e.match_replace(out=work[:], in_to_replace=tops[:, hd, 0:8], in_values=cand[:, hd, :],
                                                             imm_value=-1e30), reads=[r_cand, r_tops], writes=[r_work])
                P.op("dve", lambda e, hd=hd: e.max(out=tops[:, hd, 8:16], in_=work[:]), reads=[r_work], writes=[r_tops])
                P.op("dve", lambda e, hd=hd: e.max_index(out=pos[:, hd, 8:16], in_max=tops[:, hd, 8:16], in_values=work[:]),
                     reads=[r_work, r_tops], writes=[r_pos])
            P.op("dve", lambda e: e.tensor_tensor(out=ex[:], in0=tops[:], in1=tops[:, :, 0:1].to_broadcast([128, 8, 16]),
                                                  op=ALU.subtract), reads=[r_tops], writes=[r_ex])
            P.op("act", lambda e: e.activation(out=ex[:], in_=ex[:], func=AF.Exp), reads=[r_ex], writes=[r_ex])
            P.op("dve", lambda e: e.tensor_reduce(out=esum[:], in_=ex[:], axis=AX.X, op=ALU.add), reads=[r_ex], writes=[r_esum])
            P.op("dve", lambda e: e.reciprocal(out=esum[:], in_=esum[:]), reads=[r_esum], writes=[r_esum])
            P.op("dve", lambda e: e.tensor_tensor(out=wts[:].rearrange("p (h n) -> p h n", h=8), in0=ex[:],
                                                  in1=esum[:].unsqueeze(2).to_broadcast([128, 8, 16]), op=ALU.mult),
                 reads=[r_ex, r_esum], writes=[r_wts])
            P.op("dve", lambda e: e.tensor_copy(out=posf[:], in_=pos[:].rearrange("p h n -> p (h n)")), reads=[r_pos], writes=[r_posf])
            th = self.thr17
            iot = self.iota16
            i12v = i12f[:].rearrange("p (h two) n -> p h two n", two=2)
            P.op("dve", lambda e: e.tensor_tensor(out=ge[:], in0=posf[:].unsqueeze(2).to_broadcast([128, 128, 17]),
                                                  in1=th[:].unsqueeze(1).to_broadcast([128, 128, 17]), op=ALU.is_ge),
                 reads=[r_posf, self.r_thr17], writes=[r_ge])
            P.op("dve", lambda e: e.tensor_reduce(out=pjf[:], in_=ge[:, :, 1:17], axis=AX.X, op=ALU.add), reads=[r_ge], writes=[r_pjf])
            P.op("dve", lambda e: e.scalar_tensor_tensor(out=pkf[:], in0=pjf[:], scalar=-16.0, in1=posf[:], op0=ALU.mult, op1=ALU.add),
                 reads=[r_pjf, r_posf], writes=[r_pkf])
            ohf = oh.rearrange("p h n j -> p (h n) j")
            P.op("dve", lambda e: e.tensor_tensor(out=ohf, in0=ge[:, :, 0:16], in1=ge[:, :, 1:17], op=ALU.subtract),
                 reads=[r_ge], writes=[r_oh])
            P.op("dve", lambda e: e.tensor_tensor(out=oh, in0=oh, in1=i12v[:, :, 0, :].unsqueeze(2).to_broadcast([128, 8, 16, 16]),
                                                  op=ALU.mult), reads=[r_oh, r_i12f], writes=[r_oh])
            P.op("dve", lambda e: e.tensor_reduce(out=sel1[:].rearrange("p h n -> p (h n)"), in_=ohf, axis=AX.X, op=ALU.add),
                 reads=[r_oh], writes=[r_sel1])
            P.op("dve", lambda e: e.tensor_tensor(out=ohf, in0=pkf[:].unsqueeze(2).to_broadcast([128, 128, 16]),
                                                  in1=iot[:].unsqueeze(1).to_broadcast([128, 128, 16]), op=ALU.is_equal),
                 reads=[r_pkf, self.r_iota16], writes=[r_oh])
            P.op("dve", lambda e: e.tensor_tensor(out=oh, in0=oh, in1=i12v[:, :, 1, :].unsqueeze(2).to_broadcast([128, 8, 16, 16]),
                                                  op=ALU.mult), reads=[r_oh, r_i12f], writes=[r_oh])
            P.op("dve", lambda e: e.tensor_reduce(out=sel2[:].rearrange("p h n -> p (h n)"), in_=ohf, axis=AX.X, op=ALU.add),
                 reads=[r_oh], writes=[r_sel2])
            P.op("dve", lambda e: e.scalar_tensor_tensor(out=eidf[:], in0=sel1[:].rearrange("p h n -> p (h n)"), scalar=128.0,
                                                         in1=sel2[:].rearrange("p h n -> p (h n)"), op0=ALU.mult, op1=ALU.add),
                 reads=[r_sel1, r_sel2], writes=[r_eidf])
            if layer > 0:
                P.op("dve", lambda e: e.tensor_scalar(out=eidf[:], in0=eidf[:], scalar1=float(layer * NEXP), scalar2=None, op0=ALU.add),
                     reads=[r_eidf], writes=[r_eidf])
            P.op("dve", lambda e, eid=eid: e.tensor_copy(out=eid[:], in_=eidf[:]), reads=[r_eidf], writes=[r_eid])

        stage1(0)
        for ti, (b, s) in enumerate(tiles):
            xt, r_xt = xts[ti % 2]
            eid, r_eid = eids[ti % 2]
            h2, r_h2 = h2s[ti % 2]
            wts, r_wts = wtss[ti % 2]
            row = self.row_of(b, s)
            pend = []
            if ti + 1 < len(tiles):
                P.rec = []
                stage1(ti + 1)
                pend, P.rec = P.rec, None
            pstate = [0]

            def pump(k, pend=pend, pstate=pstate):
                while k > 0 and pstate[0] < len(pend):
                    fn, a = pend[pstate[0]]
                    fn(*a)
                    pstate[0] += 1
                    k -= 1
            ngrp = 128 // GSL
            for tab_i, tab in enumerate((utab, vtab)):
                if tab_i == 1:
                    P.op("act", lambda e: e.activation(out=actw[:], in_=dots[:], func=AF.Gelu_apprx_tanh),
                         reads=[r_dots], writes=[r_actw])
                    P.op("dve", lambda e, wts=wts: e.tensor_tensor(out=actw[:], in0=actw[:], in1=wts[:], op=ALU.mult),
                         reads=[r_actw, r_wts], writes=[r_actw])
                for gi in range(ngrp):
                    rb, r_rb = ring[ring_i[0] % NRING]
                    ring_i[0] += 1
                    for sl in range(GSL):
                        cidx = gi * GSL + sl
                        P.dma("pool", lambda e, rb=rb, sl=sl, cidx=cidx, tab=tab, eid=eid: e.indirect_dma_start(
                            out=rb[:, sl, :], out_offset=None, in_=tab,
                            in_offset=bass.IndirectOffsetOnAxis(ap=eid[:, cidx:cidx + 1], axis=0)),
                            reads=[r_eid, self.rIN], writes=[r_rb])
                    for sl in range(GSL):
                        cidx = gi * GSL + sl
                        if tab_i == 0:
                            P.op("dve", lambda e, rb=rb, sl=sl, cidx=cidx, h2=h2: e.scalar_tensor_tensor(
                                out=junk[:], in0=rb[:, sl, :], scalar=1.0, in1=h2[:], op0=ALU.mult, op1=ALU.mult,
                                accum_out=dots[:, cidx:cidx + 1]),
                                reads=[r_rb, r_h2], writes=[r_junk, r_dots])
                        else:
                            dg, r_dg = dgs[cidx % 4]
                            P.op("act", lambda e, dg=dg, cidx=cidx: e.activation(out=dg[:], in_=ident[:], func=AF.Copy,
                                                                                 scale=actw[:, cidx:cidx + 1]),
                                 reads=[r_actw, self.r_ident], writes=[r_dg])
                            for n2 in range(2):
                                bk, r_bk = accb[n2]
                                P.op("pe", lambda e, bk=bk, dg=dg, rb=rb, sl=sl, n2=n2, cidx=cidx: e.matmul(
                                    bk[:], lhsT=dg[:], rhs=rb[:, sl, n2 * 512:(n2 + 1) * 512],
                                    start=(cidx == 0), stop=(cidx == 127)),
                                    reads=[r_dg, r_rb], writes=[r_bk], acc=(cidx > 0))
                        pump(1)
            if final_out and s >= TCTX:
                dst, r_dst = A["y"][b, (s - TCTX) * 128:(s - TCTX + 1) * 128, :], self.rY
            else:
                dst, r_dst = A["X0"][b, s * 128:(s + 1) * 128, :], self.rX0
            self.residual_ln_store(accb, xt, r_xt, row, sm, ytile, x1tile, dst, r_dst)
            pump(10 ** 9)
        P.bank_mod = 8
        P.end_phase()

    def build(self):
        self.consts()
        if self.dbg.get("consts_only"):
            self.P.close()
            return self.nc
        for layer in self.dbg.get("layers", range(self.depth)):
            final = layer == self.depth - 1
            self.mod_phase(layer, 0)
            if self.dbg.get("mod_only"):
                break
            if layer % 2 == 0:
                self.cmlp_phase(layer)
            else:
                self.gla_phase(layer)
            if self.dbg.get("stop_after_mixer") == layer:
                break
            self.mod_phase(layer, 1)
            self.peer_phase(layer, final)
        self.P.close()
        return self.nc


_W_NAMES = ["w_mod", "b_mod", "ln_g", "ln_b", "a_w_in", "a_norm_g", "a_norm_b", "a_w_s", "a_b_s", "a_w_out",
            "b_w_in", "b_w_gate", "b_gate_bias", "b_gn_g", "b_w_out", "p_w_q", "p_keys", "p_u", "p_v"]


def make_in_maps(inputs, n_cores, nb):
    f = lambda a: np.ascontiguousarray(np.asarray(a, dtype=np.float32))
    shared = {k: f(inputs[k]) for k in _W_NAMES}
    shared["p_u"] = shared["p_u"].reshape(DEPTH * NEXP, D)
    shared["p_v"] = shared["p_v"].reshape(DEPTH * NEXP, D)
    shared["c_ctx"] = f(inputs["c_ctx"]).reshape(1, D)
    x, c, ctx = f(inputs["x"]), f(inputs["c"]), f(inputs["ctx"])
    maps = []
    for i in range(n_cores):
        m = dict(shared)
        m["x"] = x[i * nb:(i + 1) * nb]
        m["c"] = c[i * nb:(i + 1) * nb]
        m["ctx"] = ctx[i * nb:(i + 1) * nb]
        maps.append(m)
    return maps


def kernel(**inputs):
    nb = 2
    nc = Builder(nb=nb).build()
    in_maps = make_in_maps(inputs, NCORES, nb)
    res = run_bass_kernel_spmd(nc, in_maps, core_ids=list(range(NCORES)))
    return np.concatenate([r["y"] for r in res.results], axis=0).astype(np.float32)
```

```python
import contextlib
import numpy as np
import concourse.bass as bass
import concourse.mybir as mybir
from concourse.bass_utils import run_bass_kernel_spmd

F32 = mybir.dt.float32
F32R = mybir.dt.float32r
BF16 = mybir.dt.bfloat16
I32 = mybir.dt.int32
U32 = mybir.dt.uint32
ALU = mybir.AluOpType
AF = mybir.ActivationFunctionType
AX = mybir.AxisListType

D = 1024
SEQ = 2048
CTX = 256
DEPTH = 4
NCORES = 8
TCTX = CTX // 128
TLAT = SEQ // 128
TPB = TCTX + TLAT
ALPHA = float((2.0 * DEPTH) ** 0.25)
EPS = 1e-5
NEXP = 16384
GSL = 2
NRING = 5

ENGS = ("pe", "dve", "act", "pool", "sp")
HANDLES = {"pe": "tensor", "dve": "vector", "act": "scalar", "pool": "gpsimd", "sp": "sync"}


class Res:
    __slots__ = ("name", "w", "r", "dsem_in", "dsem_out")

    def __init__(self, name):
        self.name = name
        self.w = None
        self.r = []
        self.dsem_in = None
        self.dsem_out = None


class Prog:
    def __init__(self, nc):
        self.nc = nc
        self.gstack = contextlib.ExitStack()
        self.pstack = None
        self.ops = {e: [] for e in ENGS}
        self.sems = {}
        self.semval = {}
        self.waited = {e: {} for e in ENGS}
        for e in ENGS:
            if e != "sp":
                self._newsem("P_" + e)
        self.ndsem = 0
        self.free_dsems = []
        self.phase_dsems = []
        self.banks = []
        self.bank_i = 0
        self.bank_mod = 8
        self.nops = 0
        self.rec = None

    def _newsem(self, key):
        h = self.gstack.enter_context(self.nc.semaphore(key))
        self.sems[key] = h
        self.semval[key] = 0
        return key

    def dsem(self):
        if self.free_dsems:
            k = self.free_dsems.pop()
        else:
            self.ndsem += 1
            k = self._newsem("D%d" % self.ndsem)
        self.phase_dsems.append(k)
        return k

    def swsem(self, i):
        k = "SW%d" % i
        if k not in self.sems:
            self._newsem(k)
        return k

    def gsbuf(self, name, shape, dt):
        t = self.gstack.enter_context(self.nc.sbuf_tensor(name, list(shape), dt))
        r = Res("sb:" + name)
        self.ndsem += 1
        r.dsem_in = self._newsem("G%d" % self.ndsem)
        return t, r

    def sbuf(self, name, shape, dt):
        name = "%s_p%d" % (name, self.phase_id)
        t = self.pstack.enter_context(self.nc.sbuf_tensor(name, list(shape), dt))
        return t, Res("sb:" + name)

    def init_banks(self):
        for i in range(8):
            t = self.gstack.enter_context(self.nc.psum_tensor("bank%d" % i, [128, 512], F32))
            self.banks.append((t, Res("ps:bank%d" % i)))

    def bank(self):
        b = self.banks[self.bank_i % self.bank_mod]
        self.bank_i += 1
        return b

    def begin_phase(self):
        self.phase_id = getattr(self, "phase_id", 0) + 1
        self.pstack = contextlib.ExitStack()
        self.ops = {e: [] for e in ENGS}
        self.phase_dsems = []

    def end_phase(self, final=False):
        self.emit(final)
        self.pstack.close()
        self.pstack = None
        for e in ENGS:
            for k, v in self.semval.items():
                self.waited[e][k] = v
        self.free_dsems.extend(self.phase_dsems)
        self.phase_dsems = []

    def _waits(self, eng, reads, writes, mysem, acc):
        evs = []
        for r in reads:
            if r.w is not None:
                evs.append(r.w)
        for w in writes:
            if w.w is not None and not (acc and w.w[0] == mysem):
                evs.append(w.w)
            for ev in w.r:
                evs.append(ev)
        wd = self.waited[eng]
        best = {}
        for (k, v) in evs:
            if wd.get(k, 0) >= v:
                continue
            best[k] = max(best.get(k, 0), v)
        for k, v in best.items():
            wd[k] = v
        return list(best.items())

    def op(self, eng, fn, reads=(), writes=(), acc=False):
        if self.rec is not None:
            self.rec.append((self.op, (eng, fn, reads, writes, acc)))
            return None
        key = "P_" + eng
        waits = self._waits(eng, reads, writes, key, acc)
        self.semval[key] += 1
        ev = (key, self.semval[key])
        self.ops[eng].append((fn, waits, (key, 1)))
        self.nops += 1
        for r in reads:
            r.r.append(ev)
        for w in writes:
            w.w = ev
            w.r = []
        return ev

    def dma(self, eng, fn, reads=(), writes=(), sem=None):
        if self.rec is not None:
            self.rec.append((self.dma, (eng, fn, reads, writes, sem)))
            return None
        if sem is None:
            if writes and writes[0].name.startswith("sb:"):
                t = writes[0]
                if t.dsem_in is None:
                    t.dsem_in = self.dsem()
                sem = t.dsem_in
            else:
                t = reads[0]
                if t.dsem_out is None:
                    t.dsem_out = self.dsem()
                sem = t.dsem_out
        waits = self._waits(eng, reads, writes, sem, True)
        self.semval[sem] += 16
        ev = (sem, self.semval[sem])
        self.ops[eng].append((fn, waits, (sem, 16)))
        self.nops += 1
        for r in reads:
            r.r.append(ev)
        for w in writes:
            w.w = ev
            w.r = []
        return ev

    def emit(self, final=False):
        nc = self.nc
        sems = self.sems
        fin = [(k, v) for k, v in self.semval.items() if v > 0]
        with nc.Block() as block:
            for e in ENGS:
                ops = self.ops[e]

                def body(eh, ops=ops, e=e):
                    for fn, waits, inc in ops:
                        for k, v in waits:
                            eh.wait_ge(sems[k], v)
                        fn(eh).then_inc(sems[inc[0]], inc[1])
                    if e == "sp":
                        for k, v in fin:
                            eh.wait_ge(sems[k], v)

                getattr(block, HANDLES[e])(body)

    def close(self):
        self.gstack.close()


class Builder:
    def __init__(self, nb=2, depth=DEPTH, dbg=None):
        self.nb = nb
        self.depth = depth
        self.R = nb + 1
        self.dbg = dbg or {}
        nc = self.nc = bass.Bass("TRN2", target_bir_lowering=False)
        self.P = Prog(nc)
        dt = nc.dram_tensor
        A = {}

        def inp(name, shape, dtype=F32):
            A[name] = dt(name, list(shape), dtype, kind="ExternalInput").ap()

        inp("x", [nb, SEQ, D]); inp("c", [nb, D]); inp("ctx", [nb, CTX, D]); inp("c_ctx", [1, D])
        inp("w_mod", [DEPTH, D, 6 * D]); inp("b_mod", [DEPTH, 6 * D])
        inp("ln_g", [DEPTH, 2, D]); inp("ln_b", [DEPTH, 2, D])
        inp("a_w_in", [2, D, 2 * D]); inp("a_norm_g", [2, D]); inp("a_norm_b", [2, D])
        inp("a_w_s", [2, 8, 128, 128]); inp("a_b_s", [2, 8, 128]); inp("a_w_out", [2, D, D])
        inp("b_w_in", [2, D, 3104]); inp("b_w_gate", [2, 2, 16, 512]); inp("b_gate_bias", [2, 2, 512])
        inp("b_gn_g", [2, D]); inp("b_w_out", [2, D, D])
        inp("p_w_q", [DEPTH, D, D]); inp("p_keys", [DEPTH, 2, 128, 64])
        inp("p_u", [DEPTH * NEXP, D]); inp("p_v", [DEPTH * NEXP, D])
        A["y"] = dt("y", [nb, SEQ, D], F32, kind="ExternalOutput").ap()
        skind = "ExternalOutput" if self.dbg else "Internal"
        A["X0"] = dt("X0", [nb, CTX + SEQ, D], F32, kind=skind).ap()
        A["X1"] = dt("X1", [nb, CTX + SEQ, D], F32, kind=skind).ap()
        A["OF"] = dt("OF", [nb, CTX + SEQ, D], F32, kind="Internal").ap()
        A["TAB"] = dt("TAB", [DEPTH * NEXP, 2 * D], BF16, kind="Internal").ap()
        self.rTAB = Res("dr:TAB")
        self.A = A
        self.rX0 = Res("dr:X0"); self.rX1 = Res("dr:X1"); self.rOF = Res("dr:OF"); self.rY = Res("dr:y")
        self.rIN = Res("dr:in")

    def src_ap(self, layer, b, s):
        if layer == 0:
            if s < TCTX:
                return self.A["ctx"][b, s * 128:(s + 1) * 128, :], self.rIN
            return self.A["x"][b, (s - TCTX) * 128:(s - TCTX + 1) * 128, :], self.rIN
        return self.A["X0"][b, s * 128:(s + 1) * 128, :], self.rX0

    def row_of(self, b, s):
        return self.nb if s < TCTX else b

    def consts(self):
        P = self.P
        nb, R = self.nb, self.R
        self.ident, self.r_ident = P.gsbuf("ident", [128, 128], F32)
        self.tri = {}
        for nm in ("IF", "EF", "IB", "EB"):
            self.tri[nm] = P.gsbuf("tri" + nm, [128, 128], F32)
        self.maskF, self.r_maskF = P.gsbuf("maskF", [128, 4, 128], F32)
        self.maskB, self.r_maskB = P.gsbuf("maskB", [128, 4, 128], F32)
        self.ones1, self.r_ones1 = P.gsbuf("ones1", [1, 128], F32)
        self.iota16, self.r_iota16 = P.gsbuf("iota16", [128, 16], F32)
        self.sg, self.r_sg = P.gsbuf("sg", [128, R, 8], F32)
        self.thr17, self.r_thr17 = P.gsbuf("thr17", [128, 17], F32)
        self.modt, self.r_modt = P.gsbuf("modt", [128, R, 3, D], F32)
        self.lng, self.r_lng = P.gsbuf("lng", [128, D], F32)
        self.lnb, self.r_lnb = P.gsbuf("lnb", [128, D], F32)
        P.init_banks()

        P.begin_phase()
        cT, r_cT = P.sbuf("cT", [128, R, 8], F32)
        sg, r_sg = self.sg, self.r_sg
        tmpc, r_tmpc = P.sbuf("tmpc", [128, 4, 128], F32)
        ident = self.ident
        P.op("pool", lambda e: e.memset(ident[:], 0.0), writes=[self.r_ident])
        P.op("pool", lambda e: e.affine_select(out=ident[:], in_=ident[:], pattern=[[-1, 128]], compare_op=ALU.not_equal,
                                               fill=1.0, base=0, channel_multiplier=1),
             reads=[self.r_ident], writes=[self.r_ident])
        P.op("pool", lambda e: e.memset(tmpc[:], -1.0 / 16.0), writes=[r_tmpc])
        spec = {"IF": ([[1, 128]], -1, ALU.is_ge),
                "EF": ([[-1, 128]], 1, ALU.is_gt),
                "IB": ([[-1, 128]], 1, ALU.is_ge),
                "EB": ([[1, 128]], -1, ALU.is_gt)}
        for nm, (pat, cm, cmp) in spec.items():
            t, r = self.tri[nm]
            P.op("pool", lambda e, t=t, pat=pat, cm=cm, cmp=cmp: e.affine_select(
                out=t[:], in_=tmpc[:, 0, :], pattern=pat, compare_op=cmp, fill=0.0, base=0, channel_multiplier=cm),
                reads=[r_tmpc], writes=[r])
        ones4, r_ones4 = P.sbuf("ones4", [128, 4, 128], F32)
        P.op("pool", lambda e: e.memset(ones4[:], 1.0), writes=[r_ones4])
        mF, mB = self.maskF, self.maskB
        P.op("pool", lambda e: e.affine_select(out=mF[:], in_=ones4[:], pattern=[[0, 4], [1, 128]], compare_op=ALU.is_ge,
                                               fill=0.0, base=0, channel_multiplier=-1), reads=[r_ones4], writes=[self.r_maskF])
        P.op("pool", lambda e: e.affine_select(out=mB[:], in_=ones4[:], pattern=[[0, 4], [-1, 128]], compare_op=ALU.is_ge,
                                               fill=0.0, base=0, channel_multiplier=1), reads=[r_ones4], writes=[self.r_maskB])
        o1 = self.ones1
        P.op("pool", lambda e: e.memset(o1[:], 1.0), writes=[self.r_ones1])
        io = self.iota16
        P.op("pool", lambda e: e.iota(io[:], pattern=[[1, 16]], base=0, channel_multiplier=0,
                                      allow_small_or_imprecise_dtypes=True), writes=[self.r_iota16])
        for r in range(R):
            src = self.A["c"][r, :] if r < nb else self.A["c_ctx"][0, :]
            P.dma("sp", lambda e, r=r, src=src: e.dma_start(out=cT[:, r, :], in_=src.rearrange("(kc k) -> k kc", k=128),
                                                            allow_slow_non_contiguous=True),
                  reads=[self.rIN], writes=[r_cT])
        P.op("act", lambda e: e.activation(out=sg[:], in_=cT[:], func=AF.Sigmoid), reads=[r_cT], writes=[r_sg])
        P.op("dve", lambda e: e.tensor_tensor(out=sg[:], in0=sg[:], in1=cT[:], op=ALU.mult), reads=[r_sg, r_cT], writes=[r_sg])
        th = self.thr17
        P.op("pool", lambda e: e.iota(th[:], pattern=[[16, 17]], base=0, channel_multiplier=0,
                                      allow_small_or_imprecise_dtypes=True), writes=[self.r_thr17])
        P.end_phase()

    def table_phase(self):
        P = self.P
        A = self.A
        RR = 4
        P.begin_phase()
        uin = [P.sbuf("tu%d" % k, [128, RR, D], F32) for k in range(2)]
        vin = [P.sbuf("tv%d" % k, [128, RR, D], F32) for k in range(2)]
        tout = [P.sbuf("tt%d" % k, [128, RR, 2, D], BF16) for k in range(2)]
        layers = sorted(set(self.dbg.get("layers", range(self.depth))))
        if self.dbg.get("all_tabs"):
            layers = list(range(DEPTH))
        nchunk = NEXP // (128 * RR)
        it = 0
        for layer in layers:
            for g in range(nchunk):
                r0 = layer * NEXP + g * 128 * RR
                u, r_u = uin[it % 2]
                v, r_v = vin[it % 2]
                t, r_t = tout[it % 2]
                it += 1
                P.dma("sp", lambda e, u=u, r0=r0: e.dma_start(out=u[:], in_=A["p_u"][r0:r0 + 128 * RR, :].rearrange("(p r) d -> p r d", r=RR)),
                      reads=[self.rIN], writes=[r_u])
                P.dma("sp", lambda e, v=v, r0=r0: e.dma_start(out=v[:], in_=A["p_v"][r0:r0 + 128 * RR, :].rearrange("(p r) d -> p r d", r=RR)),
                      reads=[self.rIN], writes=[r_v])
                P.op("dve", lambda e, u=u, t=t: e.tensor_copy(out=t[:, :, 0, :], in_=u[:]), reads=[r_u], writes=[r_t])
                P.op("act", lambda e, v=v, t=t: e.copy(out=t[:, :, 1, :], in_=v[:]), reads=[r_v], writes=[r_t])
                P.dma("sp", lambda e, t=t, r0=r0: e.dma_start(
                    out=A["TAB"][r0:r0 + 128 * RR, :].rearrange("(p r) (two d) -> p r two d", r=RR, two=2), in_=t[:]),
                    reads=[r_t], writes=[Res("dr:tabchunk")])
        P.end_phase()

    def mod_phase(self, layer, half):
        P = self.P
        R = self.R
        P.begin_phase()
        bm, r_bm = P.sbuf("bm", [1, 3 * D], F32)
        scr, r_screp = P.sbuf("screp", [128, R, 8, 128], F32)
        sg = self.sg
        for r in range(R):
            P.op("dve", lambda e, r=r: e.tensor_copy(out=scr[:, r, :, :],
                                                     in_=sg[:, r, :].unsqueeze(2).to_broadcast([128, 8, 128])),
                 reads=[self.r_sg], writes=[r_screp])
        wst = [P.sbuf("wst%d" % k, [128, 8, 512], F32) for k in range(2)]
        P.dma("sp", lambda e: e.dma_start(out=bm[:], in_=self.A["b_mod"][layer:layer + 1, half * 3 * D:(half + 1) * 3 * D]),
              reads=[self.rIN], writes=[r_bm])
        lng, lnb = self.lng, self.lnb
        P.dma("sp", lambda e: e.dma_start(out=lng[:], in_=self.A["ln_g"][layer, half, :].partition_broadcast(128)),
              reads=[self.rIN], writes=[self.r_lng])
        P.dma("sp", lambda e: e.dma_start(out=lnb[:], in_=self.A["ln_b"][layer, half, :].partition_broadcast(128)),
              reads=[self.rIN], writes=[self.r_lnb])
        modt, ones1 = self.modt, self.ones1
        it = 0
        for jj in range(3):
            for nn in range(2):
                c0 = (half * 3 + jj) * D + nn * 512
                w, r_w = wst[it % 2]
                it += 1
                P.dma("sp", lambda e, w=w, c0=c0: e.dma_start(
                    out=w[:], in_=self.A["w_mod"][layer, :, c0:c0 + 512].rearrange("(kc p) n -> p kc n", p=128)),
                    reads=[self.rIN], writes=[r_w])
                for r in range(R):
                    bk, r_bk = P.bank()
                    for kc in range(8):
                        P.op("pe", lambda e, bk=bk, r=r, kc=kc, w=w: e.matmul(bk[:], lhsT=scr[:, r, kc, :], rhs=w[:, kc, :],
                                                                             start=(kc == 0), stop=False),
                             reads=[r_screp, r_w], writes=[r_bk], acc=(kc > 0))
                    P.op("pe", lambda e, bk=bk, jj=jj, nn=nn: e.matmul(bk[:], lhsT=ones1[0:1, :],
                                                                       rhs=bm[0:1, jj * D + nn * 512: jj * D + nn * 512 + 512],
                                                                       start=False, stop=True),
                         reads=[self.r_ones1, r_bm], writes=[r_bk], acc=True)
                    addc = 1.0 if jj == 1 else 0.0
                    P.op("dve", lambda e, bk=bk, r=r, jj=jj, nn=nn, addc=addc: e.tensor_scalar(
                        out=modt[:, r, jj, nn * 512:(nn + 1) * 512], in0=bk[:], scalar1=addc, scalar2=None, op0=ALU.add),
                        reads=[r_bk], writes=[self.r_modt])
        P.end_phase()

    def load_w_bf16(self, dst, r_dst, src, K, N, stg):
        P = self.P
        it = 0
        for kc in range(K // 128):
            SW = stg[0][0].shape[-1]
            for n0 in range(0, N, SW):
                n1 = min(N, n0 + SW)
                s, r_s = stg[it % len(stg)]
                eng = ("act", "dve")[it % 2]
                it += 1
                P.dma("sp", lambda e, s=s, kc=kc, n0=n0, n1=n1: e.dma_start(out=s[:, 0:n1 - n0],
                                                                            in_=src[kc * 128:(kc + 1) * 128, n0:n1]),
                      reads=[self.rIN], writes=[r_s])
                if eng == "act":
                    P.op("act", lambda e, s=s, kc=kc, n0=n0, n1=n1: e.copy(out=dst[:, kc, n0:n1], in_=s[:, 0:n1 - n0]),
                         reads=[r_s], writes=[r_dst])
                else:
                    P.op("dve", lambda e, s=s, kc=kc, n0=n0, n1=n1: e.tensor_copy(out=dst[:, kc, n0:n1], in_=s[:, 0:n1 - n0]),
                         reads=[r_s], writes=[r_dst])

    def bcast_load(self, dst, r_dst, vec):
        self.P.dma("sp", lambda e: e.dma_start(out=dst[:], in_=vec.partition_broadcast(128)), reads=[self.rIN], writes=[r_dst])

    def modulate(self, out, r_out, xt, r_xt, row):
        P = self.P
        modt = self.modt
        P.op("dve", lambda e: e.tensor_tensor(out=out[:], in0=xt[:], in1=modt[:, row, 1, :], op=ALU.mult),
             reads=[r_xt, self.r_modt], writes=[r_out])
        P.op("dve", lambda e: e.tensor_tensor(out=out[:], in0=out[:], in1=modt[:, row, 0, :], op=ALU.add),
             reads=[r_out, self.r_modt], writes=[r_out])

    def transpose_to(self, dst, r_dst, src, r_src, nchunks=8):
        P = self.P
        ident = self.ident
        for g in range(0, nchunks, 4):
            bk, r_bk = P.bank()
            n = min(4, nchunks - g)
            for k in range(n):
                kc = g + k
                P.op("pe", lambda e, bk=bk, k=k, kc=kc: e.transpose(out=bk[:, k * 128:(k + 1) * 128],
                                                                    in_=src[:, kc * 128:(kc + 1) * 128], identity=ident[:]),
                     reads=[r_src, self.r_ident], writes=[r_bk], acc=(k > 0))
            P.op("act", lambda e, bk=bk, g=g, n=n: e.copy(out=dst[:, g:g + n, :].rearrange("p a b -> p (a b)"),
                                                          in_=bk[:, 0:n * 128]),
                 reads=[r_bk], writes=[r_dst])

    def layer_norm(self, out, r_out, yin, r_yin, sm, gb=None):
        P = self.P
        st, r_st = sm["st"]
        mv, r_mv = sm["mv"]
        sd, r_sd = sm["sd"]
        for h2 in range(2):
            P.op("dve", lambda e, h2=h2: e.bn_stats(out=st[:, h2, :], in_=yin[:, h2 * 512:(h2 + 1) * 512]),
                 reads=[r_yin], writes=[r_st])
        P.op("dve", lambda e: e.bn_aggr(out=mv[:], in_=st[:].rearrange("p a b -> p (a b)")), reads=[r_st], writes=[r_mv])
        P.op("act", lambda e: e.activation(out=sd[:, 0:1], in_=mv[:, 1:2], func=AF.Sqrt, bias=sm["eps"][0][:, 0:1], scale=1.0),
             reads=[r_mv, sm["eps"][1]], writes=[r_sd])
        P.op("dve", lambda e: e.reciprocal(out=sd[:, 1:2], in_=sd[:, 0:1]), reads=[r_sd], writes=[r_sd])
        P.op("dve", lambda e: e.tensor_scalar(out=out[:], in0=yin[:], scalar1=mv[:, 0:1], scalar2=sd[:, 1:2],
                                              op0=ALU.subtract, op1=ALU.mult),
             reads=[r_yin, r_mv, r_sd], writes=[r_out])
        if gb is not None:
            (g, r_g), (b, r_b) = gb
            P.op("dve", lambda e: e.tensor_tensor(out=out[:], in0=out[:], in1=g[:], op=ALU.mult), reads=[r_out, r_g], writes=[r_out])
            P.op("dve", lambda e: e.tensor_tensor(out=out[:], in0=out[:], in1=b[:], op=ALU.add), reads=[r_out, r_b], writes=[r_out])

    def small_scratch(self):
        P = self.P
        sm = {"st": P.sbuf("ln_st", [128, 2, 6], F32), "mv": P.sbuf("ln_mv", [128, 2], F32), "sd": P.sbuf("ln_sd", [128, 2], F32),
              "eps": P.sbuf("ln_eps", [128, 1], F32)}
        ep = sm["eps"][0]
        P.op("pool", lambda e: e.memset(ep[:], EPS), writes=[sm["eps"][1]])
        return sm

    def residual_ln_store(self, mix_banks, xt, r_xt, row, sm, ytile, x1tile, dst_ap, r_dst):
        P = self.P
        modt = self.modt
        y, r_y = ytile
        x1, r_x1 = x1tile
        if isinstance(mix_banks, list):
            for n2, (bk, r_bk) in enumerate(mix_banks):
                P.op("dve", lambda e, bk=bk, n2=n2: e.tensor_tensor(out=y[:, n2 * 512:(n2 + 1) * 512], in0=bk[:],
                                                                    in1=modt[:, row, 2, n2 * 512:(n2 + 1) * 512], op=ALU.mult),
                     reads=[r_bk, self.r_modt], writes=[r_y])
        else:
            mt, r_mt = mix_banks
            P.op("dve", lambda e: e.tensor_tensor(out=y[:], in0=mt[:], in1=modt[:, row, 2, :], op=ALU.mult),
                 reads=[r_mt, self.r_modt], writes=[r_y])
        P.op("dve", lambda e: e.scalar_tensor_tensor(out=y[:], in0=xt[:], scalar=ALPHA, in1=y[:], op0=ALU.mult, op1=ALU.add),
             reads=[r_xt, r_y], writes=[r_y])
        self.layer_norm(x1, r_x1, y, r_y, sm, gb=((self.lng, self.r_lng), (self.lnb, self.r_lnb)))
        P.dma("sp", lambda e: e.dma_start(out=dst_ap, in_=x1[:]), reads=[r_x1], writes=[r_dst])

    def cmlp_phase(self, layer):
        P = self.P
        A = self.A
        j = layer // 2
        last = layer == DEPTH - 1
        P.begin_phase()
        sm = self.small_scratch()
        stg = [P.sbuf("stg%d" % k, [128, 2048], F32) for k in range(2)]
        w_in, r_w_in = P.sbuf("w_in", [128, 8, 2048], BF16)
        w_out, r_w_out = P.sbuf("w_out", [128, 8, D], BF16)
        wsT, r_wsT = P.sbuf("wsT", [128, 8, 128], BF16)
        wsraw, r_wsraw = P.sbuf("wsraw", [128, 8, 128], F32)
        bsb, r_bsb = P.sbuf("bsb", [128, 8, 128], F32)
        ng, r_ng = P.sbuf("ng", [128, D], F32)
        nbt, r_nbt = P.sbuf("nbt", [128, D], F32)
        self.load_w_bf16(w_in, r_w_in, A["a_w_in"][j], D, 2 * D, stg)
        self.load_w_bf16(w_out, r_w_out, A["a_w_out"][j], D, D, stg)
        self.bcast_load(ng, r_ng, A["a_norm_g"][j, :])
        self.bcast_load(nbt, r_nbt, A["a_norm_b"][j, :])
        P.dma("sp", lambda e: e.dma_start(out=bsb[:].rearrange("p a b -> p (a b)"),
                                          in_=A["a_b_s"][j].rearrange("h p -> (h p)").partition_broadcast(128)),
              reads=[self.rIN], writes=[r_bsb])
        P.dma("sp", lambda e: e.dma_start(out=wsraw[:], in_=A["a_w_s"][j].rearrange("h p q -> p h q")),
              reads=[self.rIN], writes=[r_wsraw])
        self.transpose_to(wsT, r_wsT, wsraw[:].rearrange("p a b -> p (a b)"), r_wsraw)

        xts = [P.sbuf("xt%d" % k, [128, D], F32) for k in range(2)]
        hf, r_hf = P.sbuf("hf", [128, D], F32)
        hT, r_hT = P.sbuf("hT", [128, 8, 128], BF16)
        uT, r_uT = P.sbuf("uT", [128, 8, 128], F32)
        vs, r_vs = P.sbuf("vs", [128, D], F32)
        vn, r_vn = P.sbuf("vn", [128, D], BF16)
        vnf, r_vnf = P.sbuf("vnf", [128, D], F32)
        usT, r_usT = P.sbuf("usT", [128, 8, 128], BF16)
        tmp, r_tmp = P.sbuf("tmpu", [128, 512], F32)
        ytile = P.sbuf("yt", [128, D], F32)
        x1tile = P.sbuf("x1t", [128, D], F32)

        tiles = [(b, s) for b in range(self.nb) for s in range(TPB) if not (last and s < TCTX)]
        tiles = tiles[:self.dbg.get("max_tiles", 10 ** 9)]
        for ti, (b, s) in enumerate(tiles):
            xt, r_xt = xts[ti % 2]
            src, r_src = self.src_ap(layer, b, s)
            row = self.row_of(b, s)
            P.dma("sp", lambda e, xt=xt, src=src: e.dma_start(out=xt[:], in_=src), reads=[r_src], writes=[r_xt])
            self.modulate(hf, r_hf, xt, r_xt, row)
            self.transpose_to(hT, r_hT, hf, r_hf)
            for g in range(2):
                bk, r_bk = P.bank()
                for k in range(4):
                    fc = g * 4 + k
                    for kc in range(8):
                        P.op("pe", lambda e, bk=bk, k=k, fc=fc, kc=kc: e.matmul(
                            bk[:, k * 128:(k + 1) * 128], lhsT=w_in[:, kc, fc * 128:(fc + 1) * 128], rhs=hT[:, kc, :],
                            start=(kc == 0), stop=(kc == 7)),
                            reads=[r_w_in, r_hT], writes=[r_bk], acc=not (k == 0 and kc == 0))
                P.op("act", lambda e, bk=bk, g=g: e.activation(out=uT[:, g * 4:(g + 1) * 4, :].rearrange("p a b -> p (a b)"),
                                                               in_=bk[:], func=AF.Gelu_apprx_tanh),
                     reads=[r_bk], writes=[r_uT])
            for n2 in range(2):
                bk, r_bk = P.bank()
                for kc in range(8):
                    P.op("pe", lambda e, bk=bk, n2=n2, kc=kc: e.matmul(
                        bk[:], lhsT=hT[:, kc, :], rhs=w_in[:, kc, D + n2 * 512: D + (n2 + 1) * 512],
                        start=(kc == 0), stop=(kc == 7)),
                        reads=[r_w_in, r_hT], writes=[r_bk], acc=(kc > 0))
                P.op("act", lambda e, bk=bk, n2=n2: e.activation(out=vs[:, n2 * 512:(n2 + 1) * 512], in_=bk[:],
                                                                 func=AF.Gelu_apprx_tanh),
                     reads=[r_bk], writes=[r_vs])
            self.layer_norm(vnf, r_vnf, vs, r_vs, sm, gb=((ng, r_ng), (nbt, r_nbt)))
            P.op("act", lambda e: e.copy(out=vn[:], in_=vnf[:]), reads=[r_vnf], writes=[r_vn])
            for g in range(2):
                bk, r_bk = P.bank()
                for k in range(4):
                    hd = g * 4 + k
                    P.op("pe", lambda e, bk=bk, k=k, hd=hd: e.matmul(bk[:, k * 128:(k + 1) * 128],
                                                                     lhsT=vn[:, hd * 128:(hd + 1) * 128], rhs=wsT[:, hd, :],
                                                                     start=True, stop=True),
                         reads=[r_vn, r_wsT], writes=[r_bk], acc=(k > 0))
                P.op("dve", lambda e, bk=bk, g=g: e.tensor_tensor(
                    out=tmp[:], in0=bk[:], in1=bsb[:, g * 4:(g + 1) * 4, :].rearrange("p a b -> p (a b)"), op=ALU.add),
                    reads=[r_bk, r_bsb], writes=[r_tmp])
                P.op("dve", lambda e, g=g: e.tensor_tensor(
                    out=usT[:, g * 4:(g + 1) * 4, :].rearrange("p a b -> p (a b)"), in0=tmp[:],
                    in1=uT[:, g * 4:(g + 1) * 4, :].rearrange("p a b -> p (a b)"), op=ALU.mult),
                    reads=[r_tmp, r_uT], writes=[r_usT])
            mixb = []
            for n2 in range(2):
                bk, r_bk = P.bank()
                for fc in range(8):
                    P.op("pe", lambda e, bk=bk, n2=n2, fc=fc: e.matmul(bk[:], lhsT=usT[:, fc, :],
                                                                       rhs=w_out[:, fc, n2 * 512:(n2 + 1) * 512],
                                                                       start=(fc == 0), stop=(fc == 7)),
                         reads=[r_usT, r_w_out], writes=[r_bk], acc=(fc > 0))
                mixb.append((bk, r_bk))
            self.residual_ln_store(mixb, xt, r_xt, row, sm, ytile, x1tile, A["X1"][b, s * 128:(s + 1) * 128, :], self.rX1)
        P.end_phase()

    def gla_phase(self, layer):
        P = self.P
        A = self.A
        j = layer // 2
        last = layer == DEPTH - 1
        P.begin_phase()
        sm = self.small_scratch()
        ofs = [P.sbuf("of%d" % k, [128, D], F32) for k in range(2)]
        stg = ofs
        w_in, r_w_in = P.sbuf("gw_in", [128, 8, 3104], BF16)
        w_out, r_w_out = P.sbuf("gw_out", [128, 8, D], BF16)
        wg, r_wg = P.sbuf("wg", [16, 2, 512], F32)
        gbias, r_gbias = P.sbuf("gbias", [1, 2, 512], F32)
        gng, r_gng = P.sbuf("gng", [128, D], F32)
        self.load_w_bf16(w_in, r_w_in, A["b_w_in"][j], D, 3104, stg)
        self.load_w_bf16(w_out, r_w_out, A["b_w_out"][j], D, D, stg)
        self.bcast_load(gng, r_gng, A["b_gn_g"][j, :])
        P.dma("sp", lambda e: e.dma_start(out=wg[:], in_=A["b_w_gate"][j].rearrange("d r n -> r d n")),
              reads=[self.rIN], writes=[r_wg])
        P.dma("sp", lambda e: e.dma_start(out=gbias[:], in_=A["b_gate_bias"][j:j + 1, :, :]), reads=[self.rIN], writes=[r_gbias])

        xts = [P.sbuf("xt%d" % k, [128, D], F32) for k in range(2)]
        hf, r_hf = P.sbuf("hf", [128, D], F32)
        hT, r_hT = P.sbuf("hT", [128, 8, 128], BF16)
        v_sb, r_v = P.sbuf("v_sb", [128, D], BF16)
        r_sb, r_r = P.sbuf("r_sb", [128, D], F32)
        glT, r_glT = P.sbuf("glT", [16, 128], F32)
        e1, r_e1 = P.sbuf("e1", [128, 512], F32)
        lsp, r_lsp = P.sbuf("lsp", [128, 512], F32)
        ebT, r_ebT = P.sbuf("ebT", [128, 4, 128], F32)
        enbT, r_enbT = P.sbuf("enbT", [128, 4, 128], F32)
        ebx, r_ebx = e1, r_e1
        qiT, r_qiT = P.sbuf("qiT", [128, 4, 128], BF16)
        kiT, r_kiT = P.sbuf("kiT", [128, 4, 128], BF16)
        kend, r_kend = P.sbuf("kend", [128, 512], BF16)
        attm, r_attm = P.sbuf("attm", [128, 4, 128], BF16)
        S, r_S = P.sbuf("S", [128, 4, 256], F32)
        Sb, r_Sb = P.sbuf("Sb", [128, 4, 256], BF16)
        osum, r_osum = P.sbuf("osum", [128, D], F32)
        gst, r_gst = P.sbuf("gst", [128, 4, 6], F32)
        gmv, r_gmv = P.sbuf("gmv", [128, 4, 2], F32)
        gsd, r_gsd = P.sbuf("gsd", [128, 4, 2], F32)
        yf, r_yf = P.sbuf("yf", [128, D], F32)
        yT, r_yT = P.sbuf("yT", [128, 8, 128], BF16)
        ytile = (osum, r_osum)
        x1tile = (yf, r_yf)
        eps = sm["eps"]

        def project_and_scan(ti, b, s, d):
            xt, r_xt = xts[ti % 2]
            src, r_src = self.src_ap(layer, b, s)
            row = self.row_of(b, s)
            P.dma("sp", lambda e: e.dma_start(out=xt[:], in_=src), reads=[r_src], writes=[r_xt])
            self.modulate(hf, r_hf, xt, r_xt, row)
            self.transpose_to(hT, r_hT, hf, r_hf)
            triI, r_triI = self.tri["IF" if d == 0 else "IB"]
            triE, r_triE = self.tri["EF" if d == 0 else "EB"]
            mask, r_mask = (self.maskF, self.r_maskF) if d == 0 else (self.maskB, self.r_maskB)
            endcol = 127 if d == 0 else 0

            def proj_fm(col0):
                bk, r_bk = P.bank()
                for h in range(4):
                    for kc in range(8):
                        P.op("pe", lambda e, h=h, kc=kc: e.matmul(
                            bk[:, h * 128:(h + 1) * 128], lhsT=w_in[:, kc, col0 + h * 128: col0 + (h + 1) * 128], rhs=hT[:, kc, :],
                            start=(kc == 0), stop=(kc == 7)),
                            reads=[r_w_in, r_hT], writes=[r_bk], acc=not (h == 0 and kc == 0))
                return bk, r_bk

            def proj_tm(col0):
                bk, r_bk = P.bank()
                for kc in range(8):
                    P.op("pe", lambda e, kc=kc: e.matmul(bk[:], lhsT=hT[:, kc, :], rhs=w_in[:, kc, col0:col0 + 512],
                                                         start=(kc == 0), stop=(kc == 7)),
                         reads=[r_w_in, r_hT], writes=[r_bk], acc=(kc > 0))
                return bk, r_bk

            bkg, r_bkg = P.bank()
            gcol = 3072 + 16 * d
            for kc in range(8):
                P.op("pe", lambda e, kc=kc: e.matmul(bkg[0:16, 0:128], lhsT=w_in[:, kc, gcol:gcol + 16], rhs=hT[:, kc, :],
                                                     start=(kc == 0), stop=(kc == 7)),
                     reads=[r_w_in, r_hT], writes=[r_bkg], acc=(kc > 0))
            P.op("act", lambda e: e.copy(out=glT[:], in_=bkg[0:16, 0:128]), reads=[r_bkg], writes=[r_glT])
            bkz, r_bkz = P.bank()
            P.op("pe", lambda e: e.matmul(bkz[:], lhsT=glT[:], rhs=wg[:, d, :], start=True, stop=False),
                 reads=[r_glT, r_wg], writes=[r_bkz])
            P.op("pe", lambda e: e.matmul(bkz[:], lhsT=self.ones1[0:1, :], rhs=gbias[0:1, d, :], start=False, stop=True),
                 reads=[self.r_ones1, r_gbias], writes=[r_bkz], acc=True)
            P.op("act", lambda e: e.activation(out=e1[:], in_=bkz[:], func=AF.Exp, scale=-1.0), reads=[r_bkz], writes=[r_e1])
            P.op("act", lambda e: e.activation(out=lsp[:], in_=e1[:], func=AF.Ln, bias=1.0, scale=1.0), reads=[r_e1], writes=[r_lsp])
            bkb, r_bkb = P.bank()
            for h in range(4):
                P.op("pe", lambda e, h=h: e.matmul(bkb[:, h * 128:(h + 1) * 128], lhsT=lsp[:, h * 128:(h + 1) * 128], rhs=triI[:],
                                                   start=True, stop=True),
                     reads=[r_lsp, r_triI], writes=[r_bkb], acc=(h > 0))
            bkx, r_bkx = P.bank()
            P.op("pe", lambda e: e.matmul(bkx[:], lhsT=triE[:], rhs=lsp[:], start=True, stop=True),
                 reads=[r_lsp, r_triE], writes=[r_bkx])
            P.op("act", lambda e: e.activation(out=ebT[:].rearrange("p a b -> p (a b)"), in_=bkb[:], func=AF.Exp),
                 reads=[r_bkb], writes=[r_ebT])
            P.op("act", lambda e: e.activation(out=enbT[:].rearrange("p a b -> p (a b)"), in_=bkb[:], func=AF.Exp, scale=-1.0),
                 reads=[r_bkb], writes=[r_enbT])
            P.op("act", lambda e: e.activation(out=ebx[:], in_=bkx[:], func=AF.Exp), reads=[r_bkx], writes=[r_ebx])
            bq, r_bq = proj_fm(0)
            P.op("dve", lambda e: e.scalar_tensor_tensor(out=qiT[:].rearrange("p a b -> p (a b)"), in0=bq[:], scalar=128.0 ** -0.5,
                                                         in1=ebT[:].rearrange("p a b -> p (a b)"), op0=ALU.mult, op1=ALU.mult),
                 reads=[r_bq, r_ebT], writes=[r_qiT])
            bkT, r_bkT = proj_fm(512)
            P.op("dve", lambda e: e.tensor_tensor(out=kiT[:].rearrange("p a b -> p (a b)"), in0=bkT[:],
                                                  in1=enbT[:].rearrange("p a b -> p (a b)"), op=ALU.mult),
                 reads=[r_bkT, r_enbT], writes=[r_kiT])
            bk_, r_bk_ = proj_tm(512)
            P.op("dve", lambda e: e.tensor_tensor(out=kend[:], in0=bk_[:], in1=ebx[:], op=ALU.mult),
                 reads=[r_bk_, r_ebx], writes=[r_kend])
            for n2 in range(2):
                bv, r_bv = proj_tm(1024 + n2 * 512)
                P.op("act", lambda e, n2=n2, bv=bv: e.copy(out=v_sb[:, n2 * 512:(n2 + 1) * 512], in_=bv[:]),
                     reads=[r_bv], writes=[r_v])
            if d == 1:
                for n2 in range(2):
                    br, r_br = proj_tm(2048 + n2 * 512)
                    P.op("act", lambda e, n2=n2, br=br: e.activation(out=r_sb[:, n2 * 512:(n2 + 1) * 512], in_=br[:], func=AF.Silu),
                         reads=[r_br], writes=[r_r])
            bka, r_bka = P.bank()
            for h in range(4):
                P.op("pe", lambda e, h=h: e.matmul(bka[:, h * 128:(h + 1) * 128], lhsT=kiT[:, h, :], rhs=qiT[:, h, :],
                                                   start=True, stop=True),
                     reads=[r_kiT, r_qiT], writes=[r_bka], acc=(h > 0))
            P.op("dve", lambda e: e.tensor_tensor(out=attm[:].rearrange("p a b -> p (a b)"), in0=bka[:],
                                                  in1=mask[:].rearrange("p a b -> p (a b)"), op=ALU.mult),
                 reads=[r_bka, r_mask], writes=[r_attm])
            obanks = []
            for g in range(2):
                bo, r_bo = P.bank()
                for k in range(2):
                    h = g * 2 + k
                    P.op("pe", lambda e, bo=bo, k=k, h=h: e.matmul(bo[:, k * 256:(k + 1) * 256], lhsT=attm[:, h, :],
                                                                   rhs=v_sb[:, h * 256:(h + 1) * 256], start=True, stop=False),
                         reads=[r_attm, r_v], writes=[r_bo], acc=(k > 0))
                    P.op("pe", lambda e, bo=bo, k=k, h=h: e.matmul(bo[:, k * 256:(k + 1) * 256], lhsT=qiT[:, h, :],
                                                                   rhs=Sb[:, h, :], start=False, stop=True),
                         reads=[r_qiT, r_Sb], writes=[r_bo], acc=True)
                obanks.append((bo, r_bo))
            for g in range(2):
                bs, r_bs = P.bank()
                for k in range(2):
                    h = g * 2 + k
                    P.op("pe", lambda e, bs=bs, k=k, h=h: e.matmul(bs[:, k * 256:(k + 1) * 256], lhsT=kend[:, h * 128:(h + 1) * 128],
                                                                   rhs=v_sb[:, h * 256:(h + 1) * 256], start=True, stop=True),
                         reads=[r_kend, r_v], writes=[r_bs], acc=(k > 0))
                for k in range(2):
                    h = g * 2 + k
                    P.op("dve", lambda e, bs=bs, k=k, h=h: e.scalar_tensor_tensor(
                        out=S[:, h, :], in0=S[:, h, :], scalar=ebT[:, h, endcol:endcol + 1], in1=bs[:, k * 256:(k + 1) * 256],
                        op0=ALU.mult, op1=ALU.add),
                        reads=[r_S, r_ebT, r_bs], writes=[r_S])
            P.op("act", lambda e: e.copy(out=Sb[:].rearrange("p a b -> p (a b)"), in_=S[:].rearrange("p a b -> p (a b)")),
                 reads=[r_S], writes=[r_Sb])
            return obanks, (xt, r_xt), row

        def reset_state():
            P.op("pool", lambda e: e.memset(S[:], 0.0), writes=[r_S])
            P.op("pool", lambda e: e.memset(Sb[:], 0.0), writes=[r_Sb])

        ti = 0
        for b in range(self.nb):
            reset_state()
            for s in range(TPB):
                obanks, _, _ = project_and_scan(ti, b, s, 0)
                of, r_of = ofs[ti % 2]
                for g, (bo, r_bo) in enumerate(obanks):
                    P.op("act", lambda e, bo=bo, g=g, of=of: e.copy(out=of[:, g * 512:(g + 1) * 512], in_=bo[:]),
                         reads=[r_bo], writes=[r_of])
                P.dma("sp", lambda e, of=of, b=b, s=s: e.dma_start(out=A["OF"][b, s * 128:(s + 1) * 128, :], in_=of[:]),
                      reads=[r_of], writes=[self.rOF])
                ti += 1
        for b in range(self.nb):
            reset_state()
            order = [1, 0] + list(range(TPB - 1, TCTX - 1, -1))
            for s in order:
                of, r_of = ofs[ti % 2]
                if not (last and s < TCTX):
                    P.dma("sp", lambda e, of=of, b=b, s=s: e.dma_start(out=of[:], in_=A["OF"][b, s * 128:(s + 1) * 128, :]),
                          reads=[self.rOF], writes=[r_of])
                obanks, (xt, r_xt), row = project_and_scan(ti, b, s, 1)
                ti += 1
                if last and s < TCTX:
                    continue
                for g, (bo, r_bo) in enumerate(obanks):
                    P.op("dve", lambda e, bo=bo, g=g, of=of: e.tensor_tensor(out=osum[:, g * 512:(g + 1) * 512], in0=bo[:],
                                                                            in1=of[:, g * 512:(g + 1) * 512], op=ALU.add),
                         reads=[r_bo, r_of], writes=[r_osum])
                for h in range(4):
                    P.op("dve", lambda e, h=h: e.bn_stats(out=gst[:, h, :], in_=osum[:, h * 256:(h + 1) * 256]),
                         reads=[r_osum], writes=[r_gst])
                for h in range(4):
                    P.op("dve", lambda e, h=h: e.bn_aggr(out=gmv[:, h, :], in_=gst[:, h, :]), reads=[r_gst], writes=[r_gmv])
                P.op("act", lambda e: e.activation(out=gsd[:, :, 0], in_=gmv[:, :, 1], func=AF.Sqrt, bias=eps[0][:, 0:1], scale=1.0),
                     reads=[r_gmv, eps[1]], writes=[r_gsd])
                P.op("dve", lambda e: e.reciprocal(out=gsd[:, :, 1], in_=gsd[:, :, 0]), reads=[r_gsd], writes=[r_gsd])
                for h in range(4):
                    P.op("dve", lambda e, h=h: e.tensor_scalar(out=yf[:, h * 256:(h + 1) * 256], in0=osum[:, h * 256:(h + 1) * 256],
                                                               scalar1=gmv[:, h, 0:1], scalar2=gsd[:, h, 1:2],
                                                               op0=ALU.subtract, op1=ALU.mult),
                         reads=[r_osum, r_gmv, r_gsd], writes=[r_yf])
                P.op("dve", lambda e: e.tensor_tensor(out=yf[:], in0=yf[:], in1=gng[:], op=ALU.mult), reads=[r_yf, r_gng], writes=[r_yf])
                P.op("dve", lambda e: e.tensor_tensor(out=yf[:], in0=yf[:], in1=r_sb[:], op=ALU.mult), reads=[r_yf, r_r], writes=[r_yf])
                self.transpose_to(yT, r_yT, yf, r_yf)
                mixb = []
                for n2 in range(2):
                    bk, r_bk = P.bank()
                    for fc in range(8):
                        P.op("pe", lambda e, bk=bk, n2=n2, fc=fc: e.matmul(bk[:], lhsT=yT[:, fc, :],
                                                                           rhs=w_out[:, fc, n2 * 512:(n2 + 1) * 512],
                                                                           start=(fc == 0), stop=(fc == 7)),
                             reads=[r_yT, r_w_out], writes=[r_bk], acc=(fc > 0))
                    mixb.append((bk, r_bk))
                self.residual_ln_store(mixb, xt, r_xt, row, sm, ytile, x1tile, A["X1"][b, s * 128:(s + 1) * 128, :], self.rX1)
        P.end_phase()

    def peer_phase(self, layer, final_out):
        P = self.P
        A = self.A
        last = layer == DEPTH - 1
        P.begin_phase()
        sm = self.small_scratch()
        wq, r_wq = P.sbuf("wq", [128, 8, D], F32)
        kbd, r_kbd = P.sbuf("kbd", [128, 256], F32)
        kraw, r_kraw = P.sbuf("kraw", [128, 2, 64], F32)
        P.dma("sp", lambda e: e.dma_start(out=wq[:], in_=A["p_w_q"][layer].rearrange("(kc p) n -> p kc n", p=128)),
              reads=[self.rIN], writes=[r_wq])
        P.dma("sp", lambda e: e.dma_start(out=kraw[:], in_=A["p_keys"][layer].rearrange("p k d -> k p d")),
              reads=[self.rIN], writes=[r_kraw])
        P.op("pool", lambda e: e.memset(kbd[:], 0.0), writes=[r_kbd])
        bkk, r_bkk = P.bank()
        P.op("pe", lambda e: e.transpose(out=bkk[:, 0:128], in_=kraw[:].rearrange("p a b -> p (a b)"), identity=self.ident[:]),
             reads=[r_kraw, self.r_ident], writes=[r_bkk])
        P.op("dve", lambda e: e.tensor_copy(out=kbd[0:64, 0:128], in_=bkk[0:64, 0:128]), reads=[r_bkk], writes=[r_kbd])
        P.op("dve", lambda e: e.tensor_copy(out=kbd[64:128, 128:256], in_=bkk[64:128, 0:128]), reads=[r_bkk], writes=[r_kbd])

        xts = [P.sbuf("xt%d" % k, [128, D], F32) for k in range(2)]
        h2s = [P.sbuf("h2_%d" % k, [128, D], F32) for k in range(2)]
        h2T, r_h2T = P.sbuf("h2T", [128, 8, 128], F32)
        qT, r_qT = P.sbuf("qT", [128, 8, 128], F32)
        sc, r_sc = P.sbuf("sc", [128, 16, 128], F32)
        work, r_work = P.sbuf("work", [128, 256], F32)
        s12, r_s12 = P.sbuf("s12", [128, 16, 16], F32)
        i12, r_i12 = P.sbuf("i12", [128, 16, 16], U32)
        i12f, r_i12f = P.sbuf("i12f", [128, 16, 16], F32)
        cand, r_cand = P.sbuf("cand", [128, 8, 256], F32)
        tops, r_tops = P.sbuf("tops", [128, 8, 16], F32)
        pos, r_pos = P.sbuf("pos", [128, 8, 16], U32)
        posf, r_posf = P.sbuf("posf", [128, 128], F32)
        pjf, r_pjf = P.sbuf("pjf", [128, 128], F32)
        pkf, r_pkf = P.sbuf("pkf", [128, 128], F32)
        ge, r_ge = P.sbuf("ge", [128, 128, 17], F32)
        oh, r_oh = cand[:].rearrange("p h (a b) -> p h a b", a=16), r_cand
        sel1, r_sel1 = P.sbuf("sel1", [128, 8, 16], F32)
        sel2, r_sel2 = P.sbuf("sel2", [128, 8, 16], F32)
        eidf, r_eidf = P.sbuf("eidf", [128, 128], F32)
        eids = [P.sbuf("eid%d" % k, [128, 128], I32) for k in range(2)]
        ex, r_ex = P.sbuf("ex", [128, 8, 16], F32)
        esum, r_esum = P.sbuf("esum", [128, 8], F32)
        wtss = [P.sbuf("wts%d" % k, [128, 128], F32) for k in range(2)]
        dots, r_dots = P.sbuf("dots", [128, 128], F32)
        actw, r_actw = P.sbuf("actw", [128, 128], F32)
        junk, r_junk = P.sbuf("junk", [128, D], F32)
        acc, r_acc = P.sbuf("acc", [128, D], F32)
        ring = [P.sbuf("ring%d" % k, [128, GSL, 2 * D], BF16) for k in range(NRING)]
        for k in range(NRING):
            ring[k][1].dsem_in = P.swsem(k)
        ytile = (acc, r_acc)
        x1tile = P.sbuf("x1t", [128, D], F32)
        ring_i = [0]
        r_dots_g = [Res("sb:dotsg%d" % k) for k in range(4)]
        r_actw_g = [Res("sb:actwg%d" % k) for k in range(4)]
        P.bank_mod = 6
        accb = [P.banks[6], P.banks[7]]
        dgs = [P.sbuf("dg%d" % k, [128, 128], BF16) for k in range(4)]
        ident = self.ident

        tiles = [(b, s) for b in range(self.nb) for s in range(TPB) if not (last and s < TCTX)]
        tiles = tiles[:self.dbg.get("max_tiles", 10 ** 9)]
        tab = A["TAB"]
        def stage1(ti):
            b, s = tiles[ti]
            xt, r_xt = xts[ti % 2]
            eid, r_eid = eids[ti % 2]
            h2, r_h2 = h2s[ti % 2]
            wts, r_wts = wtss[ti % 2]
            row = self.row_of(b, s)
            P.dma("sp", lambda e, xt=xt, b=b, s=s: e.dma_start(out=xt[:], in_=A["X1"][b, s * 128:(s + 1) * 128, :]),
                  reads=[self.rX1], writes=[r_xt])
            self.modulate(h2, r_h2, xt, r_xt, row)
            self.transpose_to(h2T, r_h2T, h2, r_h2)
            for g in range(2):
                bk, r_bk = P.bank()
                for k in range(4):
                    hd = g * 4 + k
                    for kc in range(8):
                        P.op("pe", lambda e, bk=bk, k=k, hd=hd, kc=kc: e.matmul(
                            bk[:, k * 128:(k + 1) * 128], lhsT=wq[:, kc, hd * 128:(hd + 1) * 128], rhs=h2T[:, kc, :],
                            start=(kc == 0), stop=(kc == 7)),
                            reads=[r_wq, r_h2T], writes=[r_bk], acc=not (k == 0 and kc == 0))
                P.op("act", lambda e, bk=bk, g=g: e.copy(out=qT[:, g * 4:(g + 1) * 4, :].rearrange("p a b -> p (a b)"), in_=bk[:]),
                     reads=[r_bk], writes=[r_qT])
            for g in range(4):
                bk, r_bk = P.bank()
                for k in range(2):
                    hd = g * 2 + k
                    P.op("pe", lambda e, bk=bk, k=k, hd=hd: e.matmul(bk[:, k * 256:(k + 1) * 256], lhsT=qT[:, hd, :], rhs=kbd[:],
                                                                     start=True, stop=True),
                         reads=[r_qT, r_kbd], writes=[r_bk], acc=(k > 0))
                P.op("act", lambda e, bk=bk, g=g: e.copy(out=sc[:, g * 4:(g + 1) * 4, :].rearrange("p a b -> p (a b)"), in_=bk[:]),
                     reads=[r_bk], writes=[r_sc])
            for g in range(16):
                P.op("dve", lambda e, g=g: e.max(out=s12[:, g, 0:8], in_=sc[:, g, :]), reads=[r_sc], writes=[r_s12])
                P.op("dve", lambda e, g=g: e.max_index(out=i12[:, g, 0:8], in_max=s12[:, g, 0:8], in_values=sc[:, g, :]),
                     reads=[r_sc, r_s12], writes=[r_i12])
                P.op("dve", lambda e, g=g: e.match_replace(out=work[:, 0:128], in_to_replace=s12[:, g, 0:8], in_values=sc[:, g, :],
                                                           imm_value=-1e30), reads=[r_sc, r_s12], writes=[r_work])
                P.op("dve", lambda e, g=g: e.max(out=s12[:, g, 8:16], in_=work[:, 0:128]), reads=[r_work], writes=[r_s12])
                P.op("dve", lambda e, g=g: e.max_index(out=i12[:, g, 8:16], in_max=s12[:, g, 8:16], in_values=work[:, 0:128]),
                     reads=[r_work, r_s12], writes=[r_i12])
            P.op("dve", lambda e: e.tensor_copy(out=i12f[:], in_=i12[:]), reads=[r_i12], writes=[r_i12f])
            s12v = s12[:].rearrange("p (h two) n -> p h two n", two=2)
            P.op("dve", lambda e: e.tensor_tensor(out=cand[:].rearrange("p h (a b) -> p h a b", a=16),
                                                  in0=s12v[:, :, 0, :].unsqueeze(3).to_broadcast([128, 8, 16, 16]),
                                                  in1=s12v[:, :, 1, :].unsqueeze(2).to_broadcast([128, 8, 16, 16]), op=ALU.add),
                 reads=[r_s12], writes=[r_cand])
            for hd in range(8):
                P.op("dve", lambda e, hd=hd: e.max(out=tops[:, hd, 0:8], in_=cand[:, hd, :]), reads=[r_cand], writes=[r_tops])
                P.op("dve", lambda e, hd=hd: e.max_index(out=pos[:, hd, 0:8], in_max=tops[:, hd, 0:8], in_values=cand[:, hd, :]),
                     reads=[r_cand, r_tops], writes=[r_pos])
                P.op("dve", lambda e, hd=hd: e.match_replace(out=work[:], in_to_replace=tops[:, hd, 0:8], in_values=cand[:, hd, :],
                                                             imm_value=-1e30), reads=[r_cand, r_tops], writes=[r_work])
                P.op("dve", lambda e, hd=hd: e.max(out=tops[:, hd, 8:16], in_=work[:]), reads=[r_work], writes=[r_tops])
                P.op("dve", lambda e, hd=hd: e.max_index(out=pos[:, hd, 8:16], in_max=tops[:, hd, 8:16], in_values=work[:]),
                     reads=[r_work, r_tops], writes=[r_pos])
            P.op("dve", lambda e: e.tensor_tensor(out=ex[:], in0=tops[:], in1=tops[:, :, 0:1].to_broadcast([128, 8, 16]),
                                                  op=ALU.subtract), reads=[r_tops], writes=[r_ex])
            P.op("act", lambda e: e.activation(out=ex[:], in_=ex[:], func=AF.Exp), reads=[r_ex], writes=[r_ex])
            P.op("dve", lambda e: e.tensor_reduce(out=esum[:], in_=ex[:], axis=AX.X, op=ALU.add), reads=[r_ex], writes=[r_esum])
            P.op("dve", lambda e: e.reciprocal(out=esum[:], in_=esum[:]), reads=[r_esum], writes=[r_esum])
            P.op("dve", lambda e: e.tensor_tensor(out=wts[:].rearrange("p (h n) -> p h n", h=8), in0=ex[:],
                                                  in1=esum[:].unsqueeze(2).to_broadcast([128, 8, 16]), op=ALU.mult),
                 reads=[r_ex, r_esum], writes=[r_wts])
            P.op("dve", lambda e: e.tensor_copy(out=posf[:], in_=pos[:].rearrange("p h n -> p (h n)")), reads=[r_pos], writes=[r_posf])
            th = self.thr17
            iot = self.iota16
            i12v = i12f[:].rearrange("p (h two) n -> p h two n", two=2)
            P.op("dve", lambda e: e.tensor_tensor(out=ge[:], in0=posf[:].unsqueeze(2).to_broadcast([128, 128, 17]),
                                                  in1=th[:].unsqueeze(1).to_broadcast([128, 128, 17]), op=ALU.is_ge),
                 reads=[r_posf, self.r_thr17], writes=[r_ge])
            P.op("dve", lambda e: e.tensor_reduce(out=pjf[:], in_=ge[:, :, 1:17], axis=AX.X, op=ALU.add), reads=[r_ge], writes=[r_pjf])
            P.op("dve", lambda e: e.scalar_tensor_tensor(out=pkf[:], in0=pjf[:], scalar=-16.0, in1=posf[:], op0=ALU.mult, op1=ALU.add),
                 reads=[r_pjf, r_posf], writes=[r_pkf])
            ohf = oh.rearrange("p h n j -> p (h n) j")
            P.op("dve", lambda e: e.tensor_tensor(out=ohf, in0=ge[:, :, 0:16], in1=ge[:, :, 1:17], op=ALU.subtract),
                 reads=[r_ge], writes=[r_oh])
            P.op("dve", lambda e: e.tensor_tensor(out=oh, in0=oh, in1=i12v[:, :, 0, :].unsqueeze(2).to_broadcast([128, 8, 16, 16]),
                                                  op=ALU.mult), reads=[r_oh, r_i12f], writes=[r_oh])
            P.op("dve", lambda e: e.tensor_reduce(out=sel1[:].rearrange("p h n -> p (h n)"), in_=ohf, axis=AX.X, op=ALU.add),
                 reads=[r_oh], writes=[r_sel1])
            P.op("dve", lambda e: e.tensor_tensor(out=ohf, in0=pkf[:].unsqueeze(2).to_broadcast([128, 128, 16]),
                                                  in1=iot[:].unsqueeze(1).to_broadcast([128, 128, 16]), op=ALU.is_equal),
                 reads=[r_pkf, self.r_iota16], writes=[r_oh])
            P.op("dve", lambda e: e.tensor_tensor(out=oh, in0=oh, in1=i12v[:, :, 1, :].unsqueeze(2).to_broadcast([128, 8, 16, 16]),
                                                  op=ALU.mult), reads=[r_oh, r_i12f], writes=[r_oh])
            P.op("dve", lambda e: e.tensor_reduce(out=sel2[:].rearrange("p h n -> p (h n)"), in_=ohf, axis=AX.X, op=ALU.add),
                 reads=[r_oh], writes=[r_sel2])
            P.op("dve", lambda e: e.scalar_tensor_tensor(out=eidf[:], in0=sel1[:].rearrange("p h n -> p (h n)"), scalar=128.0,
                                                         in1=sel2[:].rearrange("p h n -> p (h n)"), op0=ALU.mult, op1=ALU.add),
                 reads=[r_sel1, r_sel2], writes=[r_eidf])
            if layer > 0:
                P.op("dve", lambda e: e.tensor_scalar(out=eidf[:], in0=eidf[:], scalar1=float(layer * NEXP), scalar2=None, op0=ALU.add),
                     reads=[r_eidf], writes=[r_eidf])
            P.op("dve", lambda e, eid=eid: e.tensor_copy(out=eid[:], in_=eidf[:]), reads=[r_eidf], writes=[r_eid])

        stage1(0)
        for ti, (b, s) in enumerate(tiles):
            xt, r_xt = xts[ti % 2]
            eid, r_eid = eids[ti % 2]
            h2, r_h2 = h2s[ti % 2]
            wts, r_wts = wtss[ti % 2]
            row = self.row_of(b, s)
            pend = []
            if ti + 1 < len(tiles):
                P.rec = []
                stage1(ti + 1)
                pend, P.rec = P.rec, None
            pstate = [0]

            def pump(k, pend=pend, pstate=pstate):
                while k > 0 and pstate[0] < len(pend):
                    fn, a = pend[pstate[0]]
                    fn(*a)
                    pstate[0] += 1
                    k -= 1
            for gi in range(128 // GSL):
                rb, r_rb = ring[ring_i[0] % NRING]
                ring_i[0] += 1
                c0 = gi * GSL
                r_dots = r_dots_g[gi % 4]
                r_actw = r_actw_g[gi % 4]
                for sl in range(GSL):
                    cidx = c0 + sl
                    P.dma("pool", lambda e, rb=rb, sl=sl, cidx=cidx, eid=eid: e.indirect_dma_start(
                        out=rb[:, sl, :], out_offset=None, in_=tab,
                        in_offset=bass.IndirectOffsetOnAxis(ap=eid[:, cidx:cidx + 1], axis=0)),
                        reads=[r_eid, self.rTAB], writes=[r_rb])
                for sl in range(GSL):
                    cidx = c0 + sl
                    P.op("dve", lambda e, rb=rb, sl=sl, cidx=cidx, h2=h2: e.scalar_tensor_tensor(
                        out=junk[:], in0=rb[:, sl, 0:D], scalar=1.0, in1=h2[:], op0=ALU.mult, op1=ALU.mult,
                        accum_out=dots[:, cidx:cidx + 1]),
                        reads=[r_rb, r_h2], writes=[r_junk, r_dots])
                    pump(1)
                P.op("act", lambda e, c0=c0: e.activation(out=actw[:, c0:c0 + GSL], in_=dots[:, c0:c0 + GSL], func=AF.Gelu_apprx_tanh),
                     reads=[r_dots], writes=[r_actw])
                P.op("dve", lambda e, c0=c0, wts=wts: e.tensor_tensor(out=actw[:, c0:c0 + GSL], in0=actw[:, c0:c0 + GSL],
                                                                      in1=wts[:, c0:c0 + GSL], op=ALU.mult),
                     reads=[r_actw, r_wts], writes=[r_actw])
                for sl in range(GSL):
                    cidx = c0 + sl
                    dg, r_dg = dgs[cidx % 4]
                    P.op("act", lambda e, dg=dg, cidx=cidx: e.activation(out=dg[:], in_=ident[:], func=AF.Copy,
                                                                         scale=actw[:, cidx:cidx + 1]),
                         reads=[r_actw, self.r_ident], writes=[r_dg])
                    for n2 in range(2):
                        bk, r_bk = accb[n2]
                        P.op("pe", lambda e, bk=bk, dg=dg, rb=rb, sl=sl, n2=n2, cidx=cidx: e.matmul(
                            bk[:], lhsT=dg[:], rhs=rb[:, sl, D + n2 * 512: D + (n2 + 1) * 512],
                            start=(cidx == 0), stop=(cidx == 127)),
                            reads=[r_dg, r_rb], writes=[r_bk], acc=(cidx > 0))
                    pump(1)
            if final_out and s >= TCTX:
                dst, r_dst = A["y"][b, (s - TCTX) * 128:(s - TCTX + 1) * 128, :], self.rY
            else:
                dst, r_dst = A["X0"][b, s * 128:(s + 1) * 128, :], self.rX0
            self.residual_ln_store(accb, xt, r_xt, row, sm, ytile, x1tile, dst, r_dst)
            pump(10 ** 9)
        P.bank_mod = 8
        P.end_phase()

    def build(self):
        self.consts()
        if self.dbg.get("consts_only"):
            self.P.close()
            return self.nc
        if not (self.dbg.get("mod_only") or "stop_after_mixer" in self.dbg):
            self.table_phase()
        for layer in self.dbg.get("layers", range(self.depth)):
            final = layer == self.depth - 1
            self.mod_phase(layer, 0)
            if self.dbg.get("mod_only"):
                break
            if layer % 2 == 0:
                self.cmlp_phase(layer)
            else:
                self.gla_phase(layer)
            if self.dbg.get("stop_after_mixer") == layer:
                break
            self.mod_phase(layer, 1)
            self.peer_phase(layer, final)
        self.P.close()
        return self.nc


_W_NAMES = ["w_mod", "b_mod", "ln_g", "ln_b", "a_w_in", "a_norm_g", "a_norm_b", "a_w_s", "a_b_s", "a_w_out",
            "b_w_in", "b_w_gate", "b_gate_bias", "b_gn_g", "b_w_out", "p_w_q", "p_keys", "p_u", "p_v"]


def make_in_maps(inputs, n_cores, nb):
    f = lambda a: np.ascontiguousarray(np.asarray(a, dtype=np.float32))
    shared = {k: f(inputs[k]) for k in _W_NAMES}
    shared["p_u"] = shared["p_u"].reshape(DEPTH * NEXP, D)
    shared["p_v"] = shared["p_v"].reshape(DEPTH * NEXP, D)
    shared["c_ctx"] = f(inputs["c_ctx"]).reshape(1, D)
    x, c, ctx = f(inputs["x"]), f(inputs["c"]), f(inputs["ctx"])
    maps = []
    for i in range(n_cores):
        m = dict(shared)
        m["x"] = x[i * nb:(i + 1) * nb]
        m["c"] = c[i * nb:(i + 1) * nb]
        m["ctx"] = ctx[i * nb:(i + 1) * nb]
        maps.append(m)
    return maps


def kernel(**inputs):
    nb = 2
    nc = Builder(nb=nb).build()
    in_maps = make_in_maps(inputs, NCORES, nb)
    res = run_bass_kernel_spmd(nc, in_maps, core_ids=list(range(NCORES)))
    return np.concatenate([r["y"] for r in res.results], axis=0).astype(np.float32)
```

```python
import contextlib
import numpy as np
import concourse.bass as bass
import concourse.mybir as mybir
from concourse.bass_utils import run_bass_kernel_spmd

F32 = mybir.dt.float32
F32R = mybir.dt.float32r
BF16 = mybir.dt.bfloat16
I32 = mybir.dt.int32
U32 = mybir.dt.uint32
ALU = mybir.AluOpType
AF = mybir.ActivationFunctionType
AX = mybir.AxisListType

D = 1024
SEQ = 2048
CTX = 256
DEPTH = 4
NCORES = 8
TCTX = CTX // 128
TLAT = SEQ // 128
TPB = TCTX + TLAT
ALPHA = float((2.0 * DEPTH) ** 0.25)
EPS = 1e-5
NEXP = 16384
GSL = 2
NRING = 5

ENGS = ("pe", "dve", "act", "pool", "sp")
HANDLES = {"pe": "tensor", "dve": "vector", "act": "scalar", "pool": "gpsimd", "sp": "sync"}


class Res:
    __slots__ = ("name", "w", "r", "dsem_in", "dsem_out")

    def __init__(self, name):
        self.name = name
        self.w = None
        self.r = []
        self.dsem_in = None
        self.dsem_out = None


class Prog:
    def __init__(self, nc):
        self.nc = nc
        self.gstack = contextlib.ExitStack()
        self.pstack = None
        self.ops = {e: [] for e in ENGS}
        self.sems = {}
        self.semval = {}
        self.waited = {e: {} for e in ENGS}
        for e in ENGS:
            if e != "sp":
                self._newsem("P_" + e)
        self.ndsem = 0
        self.free_dsems = []
        self.phase_dsems = []
        self.banks = []
        self.bank_i = 0
        self.bank_mod = 8
        self.nops = 0
        self.rec = None

    def _newsem(self, key):
        h = self.gstack.enter_context(self.nc.semaphore(key))
        self.sems[key] = h
        self.semval[key] = 0
        return key

    def dsem(self):
        if self.free_dsems:
            k = self.free_dsems.pop()
        else:
            self.ndsem += 1
            k = self._newsem("D%d" % self.ndsem)
        self.phase_dsems.append(k)
        return k

    def swsem(self, i):
        k = "SW%d" % i
        if k not in self.sems:
            self._newsem(k)
        return k

    def gsbuf(self, name, shape, dt):
        t = self.gstack.enter_context(self.nc.sbuf_tensor(name, list(shape), dt))
        r = Res("sb:" + name)
        self.ndsem += 1
        r.dsem_in = self._newsem("G%d" % self.ndsem)
        return t, r

    def sbuf(self, name, shape, dt):
        name = "%s_p%d" % (name, self.phase_id)
        t = self.pstack.enter_context(self.nc.sbuf_tensor(name, list(shape), dt))
        return t, Res("sb:" + name)

    def init_banks(self):
        for i in range(8):
            t = self.gstack.enter_context(self.nc.psum_tensor("bank%d" % i, [128, 512], F32))
            self.banks.append((t, Res("ps:bank%d" % i)))

    def bank(self):
        b = self.banks[self.bank_i % self.bank_mod]
        self.bank_i += 1
        return b

    def begin_phase(self):
        self.phase_id = getattr(self, "phase_id", 0) + 1
        self.pstack = contextlib.ExitStack()
        self.ops = {e: [] for e in ENGS}
        self.phase_dsems = []

    def end_phase(self, final=False):
        self.emit(final)
        self.pstack.close()
        self.pstack = None
        for e in ENGS:
            for k, v in self.semval.items():
                self.waited[e][k] = v
        self.free_dsems.extend(self.phase_dsems)
        self.phase_dsems = []

    def _waits(self, eng, reads, writes, mysem, acc):
        evs = []
        for r in reads:
            if r.w is not None:
                evs.append(r.w)
        for w in writes:
            if w.w is not None and not (acc and w.w[0] == mysem):
                evs.append(w.w)
            for ev in w.r:
                evs.append(ev)
        wd = self.waited[eng]
        best = {}
        for (k, v) in evs:
            if wd.get(k, 0) >= v:
                continue
            best[k] = max(best.get(k, 0), v)
        for k, v in best.items():
            wd[k] = v
        return list(best.items())

    def op(self, eng, fn, reads=(), writes=(), acc=False):
        if self.rec is not None:
            self.rec.append((self.op, (eng, fn, reads, writes, acc)))
            return None
        key = "P_" + eng
        waits = self._waits(eng, reads, writes, key, acc)
        self.semval[key] += 1
        ev = (key, self.semval[key])
        self.ops[eng].append((fn, waits, (key, 1)))
        self.nops += 1
        for r in reads:
            r.r.append(ev)
        for w in writes:
            w.w = ev
            w.r = []
        return ev

    def dma(self, eng, fn, reads=(), writes=(), sem=None):
        if self.rec is not None:
            self.rec.append((self.dma, (eng, fn, reads, writes, sem)))
            return None
        if sem is None:
            if writes and writes[0].name.startswith("sb:"):
                t = writes[0]
                if t.dsem_in is None:
                    t.dsem_in = self.dsem()
                sem = t.dsem_in
            else:
                t = reads[0]
                if t.dsem_out is None:
                    t.dsem_out = self.dsem()
                sem = t.dsem_out
        waits = self._waits(eng, reads, writes, sem, True)
        self.semval[sem] += 16
        ev = (sem, self.semval[sem])
        self.ops[eng].append((fn, waits, (sem, 16)))
        self.nops += 1
        for r in reads:
            r.r.append(ev)
        for w in writes:
            w.w = ev
            w.r = []
        return ev

    def emit(self, final=False):
        nc = self.nc
        sems = self.sems
        fin = [(k, v) for k, v in self.semval.items() if v > 0]
        with nc.Block() as block:
            for e in ENGS:
                ops = self.ops[e]

                def body(eh, ops=ops, e=e):
                    for fn, waits, inc in ops:
                        for k, v in waits:
                            eh.wait_ge(sems[k], v)
                        fn(eh).then_inc(sems[inc[0]], inc[1])
                    if e == "sp":
                        for k, v in fin:
                            eh.wait_ge(sems[k], v)

                getattr(block, HANDLES[e])(body)

    def close(self):
        self.gstack.close()


class Builder:
    def __init__(self, nb=2, depth=DEPTH, dbg=None):
        self.nb = nb
        self.depth = depth
        self.R = nb + 1
        self.dbg = dbg or {}
        nc = self.nc = bass.Bass("TRN2", target_bir_lowering=False)
        self.P = Prog(nc)
        dt = nc.dram_tensor
        A = {}

        def inp(name, shape, dtype=F32):
            A[name] = dt(name, list(shape), dtype, kind="ExternalInput").ap()

        inp("x", [nb, SEQ, D]); inp("c", [nb, D]); inp("ctx", [nb, CTX, D]); inp("c_ctx", [1, D])
        inp("w_mod", [DEPTH, D, 6 * D]); inp("b_mod", [DEPTH, 6 * D])
        inp("ln_g", [DEPTH, 2, D]); inp("ln_b", [DEPTH, 2, D])
        inp("a_w_in", [2, D, 2 * D]); inp("a_norm_g", [2, D]); inp("a_norm_b", [2, D])
        inp("a_w_s", [2, 8, 128, 128]); inp("a_b_s", [2, 8, 128]); inp("a_w_out", [2, D, D])
        inp("b_w_in", [2, D, 3104]); inp("b_w_gate", [2, 2, 16, 512]); inp("b_gate_bias", [2, 2, 512])
        inp("b_gn_g", [2, D]); inp("b_w_out", [2, D, D])
        inp("p_w_q", [DEPTH, D, D]); inp("p_keys", [DEPTH, 2, 128, 64])
        inp("p_u", [DEPTH * NEXP, D]); inp("p_v", [DEPTH * NEXP, D])
        A["y"] = dt("y", [nb, SEQ, D], F32, kind="ExternalOutput").ap()
        skind = "ExternalOutput" if self.dbg else "Internal"
        A["X0"] = dt("X0", [nb, CTX + SEQ, D], F32, kind=skind).ap()
        A["X1"] = dt("X1", [nb, CTX + SEQ, D], F32, kind=skind).ap()
        A["OF"] = dt("OF", [nb, CTX + SEQ, D], F32, kind="Internal").ap()
        A["TAB"] = dt("TAB", [DEPTH * NEXP, 2 * D], BF16, kind="Internal").ap()
        self.rTAB = Res("dr:TAB")
        self.A = A
        self.rX0 = Res("dr:X0"); self.rX1 = Res("dr:X1"); self.rOF = Res("dr:OF"); self.rY = Res("dr:y")
        self.rIN = Res("dr:in")

    def src_ap(self, layer, b, s):
        if layer == 0:
            if s < TCTX:
                return self.A["ctx"][b, s * 128:(s + 1) * 128, :], self.rIN
            return self.A["x"][b, (s - TCTX) * 128:(s - TCTX + 1) * 128, :], self.rIN
        return self.A["X0"][b, s * 128:(s + 1) * 128, :], self.rX0

    def row_of(self, b, s):
        return self.nb if s < TCTX else b

    def consts(self):
        P = self.P
        nb, R = self.nb, self.R
        self.ident, self.r_ident = P.gsbuf("ident", [128, 128], F32)
        self.tri = {}
        for nm in ("IF", "EF", "IB", "EB"):
            self.tri[nm] = P.gsbuf("tri" + nm, [128, 128], F32)
        self.maskF, self.r_maskF = P.gsbuf("maskF", [128, 4, 128], F32)
        self.maskB, self.r_maskB = P.gsbuf("maskB", [128, 4, 128], F32)
        self.ones1, self.r_ones1 = P.gsbuf("ones1", [1, 128], F32)
        self.iota16, self.r_iota16 = P.gsbuf("iota16", [128, 16], F32)
        self.sg, self.r_sg = P.gsbuf("sg", [128, R, 8], F32)
        self.thr17, self.r_thr17 = P.gsbuf("thr17", [128, 17], F32)
        self.modt, self.r_modt = P.gsbuf("modt", [128, R, 3, D], F32)
        self.lng, self.r_lng = P.gsbuf("lng", [128, D], F32)
        self.lnb, self.r_lnb = P.gsbuf("lnb", [128, D], F32)
        P.init_banks()

        P.begin_phase()
        cT, r_cT = P.sbuf("cT", [128, R, 8], F32)
        sg, r_sg = self.sg, self.r_sg
        tmpc, r_tmpc = P.sbuf("tmpc", [128, 4, 128], F32)
        ident = self.ident
        P.op("pool", lambda e: e.memset(ident[:], 0.0), writes=[self.r_ident])
        P.op("pool", lambda e: e.affine_select(out=ident[:], in_=ident[:], pattern=[[-1, 128]], compare_op=ALU.not_equal,
                                               fill=1.0, base=0, channel_multiplier=1),
             reads=[self.r_ident], writes=[self.r_ident])
        P.op("pool", lambda e: e.memset(tmpc[:], -1.0 / 16.0), writes=[r_tmpc])
        spec = {"IF": ([[1, 128]], -1, ALU.is_ge),
                "EF": ([[-1, 128]], 1, ALU.is_gt),
                "IB": ([[-1, 128]], 1, ALU.is_ge),
                "EB": ([[1, 128]], -1, ALU.is_gt)}
        for nm, (pat, cm, cmp) in spec.items():
            t, r = self.tri[nm]
            P.op("pool", lambda e, t=t, pat=pat, cm=cm, cmp=cmp: e.affine_select(
                out=t[:], in_=tmpc[:, 0, :], pattern=pat, compare_op=cmp, fill=0.0, base=0, channel_multiplier=cm),
                reads=[r_tmpc], writes=[r])
        ones4, r_ones4 = P.sbuf("ones4", [128, 4, 128], F32)
        P.op("pool", lambda e: e.memset(ones4[:], 1.0), writes=[r_ones4])
        mF, mB = self.maskF, self.maskB
        P.op("pool", lambda e: e.affine_select(out=mF[:], in_=ones4[:], pattern=[[0, 4], [1, 128]], compare_op=ALU.is_ge,
                                               fill=0.0, base=0, channel_multiplier=-1), reads=[r_ones4], writes=[self.r_maskF])
        P.op("pool", lambda e: e.affine_select(out=mB[:], in_=ones4[:], pattern=[[0, 4], [-1, 128]], compare_op=ALU.is_ge,
                                               fill=0.0, base=0, channel_multiplier=1), reads=[r_ones4], writes=[self.r_maskB])
        o1 = self.ones1
        P.op("pool", lambda e: e.memset(o1[:], 1.0), writes=[self.r_ones1])
        io = self.iota16
        P.op("pool", lambda e: e.iota(io[:], pattern=[[1, 16]], base=0, channel_multiplier=0,
                                      allow_small_or_imprecise_dtypes=True), writes=[self.r_iota16])
        for r in range(R):
            src = self.A["c"][r, :] if r < nb else self.A["c_ctx"][0, :]
            P.dma("sp", lambda e, r=r, src=src: e.dma_start(out=cT[:, r, :], in_=src.rearrange("(kc k) -> k kc", k=128),
                                                            allow_slow_non_contiguous=True),
                  reads=[self.rIN], writes=[r_cT])
        P.op("act", lambda e: e.activation(out=sg[:], in_=cT[:], func=AF.Sigmoid), reads=[r_cT], writes=[r_sg])
        P.op("dve", lambda e: e.tensor_tensor(out=sg[:], in0=sg[:], in1=cT[:], op=ALU.mult), reads=[r_sg, r_cT], writes=[r_sg])
        th = self.thr17
        P.op("pool", lambda e: e.iota(th[:], pattern=[[16, 17]], base=0, channel_multiplier=0,
                                      allow_small_or_imprecise_dtypes=True), writes=[self.r_thr17])
        P.end_phase()

    def table_phase(self):
        P = self.P
        A = self.A
        RR = 4
        P.begin_phase()
        uin = [P.sbuf("tu%d" % k, [128, RR, D], F32) for k in range(2)]
        vin = [P.sbuf("tv%d" % k, [128, RR, D], F32) for k in range(2)]
        tout = [P.sbuf("tt%d" % k, [128, RR, 2, D], BF16) for k in range(2)]
        layers = sorted(set(self.dbg.get("layers", range(self.depth))))
        if self.dbg.get("all_tabs"):
            layers = list(range(DEPTH))
        nchunk = NEXP // (128 * RR)
        it = 0
        for layer in layers:
            for g in range(nchunk):
                r0 = layer * NEXP + g * 128 * RR
                u, r_u = uin[it % 2]
                v, r_v = vin[it % 2]
                t, r_t = tout[it % 2]
                it += 1
                P.dma("sp", lambda e, u=u, r0=r0: e.dma_start(out=u[:], in_=A["p_u"][r0:r0 + 128 * RR, :].rearrange("(p r) d -> p r d", r=RR)),
                      reads=[self.rIN], writes=[r_u])
                P.dma("sp", lambda e, v=v, r0=r0: e.dma_start(out=v[:], in_=A["p_v"][r0:r0 + 128 * RR, :].rearrange("(p r) d -> p r d", r=RR)),
                      reads=[self.rIN], writes=[r_v])
                P.op("dve", lambda e, u=u, t=t: e.tensor_copy(out=t[:, :, 0, :], in_=u[:]), reads=[r_u], writes=[r_t])
                P.op("act", lambda e, v=v, t=t: e.copy(out=t[:, :, 1, :], in_=v[:]), reads=[r_v], writes=[r_t])
                P.dma("sp", lambda e, t=t, r0=r0: e.dma_start(
                    out=A["TAB"][r0:r0 + 128 * RR, :].rearrange("(p r) (two d) -> p r two d", r=RR, two=2), in_=t[:]),
                    reads=[r_t], writes=[Res("dr:tabchunk")])
        P.end_phase()

    def mod_phase(self, layer, half):
        P = self.P
        R = self.R
        P.begin_phase()
        bm, r_bm = P.sbuf("bm", [1, 3 * D], F32)
        scr, r_screp = P.sbuf("screp", [128, R, 8, 128], F32)
        sg = self.sg
        for r in range(R):
            P.op("dve", lambda e, r=r: e.tensor_copy(out=scr[:, r, :, :],
                                                     in_=sg[:, r, :].unsqueeze(2).to_broadcast([128, 8, 128])),
                 reads=[self.r_sg], writes=[r_screp])
        wst = [P.sbuf("wst%d" % k, [128, 8, 512], F32) for k in range(2)]
        P.dma("sp", lambda e: e.dma_start(out=bm[:], in_=self.A["b_mod"][layer:layer + 1, half * 3 * D:(half + 1) * 3 * D]),
              reads=[self.rIN], writes=[r_bm])
        lng, lnb = self.lng, self.lnb
        P.dma("sp", lambda e: e.dma_start(out=lng[:], in_=self.A["ln_g"][layer, half, :].partition_broadcast(128)),
              reads=[self.rIN], writes=[self.r_lng])
        P.dma("sp", lambda e: e.dma_start(out=lnb[:], in_=self.A["ln_b"][layer, half, :].partition_broadcast(128)),
              reads=[self.rIN], writes=[self.r_lnb])
        modt, ones1 = self.modt, self.ones1
        it = 0
        for jj in range(3):
            for nn in range(2):
                c0 = (half * 3 + jj) * D + nn * 512
                w, r_w = wst[it % 2]
                it += 1
                P.dma("sp", lambda e, w=w, c0=c0: e.dma_start(
                    out=w[:], in_=self.A["w_mod"][layer, :, c0:c0 + 512].rearrange("(kc p) n -> p kc n", p=128)),
                    reads=[self.rIN], writes=[r_w])
                for r in range(R):
                    bk, r_bk = P.bank()
                    for kc in range(8):
                        P.op("pe", lambda e, bk=bk, r=r, kc=kc, w=w: e.matmul(bk[:], lhsT=scr[:, r, kc, :], rhs=w[:, kc, :],
                                                                             start=(kc == 0), stop=False),
                             reads=[r_screp, r_w], writes=[r_bk], acc=(kc > 0))
                    P.op("pe", lambda e, bk=bk, jj=jj, nn=nn: e.matmul(bk[:], lhsT=ones1[0:1, :],
                                                                       rhs=bm[0:1, jj * D + nn * 512: jj * D + nn * 512 + 512],
                                                                       start=False, stop=True),
                         reads=[self.r_ones1, r_bm], writes=[r_bk], acc=True)
                    addc = 1.0 if jj == 1 else 0.0
                    P.op("dve", lambda e, bk=bk, r=r, jj=jj, nn=nn, addc=addc: e.tensor_scalar(
                        out=modt[:, r, jj, nn * 512:(nn + 1) * 512], in0=bk[:], scalar1=addc, scalar2=None, op0=ALU.add),
                        reads=[r_bk], writes=[self.r_modt])
        P.end_phase()

    def load_w_bf16(self, dst, r_dst, src, K, N, stg):
        P = self.P
        it = 0
        for kc in range(K // 128):
            SW = stg[0][0].shape[-1]
            for n0 in range(0, N, SW):
                n1 = min(N, n0 + SW)
                s, r_s = stg[it % len(stg)]
                eng = ("act", "dve")[it % 2]
                it += 1
                P.dma("sp", lambda e, s=s, kc=kc, n0=n0, n1=n1: e.dma_start(out=s[:, 0:n1 - n0],
                                                                            in_=src[kc * 128:(kc + 1) * 128, n0:n1]),
                      reads=[self.rIN], writes=[r_s])
                if eng == "act":
                    P.op("act", lambda e, s=s, kc=kc, n0=n0, n1=n1: e.copy(out=dst[:, kc, n0:n1], in_=s[:, 0:n1 - n0]),
                         reads=[r_s], writes=[r_dst])
                else:
                    P.op("dve", lambda e, s=s, kc=kc, n0=n0, n1=n1: e.tensor_copy(out=dst[:, kc, n0:n1], in_=s[:, 0:n1 - n0]),
                         reads=[r_s], writes=[r_dst])

    def bcast_load(self, dst, r_dst, vec):
        self.P.dma("sp", lambda e: e.dma_start(out=dst[:], in_=vec.partition_broadcast(128)), reads=[self.rIN], writes=[r_dst])

    def modulate(self, out, r_out, xt, r_xt, row):
        P = self.P
        modt = self.modt
        P.op("dve", lambda e: e.tensor_tensor(out=out[:], in0=xt[:], in1=modt[:, row, 1, :], op=ALU.mult),
             reads=[r_xt, self.r_modt], writes=[r_out])
        P.op("dve", lambda e: e.tensor_tensor(out=out[:], in0=out[:], in1=modt[:, row, 0, :], op=ALU.add),
             reads=[r_out, self.r_modt], writes=[r_out])

    def transpose_to(self, dst, r_dst, src, r_src, nchunks=8):
        P = self.P
        ident = self.ident
        for g in range(0, nchunks, 4):
            bk, r_bk = P.bank()
            n = min(4, nchunks - g)
            for k in range(n):
                kc = g + k
                P.op("pe", lambda e, bk=bk, k=k, kc=kc: e.transpose(out=bk[:, k * 128:(k + 1) * 128],
                                                                    in_=src[:, kc * 128:(kc + 1) * 128], identity=ident[:]),
                     reads=[r_src, self.r_ident], writes=[r_bk], acc=(k > 0))
            P.op("act", lambda e, bk=bk, g=g, n=n: e.copy(out=dst[:, g:g + n, :].rearrange("p a b -> p (a b)"),
                                                          in_=bk[:, 0:n * 128]),
                 reads=[r_bk], writes=[r_dst])

    def layer_norm(self, out, r_out, yin, r_yin, sm, gb=None):
        P = self.P
        st, r_st = sm["st"]
        mv, r_mv = sm["mv"]
        sd, r_sd = sm["sd"]
        for h2 in range(2):
            P.op("dve", lambda e, h2=h2: e.bn_stats(out=st[:, h2, :], in_=yin[:, h2 * 512:(h2 + 1) * 512]),
                 reads=[r_yin], writes=[r_st])
        P.op("dve", lambda e: e.bn_aggr(out=mv[:], in_=st[:].rearrange("p a b -> p (a b)")), reads=[r_st], writes=[r_mv])
        P.op("act", lambda e: e.activation(out=sd[:, 0:1], in_=mv[:, 1:2], func=AF.Sqrt, bias=sm["eps"][0][:, 0:1], scale=1.0),
             reads=[r_mv, sm["eps"][1]], writes=[r_sd])
        P.op("dve", lambda e: e.reciprocal(out=sd[:, 1:2], in_=sd[:, 0:1]), reads=[r_sd], writes=[r_sd])
        P.op("dve", lambda e: e.tensor_scalar(out=out[:], in0=yin[:], scalar1=mv[:, 0:1], scalar2=sd[:, 1:2],
                                              op0=ALU.subtract, op1=ALU.mult),
             reads=[r_yin, r_mv, r_sd], writes=[r_out])
        if gb is not None:
            (g, r_g), (b, r_b) = gb
            P.op("dve", lambda e: e.tensor_tensor(out=out[:], in0=out[:], in1=g[:], op=ALU.mult), reads=[r_out, r_g], writes=[r_out])
            P.op("dve", lambda e: e.tensor_tensor(out=out[:], in0=out[:], in1=b[:], op=ALU.add), reads=[r_out, r_b], writes=[r_out])

    def small_scratch(self):
        P = self.P
        sm = {"st": P.sbuf("ln_st", [128, 2, 6], F32), "mv": P.sbuf("ln_mv", [128, 2], F32), "sd": P.sbuf("ln_sd", [128, 2], F32),
              "eps": P.sbuf("ln_eps", [128, 1], F32)}
        ep = sm["eps"][0]
        P.op("pool", lambda e: e.memset(ep[:], EPS), writes=[sm["eps"][1]])
        return sm

    def residual_ln_store(self, mix_banks, xt, r_xt, row, sm, ytile, x1tile, dst_ap, r_dst):
        P = self.P
        modt = self.modt
        y, r_y = ytile
        x1, r_x1 = x1tile
        if isinstance(mix_banks, list):
            for n2, (bk, r_bk) in enumerate(mix_banks):
                P.op("dve", lambda e, bk=bk, n2=n2: e.tensor_tensor(out=y[:, n2 * 512:(n2 + 1) * 512], in0=bk[:],
                                                                    in1=modt[:, row, 2, n2 * 512:(n2 + 1) * 512], op=ALU.mult),
                     reads=[r_bk, self.r_modt], writes=[r_y])
        else:
            mt, r_mt = mix_banks
            P.op("dve", lambda e: e.tensor_tensor(out=y[:], in0=mt[:], in1=modt[:, row, 2, :], op=ALU.mult),
                 reads=[r_mt, self.r_modt], writes=[r_y])
        P.op("dve", lambda e: e.scalar_tensor_tensor(out=y[:], in0=xt[:], scalar=ALPHA, in1=y[:], op0=ALU.mult, op1=ALU.add),
             reads=[r_xt, r_y], writes=[r_y])
        self.layer_norm(x1, r_x1, y, r_y, sm, gb=((self.lng, self.r_lng), (self.lnb, self.r_lnb)))
        P.dma("sp", lambda e: e.dma_start(out=dst_ap, in_=x1[:]), reads=[r_x1], writes=[r_dst])

    def cmlp_phase(self, layer):
        P = self.P
        A = self.A
        j = layer // 2
        last = layer == DEPTH - 1
        P.begin_phase()
        sm = self.small_scratch()
        stg = [P.sbuf("stg%d" % k, [128, 2048], F32) for k in range(2)]
        w_in, r_w_in = P.sbuf("w_in", [128, 8, 2048], BF16)
        w_out, r_w_out = P.sbuf("w_out", [128, 8, D], BF16)
        wsT, r_wsT = P.sbuf("wsT", [128, 8, 128], BF16)
        wsraw, r_wsraw = P.sbuf("wsraw", [128, 8, 128], F32)
        bsb, r_bsb = P.sbuf("bsb", [128, 8, 128], F32)
        ng, r_ng = P.sbuf("ng", [128, D], F32)
        nbt, r_nbt = P.sbuf("nbt", [128, D], F32)
        self.load_w_bf16(w_in, r_w_in, A["a_w_in"][j], D, 2 * D, stg)
        self.load_w_bf16(w_out, r_w_out, A["a_w_out"][j], D, D, stg)
        self.bcast_load(ng, r_ng, A["a_norm_g"][j, :])
        self.bcast_load(nbt, r_nbt, A["a_norm_b"][j, :])
        P.dma("sp", lambda e: e.dma_start(out=bsb[:].rearrange("p a b -> p (a b)"),
                                          in_=A["a_b_s"][j].rearrange("h p -> (h p)").partition_broadcast(128)),
              reads=[self.rIN], writes=[r_bsb])
        P.dma("sp", lambda e: e.dma_start(out=wsraw[:], in_=A["a_w_s"][j].rearrange("h p q -> p h q")),
              reads=[self.rIN], writes=[r_wsraw])
        self.transpose_to(wsT, r_wsT, wsraw[:].rearrange("p a b -> p (a b)"), r_wsraw)

        xts = [P.sbuf("xt%d" % k, [128, D], F32) for k in range(2)]
        hf, r_hf = P.sbuf("hf", [128, D], F32)
        hT, r_hT = P.sbuf("hT", [128, 8, 128], BF16)
        uT, r_uT = P.sbuf("uT", [128, 8, 128], F32)
        vs, r_vs = P.sbuf("vs", [128, D], F32)
        vn, r_vn = P.sbuf("vn", [128, D], BF16)
        vnf, r_vnf = P.sbuf("vnf", [128, D], F32)
        usT, r_usT = P.sbuf("usT", [128, 8, 128], BF16)
        tmp, r_tmp = P.sbuf("tmpu", [128, 512], F32)
        ytile = P.sbuf("yt", [128, D], F32)
        x1tile = P.sbuf("x1t", [128, D], F32)

        tiles = [(b, s) for b in range(self.nb) for s in range(TPB) if not (last and s < TCTX)]
        tiles = tiles[:self.dbg.get("max_tiles", 10 ** 9)]
        for ti, (b, s) in enumerate(tiles):
            xt, r_xt = xts[ti % 2]
            src, r_src = self.src_ap(layer, b, s)
            row = self.row_of(b, s)
            P.dma("sp", lambda e, xt=xt, src=src: e.dma_start(out=xt[:], in_=src), reads=[r_src], writes=[r_xt])
            self.modulate(hf, r_hf, xt, r_xt, row)
            self.transpose_to(hT, r_hT, hf, r_hf)
            for g in range(2):
                bk, r_bk = P.bank()
                for k in range(4):
                    fc = g * 4 + k
                    for kc in range(8):
                        P.op("pe", lambda e, bk=bk, k=k, fc=fc, kc=kc: e.matmul(
                            bk[:, k * 128:(k + 1) * 128], lhsT=w_in[:, kc, fc * 128:(fc + 1) * 128], rhs=hT[:, kc, :],
                            start=(kc == 0), stop=(kc == 7)),
                            reads=[r_w_in, r_hT], writes=[r_bk], acc=not (k == 0 and kc == 0))
                P.op("act", lambda e, bk=bk, g=g: e.activation(out=uT[:, g * 4:(g + 1) * 4, :].rearrange("p a b -> p (a b)"),
                                                               in_=bk[:], func=AF.Gelu_apprx_tanh),
                     reads=[r_bk], writes=[r_uT])
            for n2 in range(2):
                bk, r_bk = P.bank()
                for kc in range(8):
                    P.op("pe", lambda e, bk=bk, n2=n2, kc=kc: e.matmul(
                        bk[:], lhsT=hT[:, kc, :], rhs=w_in[:, kc, D + n2 * 512: D + (n2 + 1) * 512],
                        start=(kc == 0), stop=(kc == 7)),
                        reads=[r_w_in, r_hT], writes=[r_bk], acc=(kc > 0))
                P.op("act", lambda e, bk=bk, n2=n2: e.activation(out=vs[:, n2 * 512:(n2 + 1) * 512], in_=bk[:],
                                                                 func=AF.Gelu_apprx_tanh),
                     reads=[r_bk], writes=[r_vs])
            self.layer_norm(vnf, r_vnf, vs, r_vs, sm, gb=((ng, r_ng), (nbt, r_nbt)))
            P.op("act", lambda e: e.copy(out=vn[:], in_=vnf[:]), reads=[r_vnf], writes=[r_vn])
            for g in range(2):
                bk, r_bk = P.bank()
                for k in range(4):
                    hd = g * 4 + k
                    P.op("pe", lambda e, bk=bk, k=k, hd=hd: e.matmul(bk[:, k * 128:(k + 1) * 128],
                                                                     lhsT=vn[:, hd * 128:(hd + 1) * 128], rhs=wsT[:, hd, :],
                                                                     start=True, stop=True),
                         reads=[r_vn, r_wsT], writes=[r_bk], acc=(k > 0))
                P.op("dve", lambda e, bk=bk, g=g: e.tensor_tensor(
                    out=tmp[:], in0=bk[:], in1=bsb[:, g * 4:(g + 1) * 4, :].rearrange("p a b -> p (a b)"), op=ALU.add),
                    reads=[r_bk, r_bsb], writes=[r_tmp])
                P.op("dve", lambda e, g=g: e.tensor_tensor(
                    out=usT[:, g * 4:(g + 1) * 4, :].rearrange("p a b -> p (a b)"), in0=tmp[:],
                    in1=uT[:, g * 4:(g + 1) * 4, :].rearrange("p a b -> p (a b)"), op=ALU.mult),
                    reads=[r_tmp, r_uT], writes=[r_usT])
            mixb = []
            for n2 in range(2):
                bk, r_bk = P.bank()
                for fc in range(8):
                    P.op("pe", lambda e, bk=bk, n2=n2, fc=fc: e.matmul(bk[:], lhsT=usT[:, fc, :],
                                                                       rhs=w_out[:, fc, n2 * 512:(n2 + 1) * 512],
                                                                       start=(fc == 0), stop=(fc == 7)),
                         reads=[r_usT, r_w_out], writes=[r_bk], acc=(fc > 0))
                mixb.append((bk, r_bk))
            self.residual_ln_store(mixb, xt, r_xt, row, sm, ytile, x1tile, A["X1"][b, s * 128:(s + 1) * 128, :], self.rX1)
        P.end_phase()

    def gla_phase(self, layer):
        P = self.P
        A = self.A
        j = layer // 2
        last = layer == DEPTH - 1
        P.begin_phase()
        sm = self.small_scratch()
        ofs = [P.sbuf("of%d" % k, [128, D], F32) for k in range(2)]
        stg = ofs
        w_in, r_w_in = P.sbuf("gw_in", [128, 8, 3104], BF16)
        w_out, r_w_out = P.sbuf("gw_out", [128, 8, D], BF16)
        wg, r_wg = P.sbuf("wg", [16, 2, 512], F32)
        gbias, r_gbias = P.sbuf("gbias", [1, 2, 512], F32)
        gng, r_gng = P.sbuf("gng", [128, D], F32)
        self.load_w_bf16(w_in, r_w_in, A["b_w_in"][j], D, 3104, stg)
        self.load_w_bf16(w_out, r_w_out, A["b_w_out"][j], D, D, stg)
        self.bcast_load(gng, r_gng, A["b_gn_g"][j, :])
        P.dma("sp", lambda e: e.dma_start(out=wg[:], in_=A["b_w_gate"][j].rearrange("d r n -> r d n")),
              reads=[self.rIN], writes=[r_wg])
        P.dma("sp", lambda e: e.dma_start(out=gbias[:], in_=A["b_gate_bias"][j:j + 1, :, :]), reads=[self.rIN], writes=[r_gbias])

        xts = [P.sbuf("xt%d" % k, [128, D], F32) for k in range(2)]
        hf, r_hf = P.sbuf("hf", [128, D], F32)
        hT, r_hT = P.sbuf("hT", [128, 8, 128], BF16)
        v_sb, r_v = P.sbuf("v_sb", [128, D], BF16)
        r_sb, r_r = P.sbuf("r_sb", [128, D], F32)
        glT, r_glT = P.sbuf("glT", [16, 128], F32)
        e1, r_e1 = P.sbuf("e1", [128, 512], F32)
        lsp, r_lsp = P.sbuf("lsp", [128, 512], F32)
        ebT, r_ebT = P.sbuf("ebT", [128, 4, 128], F32)
        enbT, r_enbT = P.sbuf("enbT", [128, 4, 128], F32)
        ebx, r_ebx = e1, r_e1
        qiT, r_qiT = P.sbuf("qiT", [128, 4, 128], BF16)
        kiT, r_kiT = P.sbuf("kiT", [128, 4, 128], BF16)
        kend, r_kend = P.sbuf("kend", [128, 512], BF16)
        attm, r_attm = P.sbuf("attm", [128, 4, 128], BF16)
        S, r_S = P.sbuf("S", [128, 4, 256], F32)
        Sb, r_Sb = P.sbuf("Sb", [128, 4, 256], BF16)
        osum, r_osum = P.sbuf("osum", [128, D], F32)
        gst, r_gst = P.sbuf("gst", [128, 4, 6], F32)
        gmv, r_gmv = P.sbuf("gmv", [128, 4, 2], F32)
        gsd, r_gsd = P.sbuf("gsd", [128, 4, 2], F32)
        yf, r_yf = P.sbuf("yf", [128, D], F32)
        yT, r_yT = P.sbuf("yT", [128, 8, 128], BF16)
        ytile = (osum, r_osum)
        x1tile = (yf, r_yf)
        eps = sm["eps"]

        def project_and_scan(ti, b, s, d):
            xt, r_xt = xts[ti % 2]
            src, r_src = self.src_ap(layer, b, s)
            row = self.row_of(b, s)
            P.dma("sp", lambda e: e.dma_start(out=xt[:], in_=src), reads=[r_src], writes=[r_xt])
            self.modulate(hf, r_hf, xt, r_xt, row)
            self.transpose_to(hT, r_hT, hf, r_hf)
            triI, r_triI = self.tri["IF" if d == 0 else "IB"]
            triE, r_triE = self.tri["EF" if d == 0 else "EB"]
            mask, r_mask = (self.maskF, self.r_maskF) if d == 0 else (self.maskB, self.r_maskB)
            endcol = 127 if d == 0 else 0

            def proj_fm(col0):
                bk, r_bk = P.bank()
                for h in range(4):
                    for kc in range(8):
                        P.op("pe", lambda e, h=h, kc=kc: e.matmul(
                            bk[:, h * 128:(h + 1) * 128], lhsT=w_in[:, kc, col0 + h * 128: col0 + (h + 1) * 128], rhs=hT[:, kc, :],
                            start=(kc == 0), stop=(kc == 7)),
                            reads=[r_w_in, r_hT], writes=[r_bk], acc=not (h == 0 and kc == 0))
                return bk, r_bk

            def proj_tm(col0):
                bk, r_bk = P.bank()
                for kc in range(8):
                    P.op("pe", lambda e, kc=kc: e.matmul(bk[:], lhsT=hT[:, kc, :], rhs=w_in[:, kc, col0:col0 + 512],
                                                         start=(kc == 0), stop=(kc == 7)),
                         reads=[r_w_in, r_hT], writes=[r_bk], acc=(kc > 0))
                return bk, r_bk

            bkg, r_bkg = P.bank()
            gcol = 3072 + 16 * d
            for kc in range(8):
                P.op("pe", lambda e, kc=kc: e.matmul(bkg[0:16, 0:128], lhsT=w_in[:, kc, gcol:gcol + 16], rhs=hT[:, kc, :],
                                                     start=(kc == 0), stop=(kc == 7)),
                     reads=[r_w_in, r_hT], writes=[r_bkg], acc=(kc > 0))
            P.op("act", lambda e: e.copy(out=glT[:], in_=bkg[0:16, 0:128]), reads=[r_bkg], writes=[r_glT])
            bkz, r_bkz = P.bank()
            P.op("pe", lambda e: e.matmul(bkz[:], lhsT=glT[:], rhs=wg[:, d, :], start=True, stop=False),
                 reads=[r_glT, r_wg], writes=[r_bkz])
            P.op("pe", lambda e: e.matmul(bkz[:], lhsT=self.ones1[0:1, :], rhs=gbias[0:1, d, :], start=False, stop=True),
                 reads=[self.r_ones1, r_gbias], writes=[r_bkz], acc=True)
            P.op("act", lambda e: e.activation(out=e1[:], in_=bkz[:], func=AF.Exp, scale=-1.0), reads=[r_bkz], writes=[r_e1])
            P.op("act", lambda e: e.activation(out=lsp[:], in_=e1[:], func=AF.Ln, bias=1.0, scale=1.0), reads=[r_e1], writes=[r_lsp])
            bkb, r_bkb = P.bank()
            for h in range(4):
                P.op("pe", lambda e, h=h: e.matmul(bkb[:, h * 128:(h + 1) * 128], lhsT=lsp[:, h * 128:(h + 1) * 128], rhs=triI[:],
                                                   start=True, stop=True),
                     reads=[r_lsp, r_triI], writes=[r_bkb], acc=(h > 0))
            bkx, r_bkx = P.bank()
            P.op("pe", lambda e: e.matmul(bkx[:], lhsT=triE[:], rhs=lsp[:], start=True, stop=True),
                 reads=[r_lsp, r_triE], writes=[r_bkx])
            P.op("act", lambda e: e.activation(out=ebT[:].rearrange("p a b -> p (a b)"), in_=bkb[:], func=AF.Exp),
                 reads=[r_bkb], writes=[r_ebT])
            P.op("act", lambda e: e.activation(out=enbT[:].rearrange("p a b -> p (a b)"), in_=bkb[:], func=AF.Exp, scale=-1.0),
                 reads=[r_bkb], writes=[r_enbT])
            P.op("act", lambda e: e.activation(out=ebx[:], in_=bkx[:], func=AF.Exp), reads=[r_bkx], writes=[r_ebx])
            bq, r_bq = proj_fm(0)
            P.op("dve", lambda e: e.scalar_tensor_tensor(out=qiT[:].rearrange("p a b -> p (a b)"), in0=bq[:], scalar=128.0 ** -0.5,
                                                         in1=ebT[:].rearrange("p a b -> p (a b)"), op0=ALU.mult, op1=ALU.mult),
                 reads=[r_bq, r_ebT], writes=[r_qiT])
            bkT, r_bkT = proj_fm(512)
            P.op("dve", lambda e: e.tensor_tensor(out=kiT[:].rearrange("p a b -> p (a b)"), in0=bkT[:],
                                                  in1=enbT[:].rearrange("p a b -> p (a b)"), op=ALU.mult),
                 reads=[r_bkT, r_enbT], writes=[r_kiT])
            bk_, r_bk_ = proj_tm(512)
            P.op("dve", lambda e: e.tensor_tensor(out=kend[:], in0=bk_[:], in1=ebx[:], op=ALU.mult),
                 reads=[r_bk_, r_ebx], writes=[r_kend])
            for n2 in range(2):
                bv, r_bv = proj_tm(1024 + n2 * 512)
                P.op("act", lambda e, n2=n2, bv=bv: e.copy(out=v_sb[:, n2 * 512:(n2 + 1) * 512], in_=bv[:]),
                     reads=[r_bv], writes=[r_v])
            if d == 1:
                for n2 in range(2):
                    br, r_br = proj_tm(2048 + n2 * 512)
                    P.op("act", lambda e, n2=n2, br=br: e.activation(out=r_sb[:, n2 * 512:(n2 + 1) * 512], in_=br[:], func=AF.Silu),
                         reads=[r_br], writes=[r_r])
            bka, r_bka = P.bank()
            for h in range(4):
                P.op("pe", lambda e, h=h: e.matmul(bka[:, h * 128:(h + 1) * 128], lhsT=kiT[:, h, :], rhs=qiT[:, h, :],
                                                   start=True, stop=True),
                     reads=[r_kiT, r_qiT], writes=[r_bka], acc=(h > 0))
            P.op("dve", lambda e: e.tensor_tensor(out=attm[:].rearrange("p a b -> p (a b)"), in0=bka[:],
                                                  in1=mask[:].rearrange("p a b -> p (a b)"), op=ALU.mult),
                 reads=[r_bka, r_mask], writes=[r_attm])
            obanks = []
            for g in range(2):
                bo, r_bo = P.bank()
                for k in range(2):
                    h = g * 2 + k
                    P.op("pe", lambda e, bo=bo, k=k, h=h: e.matmul(bo[:, k * 256:(k + 1) * 256], lhsT=attm[:, h, :],
                                                                   rhs=v_sb[:, h * 256:(h + 1) * 256], start=True, stop=False),
                         reads=[r_attm, r_v], writes=[r_bo], acc=(k > 0))
                    P.op("pe", lambda e, bo=bo, k=k, h=h: e.matmul(bo[:, k * 256:(k + 1) * 256], lhsT=qiT[:, h, :],
                                                                   rhs=Sb[:, h, :], start=False, stop=True),
                         reads=[r_qiT, r_Sb], writes=[r_bo], acc=True)
                obanks.append((bo, r_bo))
            for g in range(2):
                bs, r_bs = P.bank()
                for k in range(2):
                    h = g * 2 + k
                    P.op("pe", lambda e, bs=bs, k=k, h=h: e.matmul(bs[:, k * 256:(k + 1) * 256], lhsT=kend[:, h * 128:(h + 1) * 128],
                                                                   rhs=v_sb[:, h * 256:(h + 1) * 256], start=True, stop=True),
                         reads=[r_kend, r_v], writes=[r_bs], acc=(k > 0))
                for k in range(2):
                    h = g * 2 + k
                    P.op("dve", lambda e, bs=bs, k=k, h=h: e.scalar_tensor_tensor(
                        out=S[:, h, :], in0=S[:, h, :], scalar=ebT[:, h, endcol:endcol + 1], in1=bs[:, k * 256:(k + 1) * 256],
                        op0=ALU.mult, op1=ALU.add),
                        reads=[r_S, r_ebT, r_bs], writes=[r_S])
            P.op("act", lambda e: e.copy(out=Sb[:].rearrange("p a b -> p (a b)"), in_=S[:].rearrange("p a b -> p (a b)")),
                 reads=[r_S], writes=[r_Sb])
            return obanks, (xt, r_xt), row

        def reset_state():
            P.op("pool", lambda e: e.memset(S[:], 0.0), writes=[r_S])
            P.op("pool", lambda e: e.memset(Sb[:], 0.0), writes=[r_Sb])

        ti = 0
        for b in range(self.nb):
            reset_state()
            for s in range(TPB):
                obanks, _, _ = project_and_scan(ti, b, s, 0)
                of, r_of = ofs[ti % 2]
                for g, (bo, r_bo) in enumerate(obanks):
                    P.op("act", lambda e, bo=bo, g=g, of=of: e.copy(out=of[:, g * 512:(g + 1) * 512], in_=bo[:]),
                         reads=[r_bo], writes=[r_of])
                P.dma("sp", lambda e, of=of, b=b, s=s: e.dma_start(out=A["OF"][b, s * 128:(s + 1) * 128, :], in_=of[:]),
                      reads=[r_of], writes=[self.rOF])
                ti += 1
        for b in range(self.nb):
            reset_state()
            order = [1, 0] + list(range(TPB - 1, TCTX - 1, -1))
            for s in order:
                of, r_of = ofs[ti % 2]
                if not (last and s < TCTX):
                    P.dma("sp", lambda e, of=of, b=b, s=s: e.dma_start(out=of[:], in_=A["OF"][b, s * 128:(s + 1) * 128, :]),
                          reads=[self.rOF], writes=[r_of])
                obanks, (xt, r_xt), row = project_and_scan(ti, b, s, 1)
                ti += 1
                if last and s < TCTX:
                    continue
                for g, (bo, r_bo) in enumerate(obanks):
                    P.op("dve", lambda e, bo=bo, g=g, of=of: e.tensor_tensor(out=osum[:, g * 512:(g + 1) * 512], in0=bo[:],
                                                                            in1=of[:, g * 512:(g + 1) * 512], op=ALU.add),
                         reads=[r_bo, r_of], writes=[r_osum])
                for h in range(4):
                    P.op("dve", lambda e, h=h: e.bn_stats(out=gst[:, h, :], in_=osum[:, h * 256:(h + 1) * 256]),
                         reads=[r_osum], writes=[r_gst])
                for h in range(4):
                    P.op("dve", lambda e, h=h: e.bn_aggr(out=gmv[:, h, :], in_=gst[:, h, :]), reads=[r_gst], writes=[r_gmv])
                P.op("act", lambda e: e.activation(out=gsd[:, :, 0], in_=gmv[:, :, 1], func=AF.Sqrt, bias=eps[0][:, 0:1], scale=1.0),
                     reads=[r_gmv, eps[1]], writes=[r_gsd])
                P.op("dve", lambda e: e.reciprocal(out=gsd[:, :, 1], in_=gsd[:, :, 0]), reads=[r_gsd], writes=[r_gsd])
                for h in range(4):
                    P.op("dve", lambda e, h=h: e.tensor_scalar(out=yf[:, h * 256:(h + 1) * 256], in0=osum[:, h * 256:(h + 1) * 256],
                                                               scalar1=gmv[:, h, 0:1], scalar2=gsd[:, h, 1:2],
                                                               op0=ALU.subtract, op1=ALU.mult),
                         reads=[r_osum, r_gmv, r_gsd], writes=[r_yf])
                P.op("dve", lambda e: e.tensor_tensor(out=yf[:], in0=yf[:], in1=gng[:], op=ALU.mult), reads=[r_yf, r_gng], writes=[r_yf])
                P.op("dve", lambda e: e.tensor_tensor(out=yf[:], in0=yf[:], in1=r_sb[:], op=ALU.mult), reads=[r_yf, r_r], writes=[r_yf])
                self.transpose_to(yT, r_yT, yf, r_yf)
                mixb = []
                for n2 in range(2):
                    bk, r_bk = P.bank()
                    for fc in range(8):
                        P.op("pe", lambda e, bk=bk, n2=n2, fc=fc: e.matmul(bk[:], lhsT=yT[:, fc, :],
                                                                           rhs=w_out[:, fc, n2 * 512:(n2 + 1) * 512],
                                                                           start=(fc == 0), stop=(fc == 7)),
                             reads=[r_yT, r_w_out], writes=[r_bk], acc=(fc > 0))
                    mixb.append((bk, r_bk))
                self.residual_ln_store(mixb, xt, r_xt, row, sm, ytile, x1tile, A["X1"][b, s * 128:(s + 1) * 128, :], self.rX1)
        P.end_phase()

    def peer_phase(self, layer, final_out):
        P = self.P
        A = self.A
        last = layer == DEPTH - 1
        P.begin_phase()
        sm = self.small_scratch()
        wq, r_wq = P.sbuf("wq", [128, 8, D], F32)
        kbd, r_kbd = P.sbuf("kbd", [128, 256], F32)
        kraw, r_kraw = P.sbuf("kraw", [128, 2, 64], F32)
        P.dma("sp", lambda e: e.dma_start(out=wq[:], in_=A["p_w_q"][layer].rearrange("(kc p) n -> p kc n", p=128)),
              reads=[self.rIN], writes=[r_wq])
        P.dma("sp", lambda e: e.dma_start(out=kraw[:], in_=A["p_keys"][layer].rearrange("p k d -> k p d")),
              reads=[self.rIN], writes=[r_kraw])
        P.op("pool", lambda e: e.memset(kbd[:], 0.0), writes=[r_kbd])
        bkk, r_bkk = P.bank()
        P.op("pe", lambda e: e.transpose(out=bkk[:, 0:128], in_=kraw[:].rearrange("p a b -> p (a b)"), identity=self.ident[:]),
             reads=[r_kraw, self.r_ident], writes=[r_bkk])
        P.op("dve", lambda e: e.tensor_copy(out=kbd[0:64, 0:128], in_=bkk[0:64, 0:128]), reads=[r_bkk], writes=[r_kbd])
        P.op("dve", lambda e: e.tensor_copy(out=kbd[64:128, 128:256], in_=bkk[64:128, 0:128]), reads=[r_bkk], writes=[r_kbd])

        xts = [P.sbuf("xt%d" % k, [128, D], F32) for k in range(2)]
        h2s = [P.sbuf("h2_%d" % k, [128, D], F32) for k in range(2)]
        h2T, r_h2T = P.sbuf("h2T", [128, 8, 128], F32)
        qT, r_qT = P.sbuf("qT", [128, 8, 128], F32)
        sc, r_sc = P.sbuf("sc", [128, 16, 128], F32)
        work, r_work = P.sbuf("work", [128, 256], F32)
        s12, r_s12 = P.sbuf("s12", [128, 16, 16], F32)
        i12, r_i12 = P.sbuf("i12", [128, 16, 16], U32)
        i12f, r_i12f = P.sbuf("i12f", [128, 16, 16], F32)
        cand, r_cand = P.sbuf("cand", [128, 8, 256], F32)
        tops, r_tops = P.sbuf("tops", [128, 8, 16], F32)
        pos, r_pos = P.sbuf("pos", [128, 8, 16], U32)
        posf, r_posf = P.sbuf("posf", [128, 128], F32)
        pjf, r_pjf = P.sbuf("pjf", [128, 128], F32)
        pkf, r_pkf = P.sbuf("pkf", [128, 128], F32)
        ge, r_ge = P.sbuf("ge", [128, 128, 17], F32)
        oh, r_oh = cand[:].rearrange("p h (a b) -> p h a b", a=16), r_cand
        sel1, r_sel1 = P.sbuf("sel1", [128, 8, 16], F32)
        sel2, r_sel2 = P.sbuf("sel2", [128, 8, 16], F32)
        eidf, r_eidf = P.sbuf("eidf", [128, 128], F32)
        eids = [P.sbuf("eid%d" % k, [128, 128], I32) for k in range(2)]
        ex, r_ex = P.sbuf("ex", [128, 8, 16], F32)
        esum, r_esum = P.sbuf("esum", [128, 8], F32)
        wtss = [P.sbuf("wts%d" % k, [128, 128], F32) for k in range(2)]
        dots, r_dots = P.sbuf("dots", [128, 128], F32)
        actw, r_actw = P.sbuf("actw", [128, 128], F32)
        acc, r_acc = P.sbuf("acc", [128, D], F32)
        ring = [P.sbuf("ring%d" % k, [128, GSL, 2 * D], BF16) for k in range(NRING)]
        for k in range(NRING):
            ring[k][1].dsem_in = P.swsem(k)
        ytile = (acc, r_acc)
        x1tile = P.sbuf("x1t", [128, D], F32)
        ring_i = [0]
        r_s12g = [Res("sb:s12g%d" % k) for k in range(2)]
        r_i12g = [Res("sb:i12g%d" % k) for k in range(2)]
        r_workg = [Res("sb:workg%d" % k) for k in range(2)]
        r_topsg = [Res("sb:topsg%d" % k) for k in range(2)]
        r_posg = [Res("sb:posg%d" % k) for k in range(2)]
        work2, _ = P.sbuf("work2", [128, 256], F32)
        work3, _ = P.sbuf("work3", [128, 256], F32)
        r_workh = [Res("sb:workh%d" % k) for k in range(2)]
        junks = [P.sbuf("junkb%d" % k, [128, D], BF16) for k in range(2)]
        r_dots_s = [[Res("sb:dots%d_%d" % (k, q)) for q in range(GSL)] for k in range(4)]
        r_dots_g = [Res("sb:dotsg%d" % k) for k in range(4)]
        r_actw_g = [Res("sb:actwg%d" % k) for k in range(4)]
        P.bank_mod = 6
        accb = [P.banks[6], P.banks[7]]
        dgs = [P.sbuf("dg%d" % k, [128, 128], BF16) for k in range(4)]
        ident = self.ident

        tiles = [(b, s) for b in range(self.nb) for s in range(TPB) if not (last and s < TCTX)]
        tiles = tiles[:self.dbg.get("max_tiles", 10 ** 9)]
        tab = A["TAB"]
        def stage1(ti):
            b, s = tiles[ti]
            xt, r_xt = xts[ti % 2]
            eid, r_eid = eids[ti % 2]
            h2, r_h2 = h2s[ti % 2]
            wts, r_wts = wtss[ti % 2]
            row = self.row_of(b, s)
            P.dma("sp", lambda e, xt=xt, b=b, s=s: e.dma_start(out=xt[:], in_=A["X1"][b, s * 128:(s + 1) * 128, :]),
                  reads=[self.rX1], writes=[r_xt])
            self.modulate(h2, r_h2, xt, r_xt, row)
            self.transpose_to(h2T, r_h2T, h2, r_h2)
            for g in range(2):
                bk, r_bk = P.bank()
                for k in range(4):
                    hd = g * 4 + k
                    for kc in range(8):
                        P.op("pe", lambda e, bk=bk, k=k, hd=hd, kc=kc: e.matmul(
                            bk[:, k * 128:(k + 1) * 128], lhsT=wq[:, kc, hd * 128:(hd + 1) * 128], rhs=h2T[:, kc, :],
                            start=(kc == 0), stop=(kc == 7)),
                            reads=[r_wq, r_h2T], writes=[r_bk], acc=not (k == 0 and kc == 0))
                P.op("act", lambda e, bk=bk, g=g: e.copy(out=qT[:, g * 4:(g + 1) * 4, :].rearrange("p a b -> p (a b)"), in_=bk[:]),
                     reads=[r_bk], writes=[r_qT])
            for g in range(4):
                bk, r_bk = P.bank()
                for k in range(2):
                    hd = g * 2 + k
                    P.op("pe", lambda e, bk=bk, k=k, hd=hd: e.matmul(bk[:, k * 256:(k + 1) * 256], lhsT=qT[:, hd, :], rhs=kbd[:],
                                                                     start=True, stop=True),
                         reads=[r_qT, r_kbd], writes=[r_bk], acc=(k > 0))
                P.op("act", lambda e, bk=bk, g=g: e.copy(out=sc[:, g * 4:(g + 1) * 4, :].rearrange("p a b -> p (a b)"), in_=bk[:]),
                     reads=[r_bk], writes=[r_sc])
            for g0 in range(0, 16, 2):
                gs = (g0, g0 + 1)
                for k, g in enumerate(gs):
                    P.op("dve", lambda e, g=g: e.max(out=s12[:, g, 0:8], in_=sc[:, g, :]), reads=[r_sc], writes=[r_s12g[k]])
                for k, g in enumerate(gs):
                    P.op("dve", lambda e, g=g: e.max_index(out=i12[:, g, 0:8], in_max=s12[:, g, 0:8], in_values=sc[:, g, :]),
                         reads=[r_sc, r_s12g[k]], writes=[r_i12g[k]])
                for k, g in enumerate(gs):
                    P.op("dve", lambda e, g=g, k=k: e.match_replace(out=work[:, k * 128:(k + 1) * 128], in_to_replace=s12[:, g, 0:8],
                                                                    in_values=sc[:, g, :], imm_value=-1e30),
                         reads=[r_sc, r_s12g[k]], writes=[r_workg[k]])
                for k, g in enumerate(gs):
                    P.op("dve", lambda e, g=g, k=k: e.max(out=s12[:, g, 8:16], in_=work[:, k * 128:(k + 1) * 128]),
                         reads=[r_workg[k]], writes=[r_s12g[k]])
                for k, g in enumerate(gs):
                    P.op("dve", lambda e, g=g, k=k: e.max_index(out=i12[:, g, 8:16], in_max=s12[:, g, 8:16],
                                                                in_values=work[:, k * 128:(k + 1) * 128]),
                         reads=[r_workg[k], r_s12g[k]], writes=[r_i12g[k]])
            P.op("dve", lambda e: e.tensor_copy(out=i12f[:], in_=i12[:]), reads=r_i12g, writes=[r_i12f])
            s12v = s12[:].rearrange("p (h two) n -> p h two n", two=2)
            P.op("dve", lambda e: e.tensor_tensor(out=cand[:].rearrange("p h (a b) -> p h a b", a=16),
                                                  in0=s12v[:, :, 0, :].unsqueeze(3).to_broadcast([128, 8, 16, 16]),
                                                  in1=s12v[:, :, 1, :].unsqueeze(2).to_broadcast([128, 8, 16, 16]), op=ALU.add),
                 reads=r_s12g, writes=[r_cand])
            wk = (work2, work3)
            for h0 in range(0, 8, 2):
                hs = (h0, h0 + 1)
                for k, hd in enumerate(hs):
                    P.op("dve", lambda e, hd=hd: e.max(out=tops[:, hd, 0:8], in_=cand[:, hd, :]), reads=[r_cand], writes=[r_topsg[k]])
                for k, hd in enumerate(hs):
                    P.op("dve", lambda e, hd=hd: e.max_index(out=pos[:, hd, 0:8], in_max=tops[:, hd, 0:8], in_values=cand[:, hd, :]),
                         reads=[r_cand, r_topsg[k]], writes=[r_posg[k]])
                for k, hd in enumerate(hs):
                    P.op("dve", lambda e, hd=hd, k=k: e.match_replace(out=wk[k][:], in_to_replace=tops[:, hd, 0:8], in_values=cand[:, hd, :],
                                                                      imm_value=-1e30), reads=[r_cand, r_topsg[k]], writes=[r_workh[k]])
                for k, hd in enumerate(hs):
                    P.op("dve", lambda e, hd=hd, k=k: e.max(out=tops[:, hd, 8:16], in_=wk[k][:]), reads=[r_workh[k]], writes=[r_topsg[k]])
                for k, hd in enumerate(hs):
                    P.op("dve", lambda e, hd=hd, k=k: e.max_index(out=pos[:, hd, 8:16], in_max=tops[:, hd, 8:16], in_values=wk[k][:]),
                         reads=[r_workh[k], r_topsg[k]], writes=[r_posg[k]])
            r_tops_all = r_topsg
            r_pos_all = r_posg
            P.op("dve", lambda e: e.tensor_tensor(out=ex[:], in0=tops[:], in1=tops[:, :, 0:1].to_broadcast([128, 8, 16]),
                                                  op=ALU.subtract), reads=r_tops_all, writes=[r_ex])
            P.op("act", lambda e: e.activation(out=ex[:], in_=ex[:], func=AF.Exp), reads=[r_ex], writes=[r_ex])
            P.op("dve", lambda e: e.tensor_reduce(out=esum[:], in_=ex[:], axis=AX.X, op=ALU.add), reads=[r_ex], writes=[r_esum])
            P.op("dve", lambda e: e.reciprocal(out=esum[:], in_=esum[:]), reads=[r_esum], writes=[r_esum])
            P.op("dve", lambda e: e.tensor_tensor(out=wts[:].rearrange("p (h n) -> p h n", h=8), in0=ex[:],
                                                  in1=esum[:].unsqueeze(2).to_broadcast([128, 8, 16]), op=ALU.mult),
                 reads=[r_ex, r_esum], writes=[r_wts])
            P.op("dve", lambda e: e.tensor_copy(out=posf[:], in_=pos[:].rearrange("p h n -> p (h n)")), reads=r_pos_all, writes=[r_posf])
            th = self.thr17
            iot = self.iota16
            i12v = i12f[:].rearrange("p (h two) n -> p h two n", two=2)
            P.op("dve", lambda e: e.tensor_tensor(out=ge[:], in0=posf[:].unsqueeze(2).to_broadcast([128, 128, 17]),
                                                  in1=th[:].unsqueeze(1).to_broadcast([128, 128, 17]), op=ALU.is_ge),
                 reads=[r_posf, self.r_thr17], writes=[r_ge])
            P.op("dve", lambda e: e.tensor_reduce(out=pjf[:], in_=ge[:, :, 1:17], axis=AX.X, op=ALU.add), reads=[r_ge], writes=[r_pjf])
            P.op("dve", lambda e: e.scalar_tensor_tensor(out=pkf[:], in0=pjf[:], scalar=-16.0, in1=posf[:], op0=ALU.mult, op1=ALU.add),
                 reads=[r_pjf, r_posf], writes=[r_pkf])
            ohf = oh.rearrange("p h n j -> p (h n) j")
            P.op("dve", lambda e: e.tensor_tensor(out=ohf, in0=ge[:, :, 0:16], in1=ge[:, :, 1:17], op=ALU.subtract),
                 reads=[r_ge], writes=[r_oh])
            P.op("dve", lambda e: e.tensor_tensor(out=oh, in0=oh, in1=i12v[:, :, 0, :].unsqueeze(2).to_broadcast([128, 8, 16, 16]),
                                                  op=ALU.mult), reads=[r_oh, r_i12f], writes=[r_oh])
            P.op("dve", lambda e: e.tensor_reduce(out=sel1[:].rearrange("p h n -> p (h n)"), in_=ohf, axis=AX.X, op=ALU.add),
                 reads=[r_oh], writes=[r_sel1])
            P.op("dve", lambda e: e.tensor_tensor(out=ohf, in0=pkf[:].unsqueeze(2).to_broadcast([128, 128, 16]),
                                                  in1=iot[:].unsqueeze(1).to_broadcast([128, 128, 16]), op=ALU.is_equal),
                 reads=[r_pkf, self.r_iota16], writes=[r_oh])
            P.op("dve", lambda e: e.tensor_tensor(out=oh, in0=oh, in1=i12v[:, :, 1, :].unsqueeze(2).to_broadcast([128, 8, 16, 16]),
                                                  op=ALU.mult), reads=[r_oh, r_i12f], writes=[r_oh])
            P.op("dve", lambda e: e.tensor_reduce(out=sel2[:].rearrange("p h n -> p (h n)"), in_=ohf, axis=AX.X, op=ALU.add),
                 reads=[r_oh], writes=[r_sel2])
            P.op("dve", lambda e: e.scalar_tensor_tensor(out=eidf[:], in0=sel1[:].rearrange("p h n -> p (h n)"), scalar=128.0,
                                                         in1=sel2[:].rearrange("p h n -> p (h n)"), op0=ALU.mult, op1=ALU.add),
                 reads=[r_sel1, r_sel2], writes=[r_eidf])
            if layer > 0:
                P.op("dve", lambda e: e.tensor_scalar(out=eidf[:], in0=eidf[:], scalar1=float(layer * NEXP), scalar2=None, op0=ALU.add),
                     reads=[r_eidf], writes=[r_eidf])
            P.op("dve", lambda e, eid=eid: e.tensor_copy(out=eid[:], in_=eidf[:]), reads=[r_eidf], writes=[r_eid])

        stage1(0)
        for ti, (b, s) in enumerate(tiles):
            xt, r_xt = xts[ti % 2]
            eid, r_eid = eids[ti % 2]
            h2, r_h2 = h2s[ti % 2]
            wts, r_wts = wtss[ti % 2]
            row = self.row_of(b, s)
            pend = []
            if ti + 1 < len(tiles):
                P.rec = []
                stage1(ti + 1)
                pend, P.rec = P.rec, None
            pstate = [0]

            def pump(k, pend=pend, pstate=pstate):
                while k > 0 and pstate[0] < len(pend):
                    fn, a = pend[pstate[0]]
                    fn(*a)
                    pstate[0] += 1
                    k -= 1
            for gi in range(128 // GSL):
                rb, r_rb = ring[ring_i[0] % NRING]
                ring_i[0] += 1
                c0 = gi * GSL
                r_actw = r_actw_g[gi % 4]
                for sl in range(GSL):
                    cidx = c0 + sl
                    P.dma("pool", lambda e, rb=rb, sl=sl, cidx=cidx, eid=eid: e.indirect_dma_start(
                        out=rb[:, sl, :], out_offset=None, in_=tab,
                        in_offset=bass.IndirectOffsetOnAxis(ap=eid[:, cidx:cidx + 1], axis=0)),
                        reads=[r_eid, self.rTAB], writes=[r_rb])
                for sl in range(GSL):
                    cidx = c0 + sl
                    jk, r_jk = junks[cidx % 2]
                    P.op("dve", lambda e, rb=rb, sl=sl, cidx=cidx, h2=h2, jk=jk: e.scalar_tensor_tensor(
                        out=jk[:], in0=rb[:, sl, 0:D], scalar=1.0, in1=h2[:], op0=ALU.mult, op1=ALU.mult,
                        accum_out=dots[:, cidx:cidx + 1]),
                        reads=[r_rb, r_h2], writes=[r_jk, r_dots_s[gi % 4][sl]])
                    pump(1)
                P.op("act", lambda e, c0=c0: e.activation(out=actw[:, c0:c0 + GSL], in_=dots[:, c0:c0 + GSL], func=AF.Gelu_apprx_tanh),
                     reads=r_dots_s[gi % 4], writes=[r_actw])
                P.op("dve", lambda e, c0=c0, wts=wts: e.tensor_tensor(out=actw[:, c0:c0 + GSL], in0=actw[:, c0:c0 + GSL],
                                                                      in1=wts[:, c0:c0 + GSL], op=ALU.mult),
                     reads=[r_actw, r_wts], writes=[r_actw])
                for sl in range(GSL):
                    cidx = c0 + sl
                    dg, r_dg = dgs[cidx % 4]
                    P.op("act", lambda e, dg=dg, cidx=cidx: e.activation(out=dg[:], in_=ident[:], func=AF.Copy,
                                                                         scale=actw[:, cidx:cidx + 1]),
                         reads=[r_actw, self.r_ident], writes=[r_dg])
                    for n2 in range(2):
                        bk, r_bk = accb[n2]
                        P.op("pe", lambda e, bk=bk, dg=dg, rb=rb, sl=sl, n2=n2, cidx=cidx: e.matmul(
                            bk[:], lhsT=dg[:], rhs=rb[:, sl, D + n2 * 512: D + (n2 + 1) * 512],
                            start=(cidx == 0), stop=(cidx == 127)),
                            reads=[r_dg, r_rb], writes=[r_bk], acc=(cidx > 0))
                    pump(1)
            if final_out and s >= TCTX:
                dst, r_dst = A["y"][b, (s - TCTX) * 128:(s - TCTX + 1) * 128, :], self.rY
            else:
                dst, r_dst = A["X0"][b, s * 128:(s + 1) * 128, :], self.rX0
            self.residual_ln_store(accb, xt, r_xt, row, sm, ytile, x1tile, dst, r_dst)
            pump(10 ** 9)
        P.bank_mod = 8
        P.end_phase()

    def build(self):
        self.consts()
        if self.dbg.get("consts_only"):
            self.P.close()
            return self.nc
        if not (self.dbg.get("mod_only") or "stop_after_mixer" in self.dbg):
            self.table_phase()
        for layer in self.dbg.get("layers", range(self.depth)):
            final = layer == self.depth - 1
            self.mod_phase(layer, 0)
            if self.dbg.get("mod_only"):
                break
            if layer % 2 == 0:
                self.cmlp_phase(layer)
            else:
                self.gla_phase(layer)
            if self.dbg.get("stop_after_mixer") == layer:
                break
            self.mod_phase(layer, 1)
            self.peer_phase(layer, final)
        self.P.close()
        return self.nc


_W_NAMES = ["w_mod", "b_mod", "ln_g", "ln_b", "a_w_in", "a_norm_g", "a_norm_b", "a_w_s", "a_b_s", "a_w_out",
            "b_w_in", "b_w_gate", "b_gate_bias", "b_gn_g", "b_w_out", "p_w_q", "p_keys", "p_u", "p_v"]


def make_in_maps(inputs, n_cores, nb):
    f = lambda a: np.ascontiguousarray(np.asarray(a, dtype=np.float32))
    shared = {k: f(inputs[k]) for k in _W_NAMES}
    shared["p_u"] = shared["p_u"].reshape(DEPTH * NEXP, D)
    shared["p_v"] = shared["p_v"].reshape(DEPTH * NEXP, D)
    shared["c_ctx"] = f(inputs["c_ctx"]).reshape(1, D)
    x, c, ctx = f(inputs["x"]), f(inputs["c"]), f(inputs["ctx"])
    maps = []
    for i in range(n_cores):
        m = dict(shared)
        m["x"] = x[i * nb:(i + 1) * nb]
        m["c"] = c[i * nb:(i + 1) * nb]
        m["ctx"] = ctx[i * nb:(i + 1) * nb]
        maps.append(m)
    return maps


def kernel(**inputs):
    nb = 2
    nc = Builder(nb=nb).build()
    in_maps = make_in_maps(inputs, NCORES, nb)
    res = run_bass_kernel_spmd(nc, in_maps, core_ids=list(range(NCORES)))
    return np.concatenate([r["y"] for r in res.results], axis=0).astype(np.float32)
```

```python
import contextlib
import numpy as np
import concourse.bass as bass
import concourse.mybir as mybir
from concourse.bass_utils import run_bass_kernel_spmd

F32 = mybir.dt.float32
F32R = mybir.dt.float32r
BF16 = mybir.dt.bfloat16
I32 = mybir.dt.int32
U32 = mybir.dt.uint32
ALU = mybir.AluOpType
AF = mybir.ActivationFunctionType
AX = mybir.AxisListType

D = 1024
SEQ = 2048
CTX = 256
DEPTH = 4
NCORES = 8
TCTX = CTX // 128
TLAT = SEQ // 128
TPB = TCTX + TLAT
ALPHA = float((2.0 * DEPTH) ** 0.25)
EPS = 1e-5
NEXP = 16384
GSL = 2
NRING = 5

ENGS = ("pe", "dve", "act", "pool", "sp")
HANDLES = {"pe": "tensor", "dve": "vector", "act": "scalar", "pool": "gpsimd", "sp": "sync"}


class Res:
    __slots__ = ("name", "w", "r", "dsem_in", "dsem_out")

    def __init__(self, name):
        self.name = name
        self.w = None
        self.r = []
        self.dsem_in = None
        self.dsem_out = None


class Prog:
    def __init__(self, nc):
        self.nc = nc
        self.gstack = contextlib.ExitStack()
        self.pstack = None
        self.ops = {e: [] for e in ENGS}
        self.sems = {}
        self.semval = {}
        self.waited = {e: {} for e in ENGS}
        for e in ENGS:
            if e != "sp":
                self._newsem("P_" + e)
        self.ndsem = 0
        self.free_dsems = []
        self.phase_dsems = []
        self.banks = []
        self.bank_i = 0
        self.bank_mod = 8
        self.nops = 0
        self.rec = None

    def _newsem(self, key):
        h = self.gstack.enter_context(self.nc.semaphore(key))
        self.sems[key] = h
        self.semval[key] = 0
        return key

    def dsem(self):
        if self.free_dsems:
            k = self.free_dsems.pop()
        else:
            self.ndsem += 1
            k = self._newsem("D%d" % self.ndsem)
        self.phase_dsems.append(k)
        return k

    def swsem(self, i):
        k = "SW%d" % i
        if k not in self.sems:
            self._newsem(k)
        return k

    def gsbuf(self, name, shape, dt):
        t = self.gstack.enter_context(self.nc.sbuf_tensor(name, list(shape), dt))
        r = Res("sb:" + name)
        self.ndsem += 1
        r.dsem_in = self._newsem("G%d" % self.ndsem)
        return t, r

    def sbuf(self, name, shape, dt):
        name = "%s_p%d" % (name, self.phase_id)
        t = self.pstack.enter_context(self.nc.sbuf_tensor(name, list(shape), dt))
        return t, Res("sb:" + name)

    def init_banks(self):
        for i in range(8):
            t = self.gstack.enter_context(self.nc.psum_tensor("bank%d" % i, [128, 512], F32))
            self.banks.append((t, Res("ps:bank%d" % i)))

    def bank(self):
        b = self.banks[self.bank_i % self.bank_mod]
        self.bank_i += 1
        return b

    def begin_phase(self):
        self.phase_id = getattr(self, "phase_id", 0) + 1
        self.pstack = contextlib.ExitStack()
        self.ops = {e: [] for e in ENGS}
        self.phase_dsems = []

    def end_phase(self, final=False):
        self.emit(final)
        self.pstack.close()
        self.pstack = None
        for e in ENGS:
            for k, v in self.semval.items():
                self.waited[e][k] = v
        self.free_dsems.extend(self.phase_dsems)
        self.phase_dsems = []

    def _waits(self, eng, reads, writes, mysem, acc):
        evs = []
        for r in reads:
            if r.w is not None:
                evs.append(r.w)
        for w in writes:
            if w.w is not None and not (acc and w.w[0] == mysem):
                evs.append(w.w)
            for ev in w.r:
                evs.append(ev)
        wd = self.waited[eng]
        best = {}
        for (k, v) in evs:
            if wd.get(k, 0) >= v:
                continue
            best[k] = max(best.get(k, 0), v)
        for k, v in best.items():
            wd[k] = v
        return list(best.items())

    def op(self, eng, fn, reads=(), writes=(), acc=False):
        if self.rec is not None:
            self.rec.append((self.op, (eng, fn, reads, writes, acc)))
            return None
        key = "P_" + eng
        waits = self._waits(eng, reads, writes, key, acc)
        self.semval[key] += 1
        ev = (key, self.semval[key])
        self.ops[eng].append((fn, waits, (key, 1)))
        self.nops += 1
        for r in reads:
            r.r.append(ev)
        for w in writes:
            w.w = ev
            w.r = []
        return ev

    def dma(self, eng, fn, reads=(), writes=(), sem=None):
        if self.rec is not None:
            self.rec.append((self.dma, (eng, fn, reads, writes, sem)))
            return None
        if sem is None:
            if writes and writes[0].name.startswith("sb:"):
                t = writes[0]
                if t.dsem_in is None:
                    t.dsem_in = self.dsem()
                sem = t.dsem_in
            else:
                t = reads[0]
                if t.dsem_out is None:
                    t.dsem_out = self.dsem()
                sem = t.dsem_out
        waits = self._waits(eng, reads, writes, sem, True)
        self.semval[sem] += 16
        ev = (sem, self.semval[sem])
        self.ops[eng].append((fn, waits, (sem, 16)))
        self.nops += 1
        for r in reads:
            r.r.append(ev)
        for w in writes:
            w.w = ev
            w.r = []
        return ev

    def emit(self, final=False):
        nc = self.nc
        sems = self.sems
        fin = [(k, v) for k, v in self.semval.items() if v > 0]
        with nc.Block() as block:
            for e in ENGS:
                ops = self.ops[e]

                def body(eh, ops=ops, e=e):
                    for fn, waits, inc in ops:
                        for k, v in waits:
                            eh.wait_ge(sems[k], v)
                        fn(eh).then_inc(sems[inc[0]], inc[1])
                    if e == "sp":
                        for k, v in fin:
                            eh.wait_ge(sems[k], v)

                getattr(block, HANDLES[e])(body)

    def close(self):
        self.gstack.close()


class Builder:
    def __init__(self, nb=2, depth=DEPTH, dbg=None):
        self.nb = nb
        self.depth = depth
        self.R = nb + 1
        self.dbg = dbg or {}
        nc = self.nc = bass.Bass("TRN2", target_bir_lowering=False)
        self.P = Prog(nc)
        dt = nc.dram_tensor
        A = {}

        def inp(name, shape, dtype=F32):
            A[name] = dt(name, list(shape), dtype, kind="ExternalInput").ap()

        inp("x", [nb, SEQ, D]); inp("c", [nb, D]); inp("ctx", [nb, CTX, D]); inp("c_ctx", [1, D])
        inp("w_mod", [DEPTH, D, 6 * D]); inp("b_mod", [DEPTH, 6 * D])
        inp("ln_g", [DEPTH, 2, D]); inp("ln_b", [DEPTH, 2, D])
        inp("a_w_in", [2, D, 2 * D]); inp("a_norm_g", [2, D]); inp("a_norm_b", [2, D])
        inp("a_w_s", [2, 8, 128, 128]); inp("a_b_s", [2, 8, 128]); inp("a_w_out", [2, D, D])
        inp("b_w_in", [2, D, 3104]); inp("b_w_gate", [2, 2, 16, 512]); inp("b_gate_bias", [2, 2, 512])
        inp("b_gn_g", [2, D]); inp("b_w_out", [2, D, D])
        inp("p_w_q", [DEPTH, D, D]); inp("p_keys", [DEPTH, 2, 128, 64])
        inp("p_u", [DEPTH * NEXP, D]); inp("p_v", [DEPTH * NEXP, D])
        A["y"] = dt("y", [nb, SEQ, D], F32, kind="ExternalOutput").ap()
        skind = "ExternalOutput" if self.dbg else "Internal"
        A["X0"] = dt("X0", [nb, CTX + SEQ, D], F32, kind=skind).ap()
        A["X1"] = dt("X1", [nb, CTX + SEQ, D], F32, kind=skind).ap()
        A["OF"] = dt("OF", [nb, CTX + SEQ, D], F32, kind="Internal").ap()
        A["TAB"] = dt("TAB", [DEPTH * NEXP, 2 * D], BF16, kind="Internal").ap()
        self.rTAB = Res("dr:TAB")
        self.A = A
        self.rX0 = Res("dr:X0"); self.rX1 = Res("dr:X1"); self.rOF = Res("dr:OF"); self.rY = Res("dr:y")
        self.rIN = Res("dr:in")

    def src_ap(self, layer, b, s):
        if layer == 0:
            if s < TCTX:
                return self.A["ctx"][b, s * 128:(s + 1) * 128, :], self.rIN
            return self.A["x"][b, (s - TCTX) * 128:(s - TCTX + 1) * 128, :], self.rIN
        return self.A["X0"][b, s * 128:(s + 1) * 128, :], self.rX0

    def row_of(self, b, s):
        return self.nb if s < TCTX else b

    def consts(self):
        P = self.P
        nb, R = self.nb, self.R
        self.ident, self.r_ident = P.gsbuf("ident", [128, 128], F32)
        self.tri = {}
        for nm in ("IF", "EF", "IB", "EB"):
            self.tri[nm] = P.gsbuf("tri" + nm, [128, 128], F32)
        self.maskF, self.r_maskF = P.gsbuf("maskF", [128, 4, 128], F32)
        self.maskB, self.r_maskB = P.gsbuf("maskB", [128, 4, 128], F32)
        self.ones1, self.r_ones1 = P.gsbuf("ones1", [1, 128], F32)
        self.iota16, self.r_iota16 = P.gsbuf("iota16", [128, 16], F32)
        self.sg, self.r_sg = P.gsbuf("sg", [128, R, 8], F32)
        self.thr17, self.r_thr17 = P.gsbuf("thr17", [128, 17], F32)
        self.modt, self.r_modt = P.gsbuf("modt", [128, R, 3, D], F32)
        self.lng, self.r_lng = P.gsbuf("lng", [128, D], F32)
        self.lnb, self.r_lnb = P.gsbuf("lnb", [128, D], F32)
        P.init_banks()

        P.begin_phase()
        cT, r_cT = P.sbuf("cT", [128, R, 8], F32)
        sg, r_sg = self.sg, self.r_sg
        tmpc, r_tmpc = P.sbuf("tmpc", [128, 4, 128], F32)
        ident = self.ident
        P.op("pool", lambda e: e.memset(ident[:], 0.0), writes=[self.r_ident])
        P.op("pool", lambda e: e.affine_select(out=ident[:], in_=ident[:], pattern=[[-1, 128]], compare_op=ALU.not_equal,
                                               fill=1.0, base=0, channel_multiplier=1),
             reads=[self.r_ident], writes=[self.r_ident])
        P.op("pool", lambda e: e.memset(tmpc[:], -1.0 / 16.0), writes=[r_tmpc])
        spec = {"IF": ([[1, 128]], -1, ALU.is_ge),
                "EF": ([[-1, 128]], 1, ALU.is_gt),
                "IB": ([[-1, 128]], 1, ALU.is_ge),
                "EB": ([[1, 128]], -1, ALU.is_gt)}
        for nm, (pat, cm, cmp) in spec.items():
            t, r = self.tri[nm]
            P.op("pool", lambda e, t=t, pat=pat, cm=cm, cmp=cmp: e.affine_select(
                out=t[:], in_=tmpc[:, 0, :], pattern=pat, compare_op=cmp, fill=0.0, base=0, channel_multiplier=cm),
                reads=[r_tmpc], writes=[r])
        ones4, r_ones4 = P.sbuf("ones4", [128, 4, 128], F32)
        P.op("pool", lambda e: e.memset(ones4[:], 1.0), writes=[r_ones4])
        mF, mB = self.maskF, self.maskB
        P.op("pool", lambda e: e.affine_select(out=mF[:], in_=ones4[:], pattern=[[0, 4], [1, 128]], compare_op=ALU.is_ge,
                                               fill=0.0, base=0, channel_multiplier=-1), reads=[r_ones4], writes=[self.r_maskF])
        P.op("pool", lambda e: e.affine_select(out=mB[:], in_=ones4[:], pattern=[[0, 4], [-1, 128]], compare_op=ALU.is_ge,
                                               fill=0.0, base=0, channel_multiplier=1), reads=[r_ones4], writes=[self.r_maskB])
        o1 = self.ones1
        P.op("pool", lambda e: e.memset(o1[:], 1.0), writes=[self.r_ones1])
        io = self.iota16
        P.op("pool", lambda e: e.iota(io[:], pattern=[[1, 16]], base=0, channel_multiplier=0,
                                      allow_small_or_imprecise_dtypes=True), writes=[self.r_iota16])
        for r in range(R):
            src = self.A["c"][r, :] if r < nb else self.A["c_ctx"][0, :]
            P.dma("sp", lambda e, r=r, src=src: e.dma_start(out=cT[:, r, :], in_=src.rearrange("(kc k) -> k kc", k=128),
                                                            allow_slow_non_contiguous=True),
                  reads=[self.rIN], writes=[r_cT])
        P.op("act", lambda e: e.activation(out=sg[:], in_=cT[:], func=AF.Sigmoid), reads=[r_cT], writes=[r_sg])
        P.op("dve", lambda e: e.tensor_tensor(out=sg[:], in0=sg[:], in1=cT[:], op=ALU.mult), reads=[r_sg, r_cT], writes=[r_sg])
        th = self.thr17
        P.op("pool", lambda e: e.iota(th[:], pattern=[[16, 17]], base=0, channel_multiplier=0,
                                      allow_small_or_imprecise_dtypes=True), writes=[self.r_thr17])
        P.end_phase()

    def table_phase(self):
        P = self.P
        A = self.A
        RR = 4
        P.begin_phase()
        uin = [P.sbuf("tu%d" % k, [128, RR, D], F32) for k in range(2)]
        vin = [P.sbuf("tv%d" % k, [128, RR, D], F32) for k in range(2)]
        tout = [P.sbuf("tt%d" % k, [128, RR, 2, D], BF16) for k in range(2)]
        layers = sorted(set(self.dbg.get("layers", range(self.depth))))
        if self.dbg.get("all_tabs"):
            layers = list(range(DEPTH))
        nchunk = NEXP // (128 * RR)
        it = 0
        for layer in layers:
            for g in range(nchunk):
                r0 = layer * NEXP + g * 128 * RR
                u, r_u = uin[it % 2]
                v, r_v = vin[it % 2]
                t, r_t = tout[it % 2]
                it += 1
                P.dma("sp", lambda e, u=u, r0=r0: e.dma_start(out=u[:], in_=A["p_u"][r0:r0 + 128 * RR, :].rearrange("(p r) d -> p r d", r=RR)),
                      reads=[self.rIN], writes=[r_u])
                P.dma("sp", lambda e, v=v, r0=r0: e.dma_start(out=v[:], in_=A["p_v"][r0:r0 + 128 * RR, :].rearrange("(p r) d -> p r d", r=RR)),
                      reads=[self.rIN], writes=[r_v])
                P.op("dve", lambda e, u=u, t=t: e.tensor_copy(out=t[:, :, 0, :], in_=u[:]), reads=[r_u], writes=[r_t])
                P.op("act", lambda e, v=v, t=t: e.copy(out=t[:, :, 1, :], in_=v[:]), reads=[r_v], writes=[r_t])
                P.dma("sp", lambda e, t=t, r0=r0: e.dma_start(
                    out=A["TAB"][r0:r0 + 128 * RR, :].rearrange("(p r) (two d) -> p r two d", r=RR, two=2), in_=t[:]),
                    reads=[r_t], writes=[Res("dr:tabchunk")])
        P.end_phase()

    def mod_phase(self, layer, half):
        P = self.P
        R = self.R
        P.begin_phase()
        bm, r_bm = P.sbuf("bm", [1, 3 * D], F32)
        scr, r_screp = P.sbuf("screp", [128, R, 8, 128], F32)
        sg = self.sg
        for r in range(R):
            P.op("dve", lambda e, r=r: e.tensor_copy(out=scr[:, r, :, :],
                                                     in_=sg[:, r, :].unsqueeze(2).to_broadcast([128, 8, 128])),
                 reads=[self.r_sg], writes=[r_screp])
        wst = [P.sbuf("wst%d" % k, [128, 8, 512], F32) for k in range(2)]
        P.dma("sp", lambda e: e.dma_start(out=bm[:], in_=self.A["b_mod"][layer:layer + 1, half * 3 * D:(half + 1) * 3 * D]),
              reads=[self.rIN], writes=[r_bm])
        lng, lnb = self.lng, self.lnb
        P.dma("sp", lambda e: e.dma_start(out=lng[:], in_=self.A["ln_g"][layer, half, :].partition_broadcast(128)),
              reads=[self.rIN], writes=[self.r_lng])
        P.dma("sp", lambda e: e.dma_start(out=lnb[:], in_=self.A["ln_b"][layer, half, :].partition_broadcast(128)),
              reads=[self.rIN], writes=[self.r_lnb])
        modt, ones1 = self.modt, self.ones1
        it = 0
        for jj in range(3):
            for nn in range(2):
                c0 = (half * 3 + jj) * D + nn * 512
                w, r_w = wst[it % 2]
                it += 1
                P.dma("sp", lambda e, w=w, c0=c0: e.dma_start(
                    out=w[:], in_=self.A["w_mod"][layer, :, c0:c0 + 512].rearrange("(kc p) n -> p kc n", p=128)),
                    reads=[self.rIN], writes=[r_w])
                for r in range(R):
                    bk, r_bk = P.bank()
                    for kc in range(8):
                        P.op("pe", lambda e, bk=bk, r=r, kc=kc, w=w: e.matmul(bk[:], lhsT=scr[:, r, kc, :], rhs=w[:, kc, :],
                                                                             start=(kc == 0), stop=False),
                             reads=[r_screp, r_w], writes=[r_bk], acc=(kc > 0))
                    P.op("pe", lambda e, bk=bk, jj=jj, nn=nn: e.matmul(bk[:], lhsT=ones1[0:1, :],
                                                                       rhs=bm[0:1, jj * D + nn * 512: jj * D + nn * 512 + 512],
                                                                       start=False, stop=True),
                         reads=[self.r_ones1, r_bm], writes=[r_bk], acc=True)
                    addc = 1.0 if jj == 1 else 0.0
                    P.op("dve", lambda e, bk=bk, r=r, jj=jj, nn=nn, addc=addc: e.tensor_scalar(
                        out=modt[:, r, jj, nn * 512:(nn + 1) * 512], in0=bk[:], scalar1=addc, scalar2=None, op0=ALU.add),
                        reads=[r_bk], writes=[self.r_modt])
        P.end_phase()

    def load_w_bf16(self, dst, r_dst, src, K, N, stg):
        P = self.P
        it = 0
        for kc in range(K // 128):
            SW = stg[0][0].shape[-1]
            for n0 in range(0, N, SW):
                n1 = min(N, n0 + SW)
                s, r_s = stg[it % len(stg)]
                eng = ("act", "dve")[it % 2]
                it += 1
                P.dma("sp", lambda e, s=s, kc=kc, n0=n0, n1=n1: e.dma_start(out=s[:, 0:n1 - n0],
                                                                            in_=src[kc * 128:(kc + 1) * 128, n0:n1]),
                      reads=[self.rIN], writes=[r_s])
                if eng == "act":
                    P.op("act", lambda e, s=s, kc=kc, n0=n0, n1=n1: e.copy(out=dst[:, kc, n0:n1], in_=s[:, 0:n1 - n0]),
                         reads=[r_s], writes=[r_dst])
                else:
                    P.op("dve", lambda e, s=s, kc=kc, n0=n0, n1=n1: e.tensor_copy(out=dst[:, kc, n0:n1], in_=s[:, 0:n1 - n0]),
                         reads=[r_s], writes=[r_dst])

    def bcast_load(self, dst, r_dst, vec):
        self.P.dma("sp", lambda e: e.dma_start(out=dst[:], in_=vec.partition_broadcast(128)), reads=[self.rIN], writes=[r_dst])

    def modulate(self, out, r_out, xt, r_xt, row):
        P = self.P
        modt = self.modt
        P.op("dve", lambda e: e.tensor_tensor(out=out[:], in0=xt[:], in1=modt[:, row, 1, :], op=ALU.mult),
             reads=[r_xt, self.r_modt], writes=[r_out])
        P.op("dve", lambda e: e.tensor_tensor(out=out[:], in0=out[:], in1=modt[:, row, 0, :], op=ALU.add),
             reads=[r_out, self.r_modt], writes=[r_out])

    def transpose_to(self, dst, r_dst, src, r_src, nchunks=8):
        P = self.P
        ident = self.ident
        for g in range(0, nchunks, 4):
            bk, r_bk = P.bank()
            n = min(4, nchunks - g)
            for k in range(n):
                kc = g + k
                P.op("pe", lambda e, bk=bk, k=k, kc=kc: e.transpose(out=bk[:, k * 128:(k + 1) * 128],
                                                                    in_=src[:, kc * 128:(kc + 1) * 128], identity=ident[:]),
                     reads=[r_src, self.r_ident], writes=[r_bk], acc=(k > 0))
            P.op("act", lambda e, bk=bk, g=g, n=n: e.copy(out=dst[:, g:g + n, :].rearrange("p a b -> p (a b)"),
                                                          in_=bk[:, 0:n * 128]),
                 reads=[r_bk], writes=[r_dst])

    def layer_norm(self, out, r_out, yin, r_yin, sm, gb=None):
        P = self.P
        st, r_st = sm["st"]
        mv, r_mv = sm["mv"]
        sd, r_sd = sm["sd"]
        for h2 in range(2):
            P.op("dve", lambda e, h2=h2: e.bn_stats(out=st[:, h2, :], in_=yin[:, h2 * 512:(h2 + 1) * 512]),
                 reads=[r_yin], writes=[r_st])
        P.op("dve", lambda e: e.bn_aggr(out=mv[:], in_=st[:].rearrange("p a b -> p (a b)")), reads=[r_st], writes=[r_mv])
        P.op("act", lambda e: e.activation(out=sd[:, 0:1], in_=mv[:, 1:2], func=AF.Sqrt, bias=sm["eps"][0][:, 0:1], scale=1.0),
             reads=[r_mv, sm["eps"][1]], writes=[r_sd])
        P.op("dve", lambda e: e.reciprocal(out=sd[:, 1:2], in_=sd[:, 0:1]), reads=[r_sd], writes=[r_sd])
        P.op("dve", lambda e: e.tensor_scalar(out=out[:], in0=yin[:], scalar1=mv[:, 0:1], scalar2=sd[:, 1:2],
                                              op0=ALU.subtract, op1=ALU.mult),
             reads=[r_yin, r_mv, r_sd], writes=[r_out])
        if gb is not None:
            (g, r_g), (b, r_b) = gb
            P.op("dve", lambda e: e.tensor_tensor(out=out[:], in0=out[:], in1=g[:], op=ALU.mult), reads=[r_out, r_g], writes=[r_out])
            P.op("dve", lambda e: e.tensor_tensor(out=out[:], in0=out[:], in1=b[:], op=ALU.add), reads=[r_out, r_b], writes=[r_out])

    def small_scratch(self):
        P = self.P
        sm = {"st": P.sbuf("ln_st", [128, 2, 6], F32), "mv": P.sbuf("ln_mv", [128, 2], F32), "sd": P.sbuf("ln_sd", [128, 2], F32),
              "eps": P.sbuf("ln_eps", [128, 1], F32)}
        ep = sm["eps"][0]
        P.op("pool", lambda e: e.memset(ep[:], EPS), writes=[sm["eps"][1]])
        return sm

    def residual_ln_store(self, mix_banks, xt, r_xt, row, sm, ytile, x1tile, dst_ap, r_dst):
        P = self.P
        modt = self.modt
        y, r_y = ytile
        x1, r_x1 = x1tile
        if isinstance(mix_banks, list):
            for n2, (bk, r_bk) in enumerate(mix_banks):
                P.op("dve", lambda e, bk=bk, n2=n2: e.tensor_tensor(out=y[:, n2 * 512:(n2 + 1) * 512], in0=bk[:],
                                                                    in1=modt[:, row, 2, n2 * 512:(n2 + 1) * 512], op=ALU.mult),
                     reads=[r_bk, self.r_modt], writes=[r_y])
        else:
            mt, r_mt = mix_banks
            P.op("dve", lambda e: e.tensor_tensor(out=y[:], in0=mt[:], in1=modt[:, row, 2, :], op=ALU.mult),
                 reads=[r_mt, self.r_modt], writes=[r_y])
        P.op("dve", lambda e: e.scalar_tensor_tensor(out=y[:], in0=xt[:], scalar=ALPHA, in1=y[:], op0=ALU.mult, op1=ALU.add),
             reads=[r_xt, r_y], writes=[r_y])
        self.layer_norm(x1, r_x1, y, r_y, sm, gb=((self.lng, self.r_lng), (self.lnb, self.r_lnb)))
        P.dma("sp", lambda e: e.dma_start(out=dst_ap, in_=x1[:]), reads=[r_x1], writes=[r_dst])

    def cmlp_phase(self, layer):
        P = self.P
        A = self.A
        j = layer // 2
        last = layer == DEPTH - 1
        P.begin_phase()
        sm = self.small_scratch()
        stg = [P.sbuf("stg%d" % k, [128, 2048], F32) for k in range(2)]
        w_in, r_w_in = P.sbuf("w_in", [128, 8, 2048], BF16)
        w_out, r_w_out = P.sbuf("w_out", [128, 8, D], BF16)
        wsT, r_wsT = P.sbuf("wsT", [128, 8, 128], BF16)
        wsraw, r_wsraw = P.sbuf("wsraw", [128, 8, 128], F32)
        bsb, r_bsb = P.sbuf("bsb", [128, 8, 128], F32)
        ng, r_ng = P.sbuf("ng", [128, D], F32)
        nbt, r_nbt = P.sbuf("nbt", [128, D], F32)
        self.load_w_bf16(w_in, r_w_in, A["a_w_in"][j], D, 2 * D, stg)
        self.load_w_bf16(w_out, r_w_out, A["a_w_out"][j], D, D, stg)
        self.bcast_load(ng, r_ng, A["a_norm_g"][j, :])
        self.bcast_load(nbt, r_nbt, A["a_norm_b"][j, :])
        P.dma("sp", lambda e: e.dma_start(out=bsb[:].rearrange("p a b -> p (a b)"),
                                          in_=A["a_b_s"][j].rearrange("h p -> (h p)").partition_broadcast(128)),
              reads=[self.rIN], writes=[r_bsb])
        P.dma("sp", lambda e: e.dma_start(out=wsraw[:], in_=A["a_w_s"][j].rearrange("h p q -> p h q")),
              reads=[self.rIN], writes=[r_wsraw])
        self.transpose_to(wsT, r_wsT, wsraw[:].rearrange("p a b -> p (a b)"), r_wsraw)

        xts = [P.sbuf("xt%d" % k, [128, D], F32) for k in range(2)]
        hf, r_hf = P.sbuf("hf", [128, D], F32)
        hT, r_hT = P.sbuf("hT", [128, 8, 128], BF16)
        uT, r_uT = P.sbuf("uT", [128, 8, 128], F32)
        vs, r_vs = P.sbuf("vs", [128, D], F32)
        vn, r_vn = P.sbuf("vn", [128, D], BF16)
        vnf, r_vnf = P.sbuf("vnf", [128, D], F32)
        usT, r_usT = P.sbuf("usT", [128, 8, 128], BF16)
        tmp, r_tmp = P.sbuf("tmpu", [128, 512], F32)
        ytile = P.sbuf("yt", [128, D], F32)
        x1tile = P.sbuf("x1t", [128, D], F32)

        tiles = [(b, s) for b in range(self.nb) for s in range(TPB) if not (last and s < TCTX)]
        tiles = tiles[:self.dbg.get("max_tiles", 10 ** 9)]
        for ti, (b, s) in enumerate(tiles):
            xt, r_xt = xts[ti % 2]
            src, r_src = self.src_ap(layer, b, s)
            row = self.row_of(b, s)
            P.dma("sp", lambda e, xt=xt, src=src: e.dma_start(out=xt[:], in_=src), reads=[r_src], writes=[r_xt])
            self.modulate(hf, r_hf, xt, r_xt, row)
            self.transpose_to(hT, r_hT, hf, r_hf)
            for g in range(2):
                bk, r_bk = P.bank()
                for k in range(4):
                    fc = g * 4 + k
                    for kc in range(8):
                        P.op("pe", lambda e, bk=bk, k=k, fc=fc, kc=kc: e.matmul(
                            bk[:, k * 128:(k + 1) * 128], lhsT=w_in[:, kc, fc * 128:(fc + 1) * 128], rhs=hT[:, kc, :],
                            start=(kc == 0), stop=(kc == 7)),
                            reads=[r_w_in, r_hT], writes=[r_bk], acc=not (k == 0 and kc == 0))
                P.op("act", lambda e, bk=bk, g=g: e.activation(out=uT[:, g * 4:(g + 1) * 4, :].rearrange("p a b -> p (a b)"),
                                                               in_=bk[:], func=AF.Gelu_apprx_tanh),
                     reads=[r_bk], writes=[r_uT])
            for n2 in range(2):
                bk, r_bk = P.bank()
                for kc in range(8):
                    P.op("pe", lambda e, bk=bk, n2=n2, kc=kc: e.matmul(
                        bk[:], lhsT=hT[:, kc, :], rhs=w_in[:, kc, D + n2 * 512: D + (n2 + 1) * 512],
                        start=(kc == 0), stop=(kc == 7)),
                        reads=[r_w_in, r_hT], writes=[r_bk], acc=(kc > 0))
                P.op("act", lambda e, bk=bk, n2=n2: e.activation(out=vs[:, n2 * 512:(n2 + 1) * 512], in_=bk[:],
                                                                 func=AF.Gelu_apprx_tanh),
                     reads=[r_bk], writes=[r_vs])
            self.layer_norm(vnf, r_vnf, vs, r_vs, sm, gb=((ng, r_ng), (nbt, r_nbt)))
            P.op("act", lambda e: e.copy(out=vn[:], in_=vnf[:]), reads=[r_vnf], writes=[r_vn])
            for g in range(2):
                bk, r_bk = P.bank()
                for k in range(4):
                    hd = g * 4 + k
                    P.op("pe", lambda e, bk=bk, k=k, hd=hd: e.matmul(bk[:, k * 128:(k + 1) * 128],
                                                                     lhsT=vn[:, hd * 128:(hd + 1) * 128], rhs=wsT[:, hd, :],
                                                                     start=True, stop=True),
                         reads=[r_vn, r_wsT], writes=[r_bk], acc=(k > 0))
                P.op("dve", lambda e, bk=bk, g=g: e.tensor_tensor(
                    out=tmp[:], in0=bk[:], in1=bsb[:, g * 4:(g + 1) * 4, :].rearrange("p a b -> p (a b)"), op=ALU.add),
                    reads=[r_bk, r_bsb], writes=[r_tmp])
                P.op("dve", lambda e, g=g: e.tensor_tensor(
                    out=usT[:, g * 4:(g + 1) * 4, :].rearrange("p a b -> p (a b)"), in0=tmp[:],
                    in1=uT[:, g * 4:(g + 1) * 4, :].rearrange("p a b -> p (a b)"), op=ALU.mult),
                    reads=[r_tmp, r_uT], writes=[r_usT])
            mixb = []
            for n2 in range(2):
                bk, r_bk = P.bank()
                for fc in range(8):
                    P.op("pe", lambda e, bk=bk, n2=n2, fc=fc: e.matmul(bk[:], lhsT=usT[:, fc, :],
                                                                       rhs=w_out[:, fc, n2 * 512:(n2 + 1) * 512],
                                                                       start=(fc == 0), stop=(fc == 7)),
                         reads=[r_usT, r_w_out], writes=[r_bk], acc=(fc > 0))
                mixb.append((bk, r_bk))
            self.residual_ln_store(mixb, xt, r_xt, row, sm, ytile, x1tile, A["X1"][b, s * 128:(s + 1) * 128, :], self.rX1)
        P.end_phase()

    def gla_phase(self, layer):
        P = self.P
        A = self.A
        j = layer // 2
        last = layer == DEPTH - 1
        P.begin_phase()
        sm = self.small_scratch()
        ofs = [P.sbuf("of%d" % k, [128, D], F32) for k in range(2)]
        stg = ofs
        w_in, r_w_in = P.sbuf("gw_in", [128, 8, 3104], BF16)
        w_out, r_w_out = P.sbuf("gw_out", [128, 8, D], BF16)
        wg, r_wg = P.sbuf("wg", [16, 2, 512], F32)
        gbias, r_gbias = P.sbuf("gbias", [1, 2, 512], F32)
        gng, r_gng = P.sbuf("gng", [128, D], F32)
        self.load_w_bf16(w_in, r_w_in, A["b_w_in"][j], D, 3104, stg)
        self.load_w_bf16(w_out, r_w_out, A["b_w_out"][j], D, D, stg)
        self.bcast_load(gng, r_gng, A["b_gn_g"][j, :])
        P.dma("sp", lambda e: e.dma_start(out=wg[:], in_=A["b_w_gate"][j].rearrange("d r n -> r d n")),
              reads=[self.rIN], writes=[r_wg])
        P.dma("sp", lambda e: e.dma_start(out=gbias[:], in_=A["b_gate_bias"][j:j + 1, :, :]), reads=[self.rIN], writes=[r_gbias])

        xts = [P.sbuf("xt%d" % k, [128, D], F32) for k in range(2)]
        hf, r_hf = P.sbuf("hf", [128, D], F32)
        hT, r_hT = P.sbuf("hT", [128, 8, 128], BF16)
        v_sb, r_v = P.sbuf("v_sb", [128, D], BF16)
        r_sb, r_r = P.sbuf("r_sb", [128, D], F32)
        glT, r_glT = P.sbuf("glT", [16, 128], F32)
        e1, r_e1 = P.sbuf("e1", [128, 512], F32)
        lsp, r_lsp = P.sbuf("lsp", [128, 512], F32)
        ebT, r_ebT = P.sbuf("ebT", [128, 4, 128], F32)
        enbT, r_enbT = P.sbuf("enbT", [128, 4, 128], F32)
        ebx, r_ebx = e1, r_e1
        qiT, r_qiT = P.sbuf("qiT", [128, 4, 128], BF16)
        kiT, r_kiT = P.sbuf("kiT", [128, 4, 128], BF16)
        kend, r_kend = P.sbuf("kend", [128, 512], BF16)
        attm, r_attm = P.sbuf("attm", [128, 4, 128], BF16)
        S, r_S = P.sbuf("S", [128, 4, 256], F32)
        Sb, r_Sb = P.sbuf("Sb", [128, 4, 256], BF16)
        osum, r_osum = P.sbuf("osum", [128, D], F32)
        gst, r_gst = P.sbuf("gst", [128, 4, 6], F32)
        gmv, r_gmv = P.sbuf("gmv", [128, 4, 2], F32)
        gsd, r_gsd = P.sbuf("gsd", [128, 4, 2], F32)
        yf, r_yf = P.sbuf("yf", [128, D], F32)
        yT, r_yT = P.sbuf("yT", [128, 8, 128], BF16)
        ytile = (osum, r_osum)
        x1tile = (yf, r_yf)
        eps = sm["eps"]

        def project_and_scan(ti, b, s, d):
            xt, r_xt = xts[ti % 2]
            src, r_src = self.src_ap(layer, b, s)
            row = self.row_of(b, s)
            P.dma("sp", lambda e: e.dma_start(out=xt[:], in_=src), reads=[r_src], writes=[r_xt])
            self.modulate(hf, r_hf, xt, r_xt, row)
            self.transpose_to(hT, r_hT, hf, r_hf)
            triI, r_triI = self.tri["IF" if d == 0 else "IB"]
            triE, r_triE = self.tri["EF" if d == 0 else "EB"]
            mask, r_mask = (self.maskF, self.r_maskF) if d == 0 else (self.maskB, self.r_maskB)
            endcol = 127 if d == 0 else 0

            def proj_fm(col0):
                bk, r_bk = P.bank()
                for h in range(4):
                    for kc in range(8):
                        P.op("pe", lambda e, h=h, kc=kc: e.matmul(
                            bk[:, h * 128:(h + 1) * 128], lhsT=w_in[:, kc, col0 + h * 128: col0 + (h + 1) * 128], rhs=hT[:, kc, :],
                            start=(kc == 0), stop=(kc == 7)),
                            reads=[r_w_in, r_hT], writes=[r_bk], acc=not (h == 0 and kc == 0))
                return bk, r_bk

            def proj_tm(col0):
                bk, r_bk = P.bank()
                for kc in range(8):
                    P.op("pe", lambda e, kc=kc: e.matmul(bk[:], lhsT=hT[:, kc, :], rhs=w_in[:, kc, col0:col0 + 512],
                                                         start=(kc == 0), stop=(kc == 7)),
                         reads=[r_w_in, r_hT], writes=[r_bk], acc=(kc > 0))
                return bk, r_bk

            bkg, r_bkg = P.bank()
            gcol = 3072 + 16 * d
            for kc in range(8):
                P.op("pe", lambda e, kc=kc: e.matmul(bkg[0:16, 0:128], lhsT=w_in[:, kc, gcol:gcol + 16], rhs=hT[:, kc, :],
                                                     start=(kc == 0), stop=(kc == 7)),
                     reads=[r_w_in, r_hT], writes=[r_bkg], acc=(kc > 0))
            P.op("act", lambda e: e.copy(out=glT[:], in_=bkg[0:16, 0:128]), reads=[r_bkg], writes=[r_glT])
            bkz, r_bkz = P.bank()
            P.op("pe", lambda e: e.matmul(bkz[:], lhsT=glT[:], rhs=wg[:, d, :], start=True, stop=False),
                 reads=[r_glT, r_wg], writes=[r_bkz])
            P.op("pe", lambda e: e.matmul(bkz[:], lhsT=self.ones1[0:1, :], rhs=gbias[0:1, d, :], start=False, stop=True),
                 reads=[self.r_ones1, r_gbias], writes=[r_bkz], acc=True)
            P.op("act", lambda e: e.activation(out=e1[:], in_=bkz[:], func=AF.Exp, scale=-1.0), reads=[r_bkz], writes=[r_e1])
            P.op("act", lambda e: e.activation(out=lsp[:], in_=e1[:], func=AF.Ln, bias=1.0, scale=1.0), reads=[r_e1], writes=[r_lsp])
            bkb, r_bkb = P.bank()
            for h in range(4):
                P.op("pe", lambda e, h=h: e.matmul(bkb[:, h * 128:(h + 1) * 128], lhsT=lsp[:, h * 128:(h + 1) * 128], rhs=triI[:],
                                                   start=True, stop=True),
                     reads=[r_lsp, r_triI], writes=[r_bkb], acc=(h > 0))
            bkx, r_bkx = P.bank()
            P.op("pe", lambda e: e.matmul(bkx[:], lhsT=triE[:], rhs=lsp[:], start=True, stop=True),
                 reads=[r_lsp, r_triE], writes=[r_bkx])
            P.op("act", lambda e: e.activation(out=ebT[:].rearrange("p a b -> p (a b)"), in_=bkb[:], func=AF.Exp),
                 reads=[r_bkb], writes=[r_ebT])
            P.op("act", lambda e: e.activation(out=enbT[:].rearrange("p a b -> p (a b)"), in_=bkb[:], func=AF.Exp, scale=-1.0),
                 reads=[r_bkb], writes=[r_enbT])
            P.op("act", lambda e: e.activation(out=ebx[:], in_=bkx[:], func=AF.Exp), reads=[r_bkx], writes=[r_ebx])
            bq, r_bq = proj_fm(0)
            P.op("dve", lambda e: e.scalar_tensor_tensor(out=qiT[:].rearrange("p a b -> p (a b)"), in0=bq[:], scalar=128.0 ** -0.5,
                                                         in1=ebT[:].rearrange("p a b -> p (a b)"), op0=ALU.mult, op1=ALU.mult),
                 reads=[r_bq, r_ebT], writes=[r_qiT])
            bkT, r_bkT = proj_fm(512)
            P.op("dve", lambda e: e.tensor_tensor(out=kiT[:].rearrange("p a b -> p (a b)"), in0=bkT[:],
                                                  in1=enbT[:].rearrange("p a b -> p (a b)"), op=ALU.mult),
                 reads=[r_bkT, r_enbT], writes=[r_kiT])
            bk_, r_bk_ = proj_tm(512)
            P.op("dve", lambda e: e.tensor_tensor(out=kend[:], in0=bk_[:], in1=ebx[:], op=ALU.mult),
                 reads=[r_bk_, r_ebx], writes=[r_kend])
            for n2 in range(2):
                bv, r_bv = proj_tm(1024 + n2 * 512)
                P.op("act", lambda e, n2=n2, bv=bv: e.copy(out=v_sb[:, n2 * 512:(n2 + 1) * 512], in_=bv[:]),
                     reads=[r_bv], writes=[r_v])
            if d == 1:
                for n2 in range(2):
                    br, r_br = proj_tm(2048 + n2 * 512)
                    P.op("act", lambda e, n2=n2, br=br: e.activation(out=r_sb[:, n2 * 512:(n2 + 1) * 512], in_=br[:], func=AF.Silu),
                         reads=[r_br], writes=[r_r])
            bka, r_bka = P.bank()
            for h in range(4):
                P.op("pe", lambda e, h=h: e.matmul(bka[:, h * 128:(h + 1) * 128], lhsT=kiT[:, h, :], rhs=qiT[:, h, :],
                                                   start=True, stop=True),
                     reads=[r_kiT, r_qiT], writes=[r_bka], acc=(h > 0))
            P.op("dve", lambda e: e.tensor_tensor(out=attm[:].rearrange("p a b -> p (a b)"), in0=bka[:],
                                                  in1=mask[:].rearrange("p a b -> p (a b)"), op=ALU.mult),
                 reads=[r_bka, r_mask], writes=[r_attm])
            obanks = []
            for g in range(2):
                bo, r_bo = P.bank()
                for k in range(2):
                    h = g * 2 + k
                    P.op("pe", lambda e, bo=bo, k=k, h=h: e.matmul(bo[:, k * 256:(k + 1) * 256], lhsT=attm[:, h, :],
                                                                   rhs=v_sb[:, h * 256:(h + 1) * 256], start=True, stop=False),
                         reads=[r_attm, r_v], writes=[r_bo], acc=(k > 0))
                    P.op("pe", lambda e, bo=bo, k=k, h=h: e.matmul(bo[:, k * 256:(k + 1) * 256], lhsT=qiT[:, h, :],
                                                                   rhs=Sb[:, h, :], start=False, stop=True),
                         reads=[r_qiT, r_Sb], writes=[r_bo], acc=True)
                obanks.append((bo, r_bo))
            for g in range(2):
                bs, r_bs = P.bank()
                for k in range(2):
                    h = g * 2 + k
                    P.op("pe", lambda e, bs=bs, k=k, h=h: e.matmul(bs[:, k * 256:(k + 1) * 256], lhsT=kend[:, h * 128:(h + 1) * 128],
                                                                   rhs=v_sb[:, h * 256:(h + 1) * 256], start=True, stop=True),
                         reads=[r_kend, r_v], writes=[r_bs], acc=(k > 0))
                for k in range(2):
                    h = g * 2 + k
                    P.op("dve", lambda e, bs=bs, k=k, h=h: e.scalar_tensor_tensor(
                        out=S[:, h, :], in0=S[:, h, :], scalar=ebT[:, h, endcol:endcol + 1], in1=bs[:, k * 256:(k + 1) * 256],
                        op0=ALU.mult, op1=ALU.add),
                        reads=[r_S, r_ebT, r_bs], writes=[r_S])
            P.op("act", lambda e: e.copy(out=Sb[:].rearrange("p a b -> p (a b)"), in_=S[:].rearrange("p a b -> p (a b)")),
                 reads=[r_S], writes=[r_Sb])
            return obanks, (xt, r_xt), row

        def reset_state():
            P.op("pool", lambda e: e.memset(S[:], 0.0), writes=[r_S])
            P.op("pool", lambda e: e.memset(Sb[:], 0.0), writes=[r_Sb])

        ti = 0
        for b in range(self.nb):
            reset_state()
            for s in range(TPB):
                obanks, _, _ = project_and_scan(ti, b, s, 0)
                of, r_of = ofs[ti % 2]
                for g, (bo, r_bo) in enumerate(obanks):
                    P.op("act", lambda e, bo=bo, g=g, of=of: e.copy(out=of[:, g * 512:(g + 1) * 512], in_=bo[:]),
                         reads=[r_bo], writes=[r_of])
                P.dma("sp", lambda e, of=of, b=b, s=s: e.dma_start(out=A["OF"][b, s * 128:(s + 1) * 128, :], in_=of[:]),
                      reads=[r_of], writes=[self.rOF])
                ti += 1
        for b in range(self.nb):
            reset_state()
            order = [1, 0] + list(range(TPB - 1, TCTX - 1, -1))
            for s in order:
                of, r_of = ofs[ti % 2]
                if not (last and s < TCTX):
                    P.dma("sp", lambda e, of=of, b=b, s=s: e.dma_start(out=of[:], in_=A["OF"][b, s * 128:(s + 1) * 128, :]),
                          reads=[self.rOF], writes=[r_of])
                obanks, (xt, r_xt), row = project_and_scan(ti, b, s, 1)
                ti += 1
                if last and s < TCTX:
                    continue
                for g, (bo, r_bo) in enumerate(obanks):
                    P.op("dve", lambda e, bo=bo, g=g, of=of: e.tensor_tensor(out=osum[:, g * 512:(g + 1) * 512], in0=bo[:],
                                                                            in1=of[:, g * 512:(g + 1) * 512], op=ALU.add),
                         reads=[r_bo, r_of], writes=[r_osum])
                for h in range(4):
                    P.op("dve", lambda e, h=h: e.bn_stats(out=gst[:, h, :], in_=osum[:, h * 256:(h + 1) * 256]),
                         reads=[r_osum], writes=[r_gst])
                for h in range(4):
                    P.op("dve", lambda e, h=h: e.bn_aggr(out=gmv[:, h, :], in_=gst[:, h, :]), reads=[r_gst], writes=[r_gmv])
                P.op("act", lambda e: e.activation(out=gsd[:, :, 0], in_=gmv[:, :, 1], func=AF.Sqrt, bias=eps[0][:, 0:1], scale=1.0),
                     reads=[r_gmv, eps[1]], writes=[r_gsd])
                P.op("dve", lambda e: e.reciprocal(out=gsd[:, :, 1], in_=gsd[:, :, 0]), reads=[r_gsd], writes=[r_gsd])
                for h in range(4):
                    P.op("dve", lambda e, h=h: e.tensor_scalar(out=yf[:, h * 256:(h + 1) * 256], in0=osum[:, h * 256:(h + 1) * 256],
                                                               scalar1=gmv[:, h, 0:1], scalar2=gsd[:, h, 1:2],
                                                               op0=ALU.subtract, op1=ALU.mult),
                         reads=[r_osum, r_gmv, r_gsd], writes=[r_yf])
                P.op("dve", lambda e: e.tensor_tensor(out=yf[:], in0=yf[:], in1=gng[:], op=ALU.mult), reads=[r_yf, r_gng], writes=[r_yf])
                P.op("dve", lambda e: e.tensor_tensor(out=yf[:], in0=yf[:], in1=r_sb[:], op=ALU.mult), reads=[r_yf, r_r], writes=[r_yf])
                self.transpose_to(yT, r_yT, yf, r_yf)
                mixb = []
                for n2 in range(2):
                    bk, r_bk = P.bank()
                    for fc in range(8):
                        P.op("pe", lambda e, bk=bk, n2=n2, fc=fc: e.matmul(bk[:], lhsT=yT[:, fc, :],
                                                                           rhs=w_out[:, fc, n2 * 512:(n2 + 1) * 512],
                                                                           start=(fc == 0), stop=(fc == 7)),
                             reads=[r_yT, r_w_out], writes=[r_bk], acc=(fc > 0))
                    mixb.append((bk, r_bk))
                self.residual_ln_store(mixb, xt, r_xt, row, sm, ytile, x1tile, A["X1"][b, s * 128:(s + 1) * 128, :], self.rX1)
        P.end_phase()

    def peer_phase(self, layer, final_out):
        P = self.P
        A = self.A
        last = layer == DEPTH - 1
        P.begin_phase()
        sm = self.small_scratch()
        wq, r_wq = P.sbuf("wq", [128, 8, D], F32)
        kbd, r_kbd = P.sbuf("kbd", [128, 256], F32)
        kraw, r_kraw = P.sbuf("kraw", [128, 2, 64], F32)
        P.dma("sp", lambda e: e.dma_start(out=wq[:], in_=A["p_w_q"][layer].rearrange("(kc p) n -> p kc n", p=128)),
              reads=[self.rIN], writes=[r_wq])
        P.dma("sp", lambda e: e.dma_start(out=kraw[:], in_=A["p_keys"][layer].rearrange("p k d -> k p d")),
              reads=[self.rIN], writes=[r_kraw])
        P.op("pool", lambda e: e.memset(kbd[:], 0.0), writes=[r_kbd])
        bkk, r_bkk = P.bank()
        P.op("pe", lambda e: e.transpose(out=bkk[:, 0:128], in_=kraw[:].rearrange("p a b -> p (a b)"), identity=self.ident[:]),
             reads=[r_kraw, self.r_ident], writes=[r_bkk])
        P.op("dve", lambda e: e.tensor_copy(out=kbd[0:64, 0:128], in_=bkk[0:64, 0:128]), reads=[r_bkk], writes=[r_kbd])
        P.op("dve", lambda e: e.tensor_copy(out=kbd[64:128, 128:256], in_=bkk[64:128, 0:128]), reads=[r_bkk], writes=[r_kbd])

        xts = [P.sbuf("xt%d" % k, [128, D], F32) for k in range(2)]
        h2s = [P.sbuf("h2_%d" % k, [128, D], F32) for k in range(2)]
        h2T, r_h2T = P.sbuf("h2T", [128, 8, 128], F32)
        qT, r_qT = P.sbuf("qT", [128, 8, 128], F32)
        sc, r_sc = P.sbuf("sc", [128, 16, 128], F32)
        work, r_work = P.sbuf("work", [128, 256], F32)
        s12, r_s12 = P.sbuf("s12", [128, 16, 16], F32)
        i12, r_i12 = P.sbuf("i12", [128, 16, 16], U32)
        i12f, r_i12f = P.sbuf("i12f", [128, 16, 16], F32)
        cand, r_cand = P.sbuf("cand", [128, 8, 256], F32)
        tops, r_tops = P.sbuf("tops", [128, 8, 16], F32)
        pos, r_pos = P.sbuf("pos", [128, 8, 16], U32)
        posf, r_posf = P.sbuf("posf", [128, 128], F32)
        pjf, r_pjf = P.sbuf("pjf", [128, 128], F32)
        pkf, r_pkf = P.sbuf("pkf", [128, 128], F32)
        ge, r_ge = P.sbuf("ge", [128, 128, 17], F32)
        oh, r_oh = cand[:].rearrange("p h (a b) -> p h a b", a=16), r_cand
        sel1, r_sel1 = P.sbuf("sel1", [128, 8, 16], F32)
        sel2, r_sel2 = P.sbuf("sel2", [128, 8, 16], F32)
        eidf, r_eidf = P.sbuf("eidf", [128, 128], F32)
        eids = [P.sbuf("eid%d" % k, [128, 128], I32) for k in range(2)]
        ex, r_ex = P.sbuf("ex", [128, 8, 16], F32)
        esum, r_esum = P.sbuf("esum", [128, 8], F32)
        wtss = [P.sbuf("wts%d" % k, [128, 128], F32) for k in range(2)]
        dots, r_dots = P.sbuf("dots", [128, 128], F32)
        actw, r_actw = P.sbuf("actw", [128, 128], F32)
        acc, r_acc = P.sbuf("acc", [128, D], F32)
        ring = [P.sbuf("ring%d" % k, [128, GSL, 2 * D], BF16) for k in range(NRING)]
        for k in range(NRING):
            ring[k][1].dsem_in = P.swsem(k)
        ytile = (acc, r_acc)
        x1tile = P.sbuf("x1t", [128, D], F32)
        ring_i = [0]
        r_s12g = [Res("sb:s12g%d" % k) for k in range(2)]
        r_i12g = [Res("sb:i12g%d" % k) for k in range(2)]
        r_workg = [Res("sb:workg%d" % k) for k in range(2)]
        r_topsg = [Res("sb:topsg%d" % k) for k in range(2)]
        r_posg = [Res("sb:posg%d" % k) for k in range(2)]
        work2, _ = P.sbuf("work2", [128, 256], F32)
        work3, _ = P.sbuf("work3", [128, 256], F32)
        r_workh = [Res("sb:workh%d" % k) for k in range(2)]
        junks = [P.sbuf("junkb%d" % k, [128, D], BF16) for k in range(2)]
        r_dots_s = [[Res("sb:dots%d_%d" % (k, q)) for q in range(GSL)] for k in range(4)]
        r_dots_g = [Res("sb:dotsg%d" % k) for k in range(4)]
        r_actw_g = [Res("sb:actwg%d" % k) for k in range(4)]
        P.bank_mod = 6
        accb = [P.banks[6], P.banks[7]]
        dgs = [P.sbuf("dg%d" % k, [128, 128], BF16) for k in range(4)]
        dfs = [P.sbuf("df%d" % k, [128, 128], F32) for k in range(4)]
        ident = self.ident

        tiles = [(b, s) for b in range(self.nb) for s in range(TPB) if not (last and s < TCTX)]
        tiles = tiles[:self.dbg.get("max_tiles", 10 ** 9)]
        tab = A["TAB"]
        def stage1(ti):
            b, s = tiles[ti]
            xt, r_xt = xts[ti % 2]
            eid, r_eid = eids[ti % 2]
            h2, r_h2 = h2s[ti % 2]
            wts, r_wts = wtss[ti % 2]
            row = self.row_of(b, s)
            P.dma("sp", lambda e, xt=xt, b=b, s=s: e.dma_start(out=xt[:], in_=A["X1"][b, s * 128:(s + 1) * 128, :]),
                  reads=[self.rX1], writes=[r_xt])
            self.modulate(h2, r_h2, xt, r_xt, row)
            self.transpose_to(h2T, r_h2T, h2, r_h2)
            for g in range(2):
                bk, r_bk = P.bank()
                for k in range(4):
                    hd = g * 4 + k
                    for kc in range(8):
                        P.op("pe", lambda e, bk=bk, k=k, hd=hd, kc=kc: e.matmul(
                            bk[:, k * 128:(k + 1) * 128], lhsT=wq[:, kc, hd * 128:(hd + 1) * 128], rhs=h2T[:, kc, :],
                            start=(kc == 0), stop=(kc == 7)),
                            reads=[r_wq, r_h2T], writes=[r_bk], acc=not (k == 0 and kc == 0))
                P.op("act", lambda e, bk=bk, g=g: e.copy(out=qT[:, g * 4:(g + 1) * 4, :].rearrange("p a b -> p (a b)"), in_=bk[:]),
                     reads=[r_bk], writes=[r_qT])
            for g in range(4):
                bk, r_bk = P.bank()
                for k in range(2):
                    hd = g * 2 + k
                    P.op("pe", lambda e, bk=bk, k=k, hd=hd: e.matmul(bk[:, k * 256:(k + 1) * 256], lhsT=qT[:, hd, :], rhs=kbd[:],
                                                                     start=True, stop=True),
                         reads=[r_qT, r_kbd], writes=[r_bk], acc=(k > 0))
                P.op("act", lambda e, bk=bk, g=g: e.copy(out=sc[:, g * 4:(g + 1) * 4, :].rearrange("p a b -> p (a b)"), in_=bk[:]),
                     reads=[r_bk], writes=[r_sc])
            for g0 in range(0, 16, 2):
                gs = (g0, g0 + 1)
                for k, g in enumerate(gs):
                    P.op("dve", lambda e, g=g: e.max(out=s12[:, g, 0:8], in_=sc[:, g, :]), reads=[r_sc], writes=[r_s12g[k]])
                for k, g in enumerate(gs):
                    P.op("dve", lambda e, g=g: e.max_index(out=i12[:, g, 0:8], in_max=s12[:, g, 0:8], in_values=sc[:, g, :]),
                         reads=[r_sc, r_s12g[k]], writes=[r_i12g[k]])
                for k, g in enumerate(gs):
                    P.op("dve", lambda e, g=g, k=k: e.match_replace(out=work[:, k * 128:(k + 1) * 128], in_to_replace=s12[:, g, 0:8],
                                                                    in_values=sc[:, g, :], imm_value=-1e30),
                         reads=[r_sc, r_s12g[k]], writes=[r_workg[k]])
                for k, g in enumerate(gs):
                    P.op("dve", lambda e, g=g, k=k: e.max(out=s12[:, g, 8:16], in_=work[:, k * 128:(k + 1) * 128]),
                         reads=[r_workg[k]], writes=[r_s12g[k]])
                for k, g in enumerate(gs):
                    P.op("dve", lambda e, g=g, k=k: e.max_index(out=i12[:, g, 8:16], in_max=s12[:, g, 8:16],
                                                                in_values=work[:, k * 128:(k + 1) * 128]),
                         reads=[r_workg[k], r_s12g[k]], writes=[r_i12g[k]])
            P.op("dve", lambda e: e.tensor_copy(out=i12f[:], in_=i12[:]), reads=r_i12g, writes=[r_i12f])
            s12v = s12[:].rearrange("p (h two) n -> p h two n", two=2)
            P.op("dve", lambda e: e.tensor_tensor(out=cand[:].rearrange("p h (a b) -> p h a b", a=16),
                                                  in0=s12v[:, :, 0, :].unsqueeze(3).to_broadcast([128, 8, 16, 16]),
                                                  in1=s12v[:, :, 1, :].unsqueeze(2).to_broadcast([128, 8, 16, 16]), op=ALU.add),
                 reads=r_s12g, writes=[r_cand])
            wk = (work2, work3)
            for h0 in range(0, 8, 2):
                hs = (h0, h0 + 1)
                for k, hd in enumerate(hs):
                    P.op("dve", lambda e, hd=hd: e.max(out=tops[:, hd, 0:8], in_=cand[:, hd, :]), reads=[r_cand], writes=[r_topsg[k]])
                for k, hd in enumerate(hs):
                    P.op("dve", lambda e, hd=hd: e.max_index(out=pos[:, hd, 0:8], in_max=tops[:, hd, 0:8], in_values=cand[:, hd, :]),
                         reads=[r_cand, r_topsg[k]], writes=[r_posg[k]])
                for k, hd in enumerate(hs):
                    P.op("dve", lambda e, hd=hd, k=k: e.match_replace(out=wk[k][:], in_to_replace=tops[:, hd, 0:8], in_values=cand[:, hd, :],
                                                                      imm_value=-1e30), reads=[r_cand, r_topsg[k]], writes=[r_workh[k]])
                for k, hd in enumerate(hs):
                    P.op("dve", lambda e, hd=hd, k=k: e.max(out=tops[:, hd, 8:16], in_=wk[k][:]), reads=[r_workh[k]], writes=[r_topsg[k]])
                for k, hd in enumerate(hs):
                    P.op("dve", lambda e, hd=hd, k=k: e.max_index(out=pos[:, hd, 8:16], in_max=tops[:, hd, 8:16], in_values=wk[k][:]),
                         reads=[r_workh[k], r_topsg[k]], writes=[r_posg[k]])
            r_tops_all = r_topsg
            r_pos_all = r_posg
            P.op("dve", lambda e: e.tensor_tensor(out=ex[:], in0=tops[:], in1=tops[:, :, 0:1].to_broadcast([128, 8, 16]),
                                                  op=ALU.subtract), reads=r_tops_all, writes=[r_ex])
            P.op("act", lambda e: e.activation(out=ex[:], in_=ex[:], func=AF.Exp), reads=[r_ex], writes=[r_ex])
            P.op("dve", lambda e: e.tensor_reduce(out=esum[:], in_=ex[:], axis=AX.X, op=ALU.add), reads=[r_ex], writes=[r_esum])
            P.op("dve", lambda e: e.reciprocal(out=esum[:], in_=esum[:]), reads=[r_esum], writes=[r_esum])
            P.op("dve", lambda e: e.tensor_tensor(out=wts[:].rearrange("p (h n) -> p h n", h=8), in0=ex[:],
                                                  in1=esum[:].unsqueeze(2).to_broadcast([128, 8, 16]), op=ALU.mult),
                 reads=[r_ex, r_esum], writes=[r_wts])
            P.op("dve", lambda e: e.tensor_copy(out=posf[:], in_=pos[:].rearrange("p h n -> p (h n)")), reads=r_pos_all, writes=[r_posf])
            th = self.thr17
            iot = self.iota16
            i12v = i12f[:].rearrange("p (h two) n -> p h two n", two=2)
            P.op("dve", lambda e: e.tensor_tensor(out=ge[:], in0=posf[:].unsqueeze(2).to_broadcast([128, 128, 17]),
                                                  in1=th[:].unsqueeze(1).to_broadcast([128, 128, 17]), op=ALU.is_ge),
                 reads=[r_posf, self.r_thr17], writes=[r_ge])
            P.op("dve", lambda e: e.tensor_reduce(out=pjf[:], in_=ge[:, :, 1:17], axis=AX.X, op=ALU.add), reads=[r_ge], writes=[r_pjf])
            P.op("dve", lambda e: e.scalar_tensor_tensor(out=pkf[:], in0=pjf[:], scalar=-16.0, in1=posf[:], op0=ALU.mult, op1=ALU.add),
                 reads=[r_pjf, r_posf], writes=[r_pkf])
            ohf = oh.rearrange("p h n j -> p (h n) j")
            P.op("dve", lambda e: e.tensor_tensor(out=ohf, in0=ge[:, :, 0:16], in1=ge[:, :, 1:17], op=ALU.subtract),
                 reads=[r_ge], writes=[r_oh])
            P.op("dve", lambda e: e.tensor_tensor(out=oh, in0=oh, in1=i12v[:, :, 0, :].unsqueeze(2).to_broadcast([128, 8, 16, 16]),
                                                  op=ALU.mult), reads=[r_oh, r_i12f], writes=[r_oh])
            P.op("dve", lambda e: e.tensor_reduce(out=sel1[:].rearrange("p h n -> p (h n)"), in_=ohf, axis=AX.X, op=ALU.add),
                 reads=[r_oh], writes=[r_sel1])
            P.op("dve", lambda e: e.tensor_tensor(out=ohf, in0=pkf[:].unsqueeze(2).to_broadcast([128, 128, 16]),
                                                  in1=iot[:].unsqueeze(1).to_broadcast([128, 128, 16]), op=ALU.is_equal),
                 reads=[r_pkf, self.r_iota16], writes=[r_oh])
            P.op("dve", lambda e: e.tensor_tensor(out=oh, in0=oh, in1=i12v[:, :, 1, :].unsqueeze(2).to_broadcast([128, 8, 16, 16]),
                                                  op=ALU.mult), reads=[r_oh, r_i12f], writes=[r_oh])
            P.op("dve", lambda e: e.tensor_reduce(out=sel2[:].rearrange("p h n -> p (h n)"), in_=ohf, axis=AX.X, op=ALU.add),
                 reads=[r_oh], writes=[r_sel2])
            P.op("dve", lambda e: e.scalar_tensor_tensor(out=eidf[:], in0=sel1[:].rearrange("p h n -> p (h n)"), scalar=128.0,
                                                         in1=sel2[:].rearrange("p h n -> p (h n)"), op0=ALU.mult, op1=ALU.add),
                 reads=[r_sel1, r_sel2], writes=[r_eidf])
            if layer > 0:
                P.op("dve", lambda e: e.tensor_scalar(out=eidf[:], in0=eidf[:], scalar1=float(layer * NEXP), scalar2=None, op0=ALU.add),
                     reads=[r_eidf], writes=[r_eidf])
            P.op("dve", lambda e, eid=eid: e.tensor_copy(out=eid[:], in_=eidf[:]), reads=[r_eidf], writes=[r_eid])

        stage1(0)
        for ti, (b, s) in enumerate(tiles):
            xt, r_xt = xts[ti % 2]
            eid, r_eid = eids[ti % 2]
            h2, r_h2 = h2s[ti % 2]
            wts, r_wts = wtss[ti % 2]
            row = self.row_of(b, s)
            pend = []
            if ti + 1 < len(tiles):
                P.rec = []
                stage1(ti + 1)
                pend, P.rec = P.rec, None
            pstate = [0]

            def pump(k, pend=pend, pstate=pstate):
                while k > 0 and pstate[0] < len(pend):
                    fn, a = pend[pstate[0]]
                    fn(*a)
                    pstate[0] += 1
                    k -= 1
            for gi in range(128 // GSL):
                rb, r_rb = ring[ring_i[0] % NRING]
                ring_i[0] += 1
                c0 = gi * GSL
                r_actw = r_actw_g[gi % 4]
                for sl in range(GSL):
                    cidx = c0 + sl
                    P.dma("pool", lambda e, rb=rb, sl=sl, cidx=cidx, eid=eid: e.indirect_dma_start(
                        out=rb[:, sl, :], out_offset=None, in_=tab,
                        in_offset=bass.IndirectOffsetOnAxis(ap=eid[:, cidx:cidx + 1], axis=0)),
                        reads=[r_eid, self.rTAB], writes=[r_rb])
                for sl in range(GSL):
                    cidx = c0 + sl
                    jk, r_jk = junks[cidx % 2]
                    P.op("dve", lambda e, rb=rb, sl=sl, cidx=cidx, h2=h2, jk=jk: e.scalar_tensor_tensor(
                        out=jk[:], in0=rb[:, sl, 0:D], scalar=1.0, in1=h2[:], op0=ALU.mult, op1=ALU.mult,
                        accum_out=dots[:, cidx:cidx + 1]),
                        reads=[r_rb, r_h2], writes=[r_jk, r_dots_s[gi % 4][sl]])
                    pump(1)
                P.op("act", lambda e, c0=c0: e.activation(out=actw[:, c0:c0 + GSL], in_=dots[:, c0:c0 + GSL], func=AF.Gelu_apprx_tanh),
                     reads=r_dots_s[gi % 4], writes=[r_actw])
                for sl in range(GSL):
                    cidx = c0 + sl
                    dg, r_dg = dgs[cidx % 4]
                    df, r_df = dfs[cidx % 4]
                    P.op("act", lambda e, df=df, cidx=cidx: e.activation(out=df[:], in_=ident[:], func=AF.Copy,
                                                                         scale=actw[:, cidx:cidx + 1]),
                         reads=[r_actw, self.r_ident], writes=[r_df])
                    P.op("act", lambda e, dg=dg, df=df, cidx=cidx, wts=wts: e.activation(out=dg[:], in_=df[:], func=AF.Copy,
                                                                                         scale=wts[:, cidx:cidx + 1]),
                         reads=[r_df, r_wts], writes=[r_dg])
                    for n2 in range(2):
                        bk, r_bk = accb[n2]
                        P.op("pe", lambda e, bk=bk, dg=dg, rb=rb, sl=sl, n2=n2, cidx=cidx: e.matmul(
                            bk[:], lhsT=dg[:], rhs=rb[:, sl, D + n2 * 512: D + (n2 + 1) * 512],
                            start=(cidx == 0), stop=(cidx == 127)),
                            reads=[r_dg, r_rb], writes=[r_bk], acc=(cidx > 0))
                    pump(1)
            if final_out and s >= TCTX:
                dst, r_dst = A["y"][b, (s - TCTX) * 128:(s - TCTX + 1) * 128, :], self.rY
            else:
                dst, r_dst = A["X0"][b, s * 128:(s + 1) * 128, :], self.rX0
            self.residual_ln_store(accb, xt, r_xt, row, sm, ytile, x1tile, dst, r_dst)
            pump(10 ** 9)
        P.bank_mod = 8
        P.end_phase()

    def build(self):
        self.consts()
        if self.dbg.get("consts_only"):
            self.P.close()
            return self.nc
        if not (self.dbg.get("mod_only") or "stop_after_mixer" in self.dbg):
            self.table_phase()
        for layer in self.dbg.get("layers", range(self.depth)):
            final = layer == self.depth - 1
            self.mod_phase(layer, 0)
            if self.dbg.get("mod_only"):
                break
            if layer % 2 == 0:
                self.cmlp_phase(layer)
            else:
                self.gla_phase(layer)
            if self.dbg.get("stop_after_mixer") == layer:
                break
            self.mod_phase(layer, 1)
            self.peer_phase(layer, final)
        self.P.close()
        return self.nc


_W_NAMES = ["w_mod", "b_mod", "ln_g", "ln_b", "a_w_in", "a_norm_g", "a_norm_b", "a_w_s", "a_b_s", "a_w_out",
            "b_w_in", "b_w_gate", "b_gate_bias", "b_gn_g", "b_w_out", "p_w_q", "p_keys", "p_u", "p_v"]


def make_in_maps(inputs, n_cores, nb):
    f = lambda a: np.ascontiguousarray(np.asarray(a, dtype=np.float32))
    shared = {k: f(inputs[k]) for k in _W_NAMES}
    shared["p_u"] = shared["p_u"].reshape(DEPTH * NEXP, D)
    shared["p_v"] = shared["p_v"].reshape(DEPTH * NEXP, D)
    shared["c_ctx"] = f(inputs["c_ctx"]).reshape(1, D)
    x, c, ctx = f(inputs["x"]), f(inputs["c"]), f(inputs["ctx"])
    maps = []
    for i in range(n_cores):
        m = dict(shared)
        m["x"] = x[i * nb:(i + 1) * nb]
        m["c"] = c[i * nb:(i + 1) * nb]
        m["ctx"] = ctx[i * nb:(i + 1) * nb]
        maps.append(m)
    return maps


def kernel(**inputs):
    nb = 2
    nc = Builder(nb=nb).build()
    in_maps = make_in_maps(inputs, NCORES, nb)
    res = run_bass_kernel_spmd(nc, in_maps, core_ids=list(range(NCORES)))
    return np.concatenate([r["y"] for r in res.results], axis=0).astype(np.float32)
```
